# Optimizing a Trainium2 kernel written in Bass

```python
import jax, jax.numpy as jnp
from jax import lax
import numpy as np

D_MODEL = 1024
BATCH = 8
SEQ = 4096
DEPTH = 2

HEAD_DIM = 64
N_RET_HEADS = D_MODEL // (2 * HEAD_DIM)
N_SB_HEADS = D_MODEL // (2 * HEAD_DIM)
N_FOX_HEADS = D_MODEL // HEAD_DIM
RET_WIDTH = N_RET_HEADS * HEAD_DIM
SB_WIDTH = N_SB_HEADS * HEAD_DIM
FOX_WIDTH = N_FOX_HEADS * HEAD_DIM
EVEN_MIX_WIDTH = RET_WIDTH + SB_WIDTH
EVEN_IN_SIZES = (RET_WIDTH, RET_WIDTH, RET_WIDTH, RET_WIDTH, SB_WIDTH, SB_WIDTH, SB_WIDTH)
EVEN_IN_WIDTH = sum(EVEN_IN_SIZES)
ODD_IN_SIZES = (FOX_WIDTH, FOX_WIDTH, FOX_WIDTH, N_FOX_HEADS)
ODD_IN_WIDTH = sum(ODD_IN_SIZES)
D_FF = ((8 * D_MODEL // 3 + 127) // 128) * 128
N_EXPERTS = 8
TOP_K = 2
BLOCK = 128
CHUNK = 128
ROPE_BASE = 10000.0
NORM_EPS = 1e-6
GROUP_NORM_EPS = 1e-5
FORGET_BIAS_CENTER = 2.0
N_EVEN = (DEPTH + 1) // 2
N_ODD = DEPTH // 2

kernel_name = "hybrid_retention_stickbreak_fox_moe"


def _split_points(sizes):
    return [int(v) for v in np.cumsum(sizes)[:-1]]


def _rms_norm(x, gain):
    xf = x.astype(jnp.float32)
    y = xf * lax.rsqrt(jnp.mean(xf * xf, axis=-1, keepdims=True) + NORM_EPS)
    return (y * gain.astype(jnp.float32)).astype(x.dtype)


def _split_heads(t, n_heads):
    b, s, _ = t.shape
    return t.reshape(b, s, n_heads, HEAD_DIM).transpose(0, 2, 1, 3)


def _merge_heads(t):
    b, h, s, d = t.shape
    return t.transpose(0, 2, 1, 3).reshape(b, s, h * d)


def _rotary(t):
    s, d = t.shape[2], t.shape[3]
    half = d // 2
    inv_freq = ROPE_BASE ** (-jnp.arange(half, dtype=jnp.float32) / half)
    ang = jnp.arange(s, dtype=jnp.float32)[:, None] * inv_freq[None, :]
    cos, sin = jnp.cos(ang), jnp.sin(ang)
    t1, t2 = t[..., :half], t[..., half:]
    return jnp.concatenate([t1 * cos - t2 * sin, t1 * sin + t2 * cos], axis=-1)


def _retention_chunkwise(q, k, v):
    b, h, s, d = q.shape
    nc = s // CHUNK
    log_gamma = jnp.log(1.0 - 2.0 ** (-5.0 - jnp.arange(h, dtype=jnp.float32)))
    pos = jnp.arange(CHUNK, dtype=jnp.float32)
    diff = pos[:, None] - pos[None, :]
    intra_decay = jnp.where(diff >= 0.0,
                            jnp.exp(log_gamma[:, None, None] * jnp.maximum(diff, 0.0)),
                            0.0)
    q_decay = jnp.exp(log_gamma[:, None] * (pos + 1.0))
    k_decay = jnp.exp(log_gamma[:, None] * (CHUNK - 1.0 - pos))
    chunk_decay = jnp.exp(log_gamma * CHUNK)
    qc = q.reshape(b, h, nc, CHUNK, d)
    kc = k.reshape(b, h, nc, CHUNK, d)
    vc = v.reshape(b, h, nc, CHUNK, d)
    scores = jnp.einsum("bhnqd,bhnkd->bhnqk", qc, kc) * intra_decay[None, :, None]
    inner = jnp.einsum("bhnqk,bhnke->bhnqe", scores, vc)
    kv = jnp.einsum("bhnkd,bhnke->nbhde", kc * k_decay[None, :, None, :, None], vc)

    def step(state, kv_n):
        return state * chunk_decay[None, :, None, None] + kv_n, state

    _, states = lax.scan(step, jnp.zeros((b, h, d, d), jnp.float32), kv)
    cross = jnp.einsum("bhnqd,nbhde->bhnqe", qc * q_decay[None, :, None, :, None], states)
    return (inner + cross).reshape(b, h, s, d)


def _stick_breaking_attention(q, k, v):
    b, h, s, d = q.shape
    scale = d ** -0.5
    outs = []
    for blk in range(s // BLOCK):
        lo, hi = blk * BLOCK, (blk + 1) * BLOCK
        z = jnp.einsum("bhqd,bhkd->bhqk", q[:, :, lo:hi], k[:, :, :hi]) * scale
        q_pos = lo + jnp.arange(BLOCK)
        k_pos = jnp.arange(hi)
        strict = k_pos[None, :] < q_pos[:, None]
        log_fail = jnp.where(strict, jax.nn.log_sigmoid(-z), 0.0)
        later_fail = lax.cumsum(log_fail, axis=3, reverse=True) - log_fail
        weights = jnp.where(strict, jnp.exp(jax.nn.log_sigmoid(z) + later_fail), 0.0)
        outs.append(jnp.einsum("bhqk,bhkd->bhqd", weights, v[:, :, :hi]))
    return jnp.concatenate(outs, axis=2)


def _forgetting_attention(q, k, v, log_f):
    b, h, s, d = q.shape
    scale = d ** -0.5
    cum = jnp.cumsum(log_f, axis=-1)
    outs = []
    for blk in range(s // BLOCK):
        lo, hi = blk * BLOCK, (blk + 1) * BLOCK
        z = jnp.einsum("bhqd,bhkd->bhqk", q[:, :, lo:hi], k[:, :, :hi]) * scale
        logits = z + cum[:, :, lo:hi, None] - cum[:, :, None, :hi]
        q_pos = lo + jnp.arange(BLOCK)
        k_pos = jnp.arange(hi)
        causal = k_pos[None, :] <= q_pos[:, None]
        probs = jax.nn.softmax(jnp.where(causal, logits, -jnp.inf), axis=-1)
        outs.append(jnp.einsum("bhqk,bhkd->bhqd", probs, v[:, :, :hi]))
    return jnp.concatenate(outs, axis=2)


def _even_mixer(h, w_in, ret_norm, w_out):
    f32 = jnp.float32
    proj = jnp.einsum("bsm,mn->bsn", h, w_in)
    q_r, k_r, v_r, g_r, q_s, k_s, v_s = jnp.split(proj, _split_points(EVEN_IN_SIZES), axis=-1)
    q_r = _rotary(_split_heads(q_r, N_RET_HEADS).astype(f32))
    k_r = _rotary(_split_heads(k_r, N_RET_HEADS).astype(f32)) * (HEAD_DIM ** -0.5)
    y_r = _retention_chunkwise(q_r, k_r, _split_heads(v_r, N_RET_HEADS).astype(f32))
    mu = jnp.mean(y_r, axis=-1, keepdims=True)
    var = jnp.mean(jnp.square(y_r - mu), axis=-1, keepdims=True)
    y_r = _merge_heads((y_r - mu) * lax.rsqrt(var + GROUP_NORM_EPS))
    y_r = y_r * ret_norm.astype(f32) * jax.nn.silu(g_r.astype(f32))
    y_s = _merge_heads(_stick_breaking_attention(
        _split_heads(q_s, N_SB_HEADS).astype(f32),
        _split_heads(k_s, N_SB_HEADS).astype(f32),
        _split_heads(v_s, N_SB_HEADS).astype(f32)))
    y = jnp.concatenate([y_r, y_s], axis=-1).astype(h.dtype)
    return jnp.einsum("bsm,md->bsd", y, w_out)


def _odd_mixer(h, w_in, b_forget, w_out):
    f32 = jnp.float32
    proj = jnp.einsum("bsm,mn->bsn", h, w_in)
    q, k, v, f_logit = jnp.split(proj, _split_points(ODD_IN_SIZES), axis=-1)
    log_f = jax.nn.log_sigmoid(f_logit.astype(f32) + b_forget.astype(f32)).transpose(0, 2, 1)
    y = _forgetting_attention(_split_heads(q, N_FOX_HEADS).astype(f32),
                              _split_heads(k, N_FOX_HEADS).astype(f32),
                              _split_heads(v, N_FOX_HEADS).astype(f32), log_f)
    return jnp.einsum("bsm,md->bsd", _merge_heads(y).astype(h.dtype), w_out)


def _swiglu(h, w_gate, w_up, w_down):
    a = jax.nn.silu(jnp.einsum("bsd,df->bsf", h, w_gate)) * jnp.einsum("bsd,df->bsf", h, w_up)
    return jnp.einsum("bsf,fd->bsd", a, w_down)


def _moe_swiglu(h, w_router, w_gate, w_up, w_down):
    b, s, dm = h.shape
    t = h.reshape(b * s, dm)
    logits = jnp.einsum("td,de->te", t, w_router).astype(jnp.float32)
    top_vals, top_idx = lax.top_k(logits, TOP_K)
    gates = jax.nn.softmax(top_vals, axis=-1)
    combine = jnp.sum(jax.nn.one_hot(top_idx, N_EXPERTS, dtype=jnp.float32) * gates[..., None], axis=1)
    out = jnp.zeros((b * s, dm), jnp.float32)
    for e in range(N_EXPERTS):
        a = jax.nn.silu(t @ w_gate[e]) * (t @ w_up[e])
        out = out + combine[:, e:e + 1] * (a @ w_down[e]).astype(jnp.float32)
    return out.reshape(b, s, dm).astype(h.dtype)


def _normal(key, shape, scale):
    return jax.random.normal(key, shape, jnp.float32) * scale


def setup_inputs(seed: int = 0) -> dict:
    key = jax.random.key(seed)
    ks = jax.random.split(key, 20)
    d = D_MODEL
    return {
        "x": _normal(ks[0], (BATCH, SEQ, d), 1.0),
        "attn_norm_even": 1.0 + _normal(ks[1], (N_EVEN, d), 0.02),
        "w_in_even": _normal(ks[2], (N_EVEN, d, EVEN_IN_WIDTH), d ** -0.5),
        "ret_norm_even": 1.0 + _normal(ks[3], (N_EVEN, RET_WIDTH), 0.02),
        "w_out_even": _normal(ks[4], (N_EVEN, EVEN_MIX_WIDTH, d), EVEN_MIX_WIDTH ** -0.5),
        "ffn_norm_even": 1.0 + _normal(ks[5], (N_EVEN, d), 0.02),
        "w_gate_even": _normal(ks[6], (N_EVEN, d, D_FF), d ** -0.5),
        "w_up_even": _normal(ks[7], (N_EVEN, d, D_FF), d ** -0.5),
        "w_down_even": _normal(ks[8], (N_EVEN, D_FF, d), D_FF ** -0.5),
        "attn_norm_odd": 1.0 + _normal(ks[9], (N_ODD, d), 0.02),
        "w_in_odd": _normal(ks[10], (N_ODD, d, ODD_IN_WIDTH), d ** -0.5),
        "b_forget_odd": FORGET_BIAS_CENTER + _normal(ks[11], (N_ODD, N_FOX_HEADS), 0.1),
        "w_out_odd": _normal(ks[12], (N_ODD, FOX_WIDTH, d), FOX_WIDTH ** -0.5),
        "ffn_norm_odd": 1.0 + _normal(ks[13], (N_ODD, d), 0.02),
        "w_router_odd": _normal(ks[14], (N_ODD, d, N_EXPERTS), d ** -0.5),
        "w_gate_moe_odd": _normal(ks[15], (N_ODD, N_EXPERTS, d, D_FF), d ** -0.5),
        "w_up_moe_odd": _normal(ks[16], (N_ODD, N_EXPERTS, d, D_FF), d ** -0.5),
        "w_down_moe_odd": _normal(ks[17], (N_ODD, N_EXPERTS, D_FF, d), D_FF ** -0.5),
        "final_norm": 1.0 + _normal(ks[18], (d,), 0.02),
    }


def reference(x, attn_norm_even, w_in_even, ret_norm_even, w_out_even, ffn_norm_even,
              w_gate_even, w_up_even, w_down_even, attn_norm_odd, w_in_odd, b_forget_odd,
              w_out_odd, ffn_norm_odd, w_router_odd, w_gate_moe_odd, w_up_moe_odd,
              w_down_moe_odd, final_norm):
    h = x
    for layer in range(DEPTH):
        i = layer // 2
        if layer % 2 == 0:
            h = h + _even_mixer(_rms_norm(h, attn_norm_even[i]), w_in_even[i],
                                ret_norm_even[i], w_out_even[i]).astype(h.dtype)
            h = h + _swiglu(_rms_norm(h, ffn_norm_even[i]), w_gate_even[i],
                            w_up_even[i], w_down_even[i]).astype(h.dtype)
        else:
            h = h + _odd_mixer(_rms_norm(h, attn_norm_odd[i]), w_in_odd[i],
                               b_forget_odd[i], w_out_odd[i]).astype(h.dtype)
            h = h + _moe_swiglu(_rms_norm(h, ffn_norm_odd[i]), w_router_odd[i],
                                w_gate_moe_odd[i], w_up_moe_odd[i],
                                w_down_moe_odd[i]).astype(h.dtype)
    return _rms_norm(h, final_norm)
```

```python
import numpy as np
import concourse.bass as bass
import concourse.mybir as mybir
from concourse.bass_utils import run_bass_kernel_spmd

F32 = mybir.dt.float32
BF16 = mybir.dt.bfloat16
AF = mybir.ActivationFunctionType
ALU = mybir.AluOpType
AX = mybir.AxisListType

NDSEM = {"sp": 12, "pool": 28, "act": 1, "dve": 1, "pe": 1}


FUSE_WAIT = True


class Buf:
    __slots__ = ("w", "r")

    def __init__(self):
        self.w = None
        self.r = []


class Prog:
    ENGS = ("pe", "act", "dve", "pool", "sp")

    def __init__(self, nc):
        self.nc = nc
        self.lists = {e: [] for e in self.ENGS}
        self.sb_n = 0
        arena_bytes = nc.sbuf_bytes_remaining - 6144
        arena = nc.alloc_sbuf_tensor("arena", [128, arena_bytes], mybir.dt.uint8)
        self.sb_base = nc.lookup_mloc(arena).addr
        self.sb_off = self.sb_base
        self.sb_top = self.sb_base + arena_bytes
        self.out_events = []
        self.cur_grp = None
        self.replay = None
        self.replay_store = {}
        self.grp_flag = {}

    def begin_replay(self, key):
        first = key not in self.replay_store
        if first:
            self.replay_store[key] = []
        self.replay = [self.replay_store[key], 0, first]
        return first

    def end_replay(self):
        self.replay = None

    def _replayed(self, make):
        if self.replay is None:
            return make()
        store, idx, first = self.replay
        if first:
            obj = make()
            store.append(obj)
        else:
            obj = store[idx]
        self.replay[1] = idx + 1
        return obj

    def buf(self):
        return self._replayed(Buf)

    def sb(self, shape, dtype, off=None, name=None):
        return self._replayed(lambda: self._sb(shape, dtype, off, name))

    def _sb(self, shape, dtype, off=None, name=None):
        esz = 2 if dtype == BF16 else 4
        n = 1
        for s in shape[1:]:
            n *= s
        nbytes = n * esz
        if off is None:
            off = (self.sb_off + 63) // 64 * 64
            self.sb_off = off + nbytes
        assert off >= self.sb_base and off + nbytes <= self.sb_top, (off, nbytes, self.sb_top)
        self.sb_n += 1
        t = self.nc.alloc_sbuf_tensor_at(name or f"sb{self.sb_n}", list(shape), dtype, offset=off)
        return t

    def op(self, eng, fn, reads=(), writes=(), dma=False, is_out=False):
        lst = self.lists[eng]
        idx = len(lst)
        ev = ("dma", eng, idx) if dma else ("c", eng, idx)
        deps = set()
        for b in reads:
            if b.w is not None:
                deps.add(b.w)
        for b in writes:
            if b.w is not None:
                deps.add(b.w)
            for r in b.r:
                deps.add(r)
        if not dma:
            war_only = set()
            for b in writes:
                for r in b.r:
                    if r[0] == "c" and r[1] == eng:
                        war_only.add(r)
            for b in reads:
                if b.w in war_only:
                    war_only.discard(b.w)
            for b in writes:
                if b.w in war_only:
                    war_only.discard(b.w)
            if eng == "pe":
                deps -= war_only
            if eng == "pe":
                deps = {d for d in deps if not (d[0] == "c" and d[1] == "pe")}
        deps.discard(ev)
        lst.append({"fn": fn, "deps": deps, "dma": dma, "marked": False, "grp": self.cur_grp})
        for b in reads:
            b.r.append(ev)
        for b in writes:
            b.w = ev
            b.r = []
        if is_out:
            self.out_events.append(ev)
        return ev

    def barrier(self):
        evs = set()
        for e in self.ENGS:
            lst = self.lists[e]
            last_c = None
            for i in range(len(lst) - 1, -1, -1):
                if not lst[i]["dma"] and lst[i]["fn"] is not None:
                    last_c = ("c", e, i)
                    break
            if last_c:
                evs.add(last_c)
            for i, r in enumerate(lst):
                if r["dma"] and not r.get("barriered"):
                    evs.add(("dma", e, i))
                    r["barriered"] = True
        for e in self.ENGS:
            self.lists[e].append({"fn": None, "deps": {d for d in evs if not (d[0] == "c" and d[1] == e)},
                                  "dma": False, "marked": False})

    def emit(self):
        nc = self.nc
        lists = self.lists
        for e in self.ENGS:
            seen_c = {}
            seen_d = set()
            for rec in lists[e]:
                best = {}
                dd = set()
                for d in rec["deps"]:
                    if d[0] == "c":
                        if d[2] > best.get(d[1], -1):
                            best[d[1]] = d[2]
                    else:
                        dd.add(d)
                waits = []
                for e2, i2 in best.items():
                    if seen_c.get(e2, -1) >= i2:
                        continue
                    seen_c[e2] = i2
                    waits.append(("c", e2, i2))
                    lists[e2][i2]["marked"] = True
                for d in dd:
                    if d in seen_d:
                        continue
                    seen_d.add(d)
                    waits.append(d)
                rec["waits"] = waits
        fin = []
        for d in self.out_events:
            fin.append(d)
        csem = {e: nc.alloc_semaphore(f"c_{e}") for e in self.ENGS}
        dsem = {e: [nc.alloc_semaphore(f"d_{e}{k}") for k in range(NDSEM[e])] for e in self.ENGS}
        for e in self.ENGS:
            cnt = 0
            nd = 0
            tot = [0] * NDSEM[e]
            for rec in lists[e]:
                if rec["dma"]:
                    slot = nd % NDSEM[e]
                    rec["slot"] = slot
                    rec["prev"] = tot[slot]
                    tot[slot] += 16
                    rec["val"] = tot[slot]
                    nd += 1
                elif rec["marked"]:
                    cnt += 1
                    rec["val"] = cnt
        engobj = {"pe": "tensor", "act": "scalar", "dve": "vector", "pool": "gpsimd", "sp": "sync"}

        def emit_eng(e, eng):
            lst = lists[e]
            reg = None
            n = len(lst)
            k = 0
            while k < n:
                g = lst[k].get("grp")
                k2 = k
                while k2 < n and lst[k2].get("grp") == g:
                    k2 += 1
                seg = lst[k:k2]
                if g is None:
                    for rec in seg:
                        emit_rec(e, eng, rec)
                else:
                    if reg is None:
                        reg = eng.alloc_register(f"flag_{e}")
                    eng.reg_load(reg, self.grp_flag[g])
                    gd = eng.If_ne(reg, 0)
                    gd.__enter__()
                    for rec in seg:
                        emit_rec(e, eng, rec)
                    gd.__exit__(None, None, None)
                    ncomp = sum(1 for rec in seg if (not rec["dma"]) and rec["marked"] and rec["fn"] is not None)
                    dcomp = {}
                    for rec in seg:
                        if rec["dma"] and rec["fn"] is not None:
                            dcomp[rec["slot"]] = dcomp.get(rec["slot"], 0) + 16
                    if ncomp or dcomp:
                        ge = eng.Else()
                        ge.__enter__()
                        if ncomp:
                            eng.sem_inc(csem[e], ncomp)
                        for sl, v in dcomp.items():
                            eng.sem_inc(dsem[e][sl], v)
                        ge.__exit__(None, None, None)
                k = k2
            if e == "sp":
                for w in fin:
                    r2 = lists[w[1]][w[2]]
                    eng.wait_ge(dsem[w[1]][r2["slot"]], r2["val"])

        def emit_rec(e, eng, rec):
            if True:
                wl = []
                for w in rec["waits"]:
                    r2 = lists[w[1]][w[2]]
                    if w[0] == "c":
                        wl.append((csem[w[1]], r2["val"]))
                    else:
                        wl.append((dsem[w[1]][r2["slot"]], r2["val"]))
                if rec["dma"] and rec["fn"] is not None and rec["prev"] > 0:
                    wl.append((dsem[e][rec["slot"]], rec["prev"]))
                fuse = None
                if FUSE_WAIT and rec["fn"] is not None and not rec["dma"] and wl:
                    fuse = wl.pop()
                for sm_, v_ in wl:
                    eng.wait_ge(sm_, v_)
                if rec["fn"] is None:
                    return
                ins = rec["fn"](eng)
                if fuse is not None:
                    ins._wait_ge(fuse[0], fuse[1])
                if rec["dma"]:
                    ins.then_inc(dsem[e][rec["slot"]], 16)
                elif rec["marked"]:
                    ins.then_inc(csem[e], 1)

        with nc.Block() as block:
            @block.tensor
            def _(eng):
                emit_eng("pe", eng)

            @block.scalar
            def _(eng):
                emit_eng("act", eng)

            @block.vector
            def _(eng):
                emit_eng("dve", eng)

            @block.gpsimd
            def _(eng):
                emit_eng("pool", eng)

            @block.sync
            def _(eng):
                emit_eng("sp", eng)

D = 1024
NCH = 8
DFF = 2816
NF = 22
NE = 8
EPS = 1e-6
GN_EPS = 1e-5

C_ID, C_ONE, C_TRI, C_MS, C_MI, C_BD, C_BM, C_PM = [i * 128 for i in range(8)]


def host_consts(S):
    i = np.arange(128)
    cst = np.zeros((128, 1024), np.float32)
    cst[:, C_ID:C_ID + 128] = np.eye(128)
    cst[:, C_ONE:C_ONE + 128] = 1.0
    cst[:, C_TRI:C_TRI + 128] = (i[:, None] >= i[None, :])
    cst[:, C_MS:C_MS + 128] = (i[:, None] < i[None, :])
    cst[:, C_MI:C_MI + 128] = (i[:, None] <= i[None, :])
    blk = (i[:, None] // 64 == i[None, :] // 64)
    cst[:, C_BD:C_BD + 128] = blk / 64.0
    cst[:, C_BM:C_BM + 128] = blk
    partner = (i // 64) * 64 + (i % 64 + 32) % 64
    pm = np.zeros((128, 128), np.float32)
    pm[partner, i] = 1.0
    cst[:, C_PM:C_PM + 128] = pm
    half = 32
    inv_freq = (10000.0 ** (-np.arange(half, dtype=np.float32) / half)).astype(np.float32)
    ang = (np.arange(S, dtype=np.float32)[:, None] * inv_freq[None, :]).astype(np.float32)
    cos = np.cos(ang).astype(np.float32).T
    sin = np.sin(ang).astype(np.float32).T
    d = i % 64
    cosT = cos[d % 32]
    sinS = np.where((d < 32)[:, None], -sin[d % 32], sin[d % 32])
    rot = np.stack([cosT, sinS, cosT * 0.125, sinS * 0.125]).astype(np.float32)
    lg = np.log(1.0 - 2.0 ** (-5.0 - np.arange(8, dtype=np.float32))).astype(np.float32)
    pos = np.arange(128, dtype=np.float32)
    rdt = np.zeros((4, 128, 256), np.float32)
    rkd = np.zeros((4, 128, 128), np.float32)
    rqd = np.zeros((4, 128, 512), np.float32)
    rcd = np.zeros((128, 4), np.float32)
    for j in range(4):
        for hh in range(2):
            g = lg[2 * j + hh]
            diff = pos[None, :] - pos[:, None]
            rdt[j, :, hh * 128:(hh + 1) * 128] = np.where(diff >= 0, np.exp(g * np.maximum(diff, 0)), 0.0)
            rkd[j, :, hh * 64:(hh + 1) * 64] = np.exp(g * (127.0 - pos))[:, None]
            rqd[j, hh * 64:(hh + 1) * 64, :] = np.tile(np.exp(g * (pos + 1.0)), 4)[None, :]
            rcd[hh * 64:(hh + 1) * 64, j] = np.exp(g * 128.0)
    iot = (np.arange(22, dtype=np.float32)[None, :] * 128.0 + np.arange(128, dtype=np.float32)[:, None]).astype(np.float32)
    return {"cst": cst, "rot": rot, "rdt": rdt, "rkd": rkd, "rqd": rqd, "rcd": rcd, "iot": iot}


SB_WIDE = False
GT_OVERRIDE = None
SKIP_SLOTS = False
DEBUG_ZERO_FLAGS = False


def build(S, stop=None, sparse=True, nfill_sb=0, nfill_fox=0):
    nc = bass.Bass("TRN2", target_bir_lowering=False)
    P = Prog(nc)
    NT = S // 512
    NB = S // 128
    GT = GT_OVERRIDE or (2 if NT >= 2 else 1)
    NG = NT // GT

    def din(name, shape):
        return nc.dram_tensor(name, list(shape), F32, kind="ExternalInput").ap()

    def dscr(name, shape, dt):
        return nc.dram_tensor(name, list(shape), dt).ap()

    xT = din("xT", [D, S])
    outT = None if (sparse and stop is None) else nc.dram_tensor("outT", [D, S], F32, kind="ExternalOutput").ap()
    gains = {n: din(n, [128, 8]) for n in ("an_e", "fn_e", "an_o", "fn_o", "fin")}
    rn_e = din("rn_e", [128, 4])
    bf_o = din("bf_o", [16, 1])
    wr_o = din("wr_o", [D, 8])
    wsrc = {
        "w_in_e": din("w_in_e", [D, 3584]), "w_out_e": din("w_out_e", [D, D]),
        "wg_e": din("wg_e", [D, DFF]), "wu_e": din("wu_e", [D, DFF]), "wd_e": din("wd_e", [DFF, D]),
        "w_in_o": din("w_in_o", [D, 3088]), "w_out_o": din("w_out_o", [D, D]),
        "wg_m": din("wg_m", [NE, D, DFF]), "wu_m": din("wu_m", [NE, D, DFF]), "wd_m": din("wd_m", [NE, DFF, D]),
    }
    cst_d = din("cst", [128, 1024])
    rot_d = din("rot", [4, 128, S])
    rdt_d = din("rdt", [4, 128, 256])
    rkd_d = din("rkd", [4, 128, 128])
    rqd_d = din("rqd", [4, 128, 512])
    rcd_d = din("rcd", [128, 4])
    iot_d = din("iot", [128, 22])
    fin_row = din("fin_row", [1, D])
    NS = (2 * S) // 512 + NE
    HF = DFF // 2
    out_nat = nc.dram_tensor("out", [S, D], F32, kind="ExternalOutput").ap() if (sparse and stop is None) else None
    wb = {k: dscr(k + "_b", v.shape, BF16) for k, v in wsrc.items() if not (sparse and k in ("wg_m", "wu_m", "wd_m"))}
    if sparse:
        wb["wg_m"] = [dscr(f"wgm2_{h}", [NE * 128, 8 * HF], BF16) for h in range(2)]
        wb["wu_m"] = [dscr(f"wum2_{h}", [NE * 128, 8 * HF], BF16) for h in range(2)]
        wb["wd_m"] = [dscr(f"wdm2_{h}", [NE * 128, (NF // 2) * D], BF16) for h in range(2)]
        Hn_d = dscr("Hn", [S, D], BF16)
        H3_d = dscr("H3tok", [S, D], F32)
        Xs_d = dscr("Xs", [NS * 512, D], BF16)
        Ys_d = dscr("Ys", [NS * 512, D], F32)
    wbuf = {}
    h1T = dscr("h1T", [D, S], F32)
    h2T = dscr("h2T", [D, S], F32)
    h3T = dscr("h3T", [D, S], F32)
    h4T = dscr("h4T", [D, S], F32)
    hb = {id(t): Buf() for t in (h1T, h2T, h3T, h4T, outT, xT)}
    nullb = Buf()

    def dma(q, out, in_, reads=(), writes=(), is_out=False, **kw):
        return P.op(q, lambda e: e.dma_start(out=out, in_=in_, **kw), reads=reads, writes=writes, dma=True, is_out=is_out)

    def mm(out, lhsT, rhs, start, stop, reads, writes, skip=False):
        if skip:
            return P.op("pe", lambda e: e.matmul(out, lhsT=lhsT, rhs=rhs, start=start, stop=stop, skip_group_check=True),
                        reads=reads, writes=writes)
        return P.op("pe", lambda e: e.matmul(out, lhsT=lhsT, rhs=rhs, start=start, stop=stop), reads=reads, writes=writes)

    def act(out, in_, func, reads, writes, bias=None, scale=None):
        kw = {}
        if bias is not None:
            kw["bias"] = bias
        if scale is not None:
            kw["scale"] = scale
        return P.op("act", lambda e: e.activation(out=out, in_=in_, func=func, **kw), reads=reads, writes=writes)

    def tt(out, in0, in1, op, reads, writes, eng="dve"):
        return P.op(eng, lambda e: e.tensor_tensor(out=out, in0=in0, in1=in1, op=op), reads=reads, writes=writes)

    def ts(out, in0, s1, op0, reads, writes, s2=None, op1=None, eng="dve"):
        if op1 is None:
            return P.op(eng, lambda e: e.tensor_scalar(out=out, in0=in0, scalar1=s1, scalar2=None, op0=op0), reads=reads, writes=writes)
        return P.op(eng, lambda e: e.tensor_scalar(out=out, in0=in0, scalar1=s1, scalar2=s2, op0=op0, op1=op1), reads=reads, writes=writes)

    def stt(out, in0, scalar, in1, op0, op1, reads, writes, eng="dve"):
        return P.op(eng, lambda e: e.scalar_tensor_tensor(out=out, in0=in0, scalar=scalar, in1=in1, op0=op0, op1=op1),
                    reads=reads, writes=writes)

    def cp(out, in_, reads, writes, eng="dve"):
        if eng == "act":
            return P.op("act", lambda e: e.copy(out=out, in_=in_), reads=reads, writes=writes)
        return P.op(eng, lambda e: e.tensor_copy(out=out, in_=in_), reads=reads, writes=writes)

    def recip(out, in_, reads, writes):
        return P.op("dve", lambda e: e.reciprocal(out=out, in_=in_), reads=reads, writes=writes)

    def cast_w(name):
        src, dst = wsrc[name], wb[name]
        if len(src.shape) == 3:
            bl = []
            for e_ in range(src.shape[0]):
                b = Buf()
                dma("pool", dst[e_], src[e_], writes=[b], max_dma_last_dim=4096)
                bl.append(b)
            wbuf[name] = bl
        else:
            b = Buf()
            dma("pool", dst, src, writes=[b], max_dma_last_dim=4096)
            wbuf[name] = b

    for name in ("w_in_e", "w_out_e"):
        cast_w(name)
    for name in ("wg_m", "wu_m", "wd_m"):
        wbuf[name] = []

    def cast_expert(e_):
        for name in ("wg_m", "wu_m", "wd_m"):
            if not sparse:
                b = Buf()
                dma("pool", wb[name][e_], wsrc[name][e_], writes=[b], max_dma_last_dim=4096)
                wbuf[name].append(b)
                continue
            for h in range(2):
                b = Buf()
                if name == "wd_m":
                    for fl in range(NF // 2):
                        r0 = h * HF + fl * 128
                        dma("pool", wb[name][h][e_ * 128:(e_ + 1) * 128, fl * D:(fl + 1) * D], wsrc[name][e_][r0:r0 + 128, :], writes=[b],
                            max_dma_last_dim=4096)
                else:
                    for c in range(8):
                        dma("pool", wb[name][h][e_ * 128:(e_ + 1) * 128, c * HF:(c + 1) * HF],
                            wsrc[name][e_][c * 128:(c + 1) * 128, h * HF:(h + 1) * HF], writes=[b], max_dma_last_dim=2816)
                wbuf[name].append(b)

    cst = P.sb([128, 1024], F32)
    cstb = P.sb([128, 1024], BF16)
    cb = Buf()
    gsb = {n: P.sb([128, 8], F32) for n in gains}
    rn_sb = P.sb([128, 4], F32)
    rcd_sb = P.sb([128, 4], F32)
    gb = Buf()
    dma("sp", cst[:], cst_d, writes=[cb])
    cp(cstb[:], cst[:], [cb], [cb])
    for n in gains:
        dma("sp", gsb[n][:], gains[n], writes=[gb])
    dma("sp", rn_sb[:], rn_e, writes=[gb])
    dma("sp", rcd_sb[:], rcd_d, writes=[gb])
    ident = cst[:, C_ID:C_ID + 128]
    ones32 = cst[:, C_ONE:C_ONE + 128]
    blockmask = cst[:, C_BM:C_BM + 128]
    pm32 = cst[:, C_PM:C_PM + 128]
    identb = cstb[:, C_ID:C_ID + 128]
    onesb = cstb[:, C_ONE:C_ONE + 128]
    trib = cstb[:, C_TRI:C_TRI + 128]
    maskSb = cstb[:, C_MS:C_MS + 128]
    maskIb = cstb[:, C_MI:C_MI + 128]
    maskS32 = cst[:, C_MS:C_MS + 128]
    bdb = cstb[:, C_BD:C_BD + 128]

    psall = nc.alloc_psum_tensor("psall", [128, 4096], F32)
    ps = [psall[:, i * 512:(i + 1) * 512] for i in range(8)]
    pb = [Buf() for _ in range(8)]
    persist_mark = P.sb_off
    if stop == "cast":
        hb[id(xT)] = nullb
        finish_early = True
    else:
        finish_early = False

    def view_T(dr):
        return dr.rearrange("(c p) s -> p c s", p=128)

    def finish(src):
        mark = P.sb_off
        xb_ = [(P.sb([128, 8, 512], F32), Buf()) for _ in range(2)]
        for t in range(NT):
            xs, xs_b = xb_[t % 2]
            dma("sp", xs[:], view_T(src)[:, :, t * 512:(t + 1) * 512], reads=[hb[id(src)]], writes=[xs_b])
            dma("sp", view_T(outT)[:, :, t * 512:(t + 1) * 512], xs[:], reads=[xs_b], writes=[hb[id(outT)]], is_out=True)
        P.sb_off = mark


    def norm_tiles(src, src_b, gain, tiles, dst_fn, pbank, xbufs, sqb, rsb, keep32=None):
        sq, sq_b = sqb
        r, r_b = rsb
        for n, t in enumerate(tiles):
            xs, xs_b = xbufs[n % len(xbufs)]
            dma("sp", xs[:], view_T(src)[:, :, t * 512:(t + 1) * 512], reads=[src_b], writes=[xs_b])
            act(sq[:], xs[:], AF.Square, [xs_b], [sq_b])
            for c in range(8):
                mm(ps[pbank][:], onesb, sq[:, c, :], c == 0, c == 7, [cb, sq_b], [pb[pbank]])
            act(r[:], ps[pbank][:], AF.Sqrt, [pb[pbank]], [r_b], bias=EPS, scale=1.0 / D)
            recip(r[:], r[:], [r_b], [r_b])
            o, o_b = dst_fn(t)
            for c in range(8):
                stt(o[:, c, :], xs[:, c, :], gain[:, c:c + 1], r[:], ALU.mult, ALU.mult, [xs_b, r_b, gb], [o_b])
            if keep32 is not None:
                keep32(t, xs, xs_b, r, r_b)

    def out_proj(yT, y_b, wname, res, dst):
        mark = P.sb_off
        wo = P.sb([128, 8, D], BF16)
        wo_b = Buf()
        dma("sp", wo[:], wb[wname].rearrange("(c p) n -> p c n", p=128), reads=[wbuf[wname]], writes=[wo_b])
        xb2 = [(P.sb([128, 8, 512], F32), Buf()) for _ in range(2)]
        def load_res(t):
            xs, xs_b = xb2[t % 2]
            dma("sp", xs[:], view_T(res)[:, :, t * 512:(t + 1) * 512], reads=[hb.get(id(res), nullb)], writes=[xs_b])

        load_res(0)
        for t in range(NT):
            xs, xs_b = xb2[t % 2]
            if t + 1 < NT:
                load_res(t + 1)
            for m in range(8):
                bk = m % 2
                for c in range(8):
                    mm(ps[bk][:], wo[:, c, m * 128:(m + 1) * 128], yT[:, c, t * 512:(t + 1) * 512], c == 0, c == 7,
                       [wo_b, y_b], [pb[bk]])
                tt(xs[:, m, :], ps[bk][:], xs[:, m, :], ALU.add, [pb[bk], xs_b], [xs_b])
            dma("sp", view_T(dst)[:, :, t * 512:(t + 1) * 512], xs[:], reads=[xs_b], writes=[hb[id(dst)]])
        P.sb_off = mark

    if finish_early:
        xb_ = [(P.sb([128, 8, 512], F32), Buf()) for _ in range(2)]
        for t in range(NT):
            xs, xs_b = xb_[t % 2]
            dma("sp", xs[:], view_T(xT)[:, :, t * 512:(t + 1) * 512], writes=[xs_b])
            dma("sp", view_T(outT)[:, :, t * 512:(t + 1) * 512], xs[:], reads=[xs_b], writes=[hb[id(outT)]], is_out=True)
        P.barrier()
        P.emit()
        return nc
    hnT = P.sb([128, 8, S], BF16)
    hn_b = Buf()
    yT = P.sb([128, 8, S], BF16)
    y_b = Buf()
    l0_mark = P.sb_off
    xbufs = [(P.sb([128, 8, 512], F32), Buf()) for _ in range(2)]
    sqb = (P.sb([128, 8, 512], BF16), Buf())
    rsb = (P.sb([128, 512], F32), Buf())
    norm_tiles(xT, nullb, gsb["an_e"], range(NT), lambda t: (hnT[:, :, t * 512:(t + 1) * 512], hn_b), 0, xbufs, sqb, rsb)
    P.sb_off = l0_mark
    P.barrier()

    for name in ("wg_e", "wu_e", "wd_e", "w_in_o", "w_out_o"):
        cast_w(name)
    w_in_e_v = wb["w_in_e"].rearrange("(c p) n -> p c n", p=128)

    def retention():
        mark = P.sb_off
        wq = P.sb([128, 4, 8, 128], BF16, off=None) if False else None
        wts = [P.sb([128, 4, 8, 128], BF16) for _ in range(1)]
        wall = wts[0]
        w_b = Buf()
        dtab = P.sb([128, 256], F32)
        kdt = P.sb([128, 128], F32)
        qdt = P.sb([128, 512], F32)
        tb = Buf()
        S32 = [P.sb([128, 128], F32) for _ in range(4)]
        Sbf = [P.sb([128, 128], BF16) for _ in range(4)]
        S_b = [Buf() for _ in range(4)]
        for j in range(4):
            P.op("dve", lambda e, j=j: e.memset(S32[j][:], 0.0), writes=[S_b[j]])
            P.op("dve", lambda e, j=j: e.memset(Sbf[j][:], 0.0), writes=[S_b[j]])
        rot = [(P.sb([128, 4, 512], F32), Buf()) for _ in range(2)]
        WQ, WK, WV, WG = 0, 1, 2, 3
        q32 = P.sb([128, 512], F32); q32_b = Buf()
        ta = P.sb([128, 512], F32); ta_b = Buf()
        tb2 = P.sb([128, 512], F32); tb2_b = Buf()
        qr = P.sb([128, 512], BF16); qr_b = Buf()
        qd = P.sb([128, 512], BF16); qd_b = Buf()
        kr = P.sb([128, 512], BF16); kr_b = Buf()
        kdk = P.sb([128, 4, 128], BF16); kdk_b = Buf()
        vtk = P.sb([128, 4, 128], BF16); vtk_b = Buf()
        sg = P.sb([128, 512], F32); sg_b = Buf()
        sm = [(P.sb([128, 128], BF16), Buf()) for _ in range(2)]
        o32 = P.sb([128, 512], F32); o32_b = Buf()
        obf = P.sb([128, 512], BF16); obf_b = Buf()
        cen = P.sb([128, 512], F32); cen_b = Buf()
        c2 = P.sb([128, 512], BF16); c2_b = Buf()
        rs = P.sb([128, 512], F32); rs_b = Buf()

        def proj_fm(wt, j, t, bank):
            for c in range(8):
                mm(ps[bank][:], wall[:, wt, c, :], hnT[:, c, t * 512:(t + 1) * 512], c == 0, c == 7, [w_b, hn_b], [pb[bank]])

        def rotary(j, t, wt, ci, out_bf, out_b, rt, rt_b, dec=None):
            proj_fm(wt, j, t, 0)
            cp(q32[:], ps[0][:], [pb[0]], [q32_b], eng="act")
            mm(ps[1][:], pm32, q32[:], True, True, [cb, q32_b], [pb[1]])
            tt(ta[:], q32[:], rt[:, ci, :], ALU.mult, [q32_b, rt_b], [ta_b])
            tt(tb2[:], ps[1][:], rt[:, ci + 1, :], ALU.mult, [pb[1], rt_b], [tb2_b])
            tt(ta[:], ta[:], tb2[:], ALU.add, [ta_b, tb2_b], [ta_b])
            cp(out_bf[:], ta[:], [ta_b], [out_b], eng="act")
            if dec is not None:
                tt(dec[0][:], ta[:], qdt[:], ALU.mult, [ta_b, tb], [dec[1]])

        it_ = 0
        for j in range(4):
            for k_ in range(4):
                c0 = k_ * 512 + j * 128
                dma("sp", wall[:, k_, :, :], w_in_e_v[:, :, c0:c0 + 128], reads=[wbuf["w_in_e"]], writes=[w_b])
            dma("sp", dtab[:], rdt_d[j], writes=[tb])
            dma("sp", kdt[:], rkd_d[j], writes=[tb])
            dma("sp", qdt[:], rqd_d[j], writes=[tb])
            for t in range(NT):
                rt, rt_b = rot[it_ % 2]
                it_ += 1
                for ci in range(4):
                    dma("sp", rt[:, ci, :], rot_d[ci, :, t * 512:(t + 1) * 512], writes=[rt_b])
                rotary(j, t, WQ, 0, qr, qr_b, rt, rt_b, dec=(qd, qd_b))
                rotary(j, t, WK, 2, kr, kr_b, rt, rt_b)
                proj_fm(WG, j, t, 2)
                act(sg[:], ps[2][:], AF.Silu, [pb[2]], [sg_b])
                for n in range(4):
                    tok = slice(t * 512 + n * 128, t * 512 + (n + 1) * 128)
                    for c in range(8):
                        mm(ps[3][:, n * 128:(n + 1) * 128], hnT[:, c, tok], wall[:, WV, c, :], (n == 0 and c == 0), c == 7,
                           [hn_b, w_b], [pb[3]], skip=True)
                cp(vtk[:].rearrange("p n f -> p (n f)"), ps[3][:], [pb[3]], [vtk_b], eng="act")
                ktp = ps[4][:].bitcast(BF16)
                for n in range(4):
                    P.op("pe", lambda e, n=n: e.transpose(ktp[:, n * 128:(n + 1) * 128], kr[:, n * 128:(n + 1) * 128], identb),
                         reads=[kr_b, cb], writes=[pb[4]])
                tt(kdk[:].rearrange("p n f -> p n f"), ktp[:, 0:512].rearrange("p (n f) -> p n f", n=4),
                   kdt[:, None, :].to_broadcast([128, 4, 128]), ALU.mult, [pb[4], tb], [kdk_b])
                for n in range(4):
                    cs = slice(n * 128, (n + 1) * 128)
                    for hh in range(2):
                        hs = slice(hh * 64, (hh + 1) * 64)
                        bk = 5 + hh
                        smt, smt_b = sm[hh]
                        mm(ps[7][:, hh * 128:(hh + 1) * 128], kr[hs, cs], qr[hs, cs], True, True, [kr_b, qr_b], [pb[7]], skip=True)
                        tt(smt[:], ps[7][:, hh * 128:(hh + 1) * 128], dtab[:, hh * 128:(hh + 1) * 128], ALU.mult,
                           [pb[7], tb], [smt_b])
                        mm(ps[bk][:, cs], vtk[:, n, :], smt[:], (n == 0), False, [vtk_b, smt_b], [pb[bk]], skip=True)
                        mm(ps[bk][:, cs], Sbf[j][:], qd[:, cs], False, True, [S_b[j], qd_b], [pb[bk]], skip=True)
                    mm(ps[1][:, 0:128], kdk[:, n, :], vtk[:, n, :], True, True, [kdk_b, vtk_b], [pb[1]])
                    stt(S32[j][:], S32[j][:], rcd_sb[:, j:j + 1], ps[1][:, 0:128], ALU.mult, ALU.add, [S_b[j], pb[1], gb], [S_b[j]])
                    tt(Sbf[j][:], S32[j][:], blockmask, ALU.mult, [S_b[j], cb], [S_b[j]])
                cp(o32[0:64, :], ps[5][0:64, :], [pb[5]], [o32_b], eng="act")
                cp(o32[64:128, :], ps[6][64:128, :], [pb[6]], [o32_b], eng="act")
                cp(obf[:], o32[:], [o32_b], [obf_b], eng="act")
                mm(ps[0][:], bdb, obf[:], True, True, [cb, obf_b], [pb[0]])
                tt(cen[:], o32[:], ps[0][:], ALU.subtract, [o32_b, pb[0]], [cen_b])
                act(c2[:], cen[:], AF.Square, [cen_b], [c2_b])
                mm(ps[2][:], bdb, c2[:], True, True, [cb, c2_b], [pb[2]])
                act(rs[:], ps[2][:], AF.Sqrt, [pb[2]], [rs_b], bias=GN_EPS, scale=1.0)
                recip(rs[:], rs[:], [rs_b], [rs_b])
                tt(cen[:], cen[:], rs[:], ALU.mult, [cen_b, rs_b], [cen_b])
                stt(yT[:, j, t * 512:(t + 1) * 512], cen[:], rn_sb[:, j:j + 1], sg[:], ALU.mult, ALU.mult,
                    [cen_b, sg_b, gb], [y_b])
        P.sb_off = mark

    if stop == "norm0":
        finish(xT); P.emit(); return nc
    retention()
    P.barrier()
    if stop == "ret":
        finish(xT); P.emit(); return nc

    def attention_pair(kind, w_v, wname, qc0, kc0, vc0, ychunk, btab=None, hidx=None):
        mark = P.sb_off
        first_call = P.begin_replay(kind)
        Buf = P.buf
        wq = P.sb([128, 8, 128], BF16)
        wk = P.sb([128, 8, 128], BF16)
        wv = P.sb([128, 8, 128], BF16)
        w_b = Buf()
        for wt, c0 in ((wq, qc0), (wk, kc0), (wv, vc0)):
            dma("sp", wt[:], w_v[:, :, c0:c0 + 128], reads=[wbuf[wname]], writes=[w_b])
        qTp = P.sb([128, S], BF16); q_b = Buf()
        kTp = P.sb([128, S], BF16); k_b = Buf()
        if kind == "sb":
            vt = [P.sb([128, NB, 128], BF16)]
        else:
            vt = [P.sb([128, NB, 128], BF16), P.sb([128, NB, 128], BF16)]
        v_b = Buf()
        for t in range(NT):
            ts_ = slice(t * 512, (t + 1) * 512)
            for (wt, dst, db, bk) in ((wq, qTp, q_b, 0), (wk, kTp, k_b, 1)):
                for c in range(8):
                    mm(ps[bk][:], wt[:, c, :], hnT[:, c, ts_], c == 0, c == 7, [w_b, hn_b], [pb[bk]])
                cp(dst[:, ts_], ps[bk][:], [pb[bk]], [db], eng=("act" if bk == 0 else "dve"))
            for n in range(4):
                tok = slice(t * 512 + n * 128, t * 512 + (n + 1) * 128)
                for c in range(8):
                    mm(ps[2][:, n * 128:(n + 1) * 128], hnT[:, c, tok], wv[:, c, :], (n == 0 and c == 0), c == 7,
                       [hn_b, w_b], [pb[2]], skip=True)
            if kind == "sb":
                cp(vt[0][:, t * 4:(t + 1) * 4, :].rearrange("p n f -> p (n f)"), ps[2][:], [pb[2]], [v_b], eng="act")
            else:
                pv = ps[2][:].rearrange("p (n f) -> p n f", n=4)
                cp(vt[0][:, t * 4:(t + 1) * 4, 0:64], pv[:, :, 0:64], [pb[2]], [v_b], eng="act")
                cp(vt[1][:, t * 4:(t + 1) * 4, 0:64], pv[:, :, 64:128], [pb[2]], [v_b], eng="dve")
        if kind == "fox" and first_call:
            for hh in range(2):
                P.op("dve", lambda e, hh=hh: e.memset(vt[hh][:, :, 64:128], 1.0), writes=[v_b])
        if kind == "sb" and SB_WIDE:
            PD = 3
            es_p = [(P.sb([128, 2, 512], F32), Buf()) for _ in range(PD)]
            Ls_p = [(P.sb([128, 2, 512], BF16), Buf()) for _ in range(PD)]
            gs_p = [(P.sb([128, 2, 512], F32), Buf()) for _ in range(2)]
            ws_p = [(P.sb([128, 2, 512], BF16), Buf()) for _ in range(PD)]
            la = P.sb([128, 2, 512], BF16); la_b = Buf()
            zbufs = [Buf(), Buf()]; cbuf = [Buf()]
            pzs = [psall[:, k * 1024:(k + 1) * 1024].rearrange("p (h n) -> p h n", h=2) for k in range(2)]
            pc = [psall[:, 2048:3072].rearrange("p (h n) -> p h n", h=2)]
            punits = []
            for qt in range(NT):
                nblk = 4 * qt + 4
                for idx, jb in enumerate(range(nblk - 1, -1, -1)):
                    punits.append((qt, jb, idx == 0, idx == nblk - 1))

            def pgeom(u):
                qt, jb, first, last = u
                c0 = 128 * max(0, jb - 4 * qt)
                return c0, slice(c0, 512), slice(qt * 512 + c0, (qt + 1) * 512), jb >= 4 * qt

            def pA(u, i):
                qt, jb, first, last = u
                c0, cols, qcols, diag = pgeom(u)
                es, es_b = es_p[i % PD]; Ls, Ls_b = Ls_p[i % PD]
                pz = pzs[i % 2]; zbuf = zbufs[i % 2]
                for hh in range(2):
                    hs = slice(hh * 64, (hh + 1) * 64)
                    mm(pz[:, hh, cols], kTp[hs, jb * 128:(jb + 1) * 128], qTp[hs, qcols], True, True, [k_b, q_b], [zbuf])
                act(es[:, :, cols], pz[:, :, cols], AF.Exp, [zbuf], [es_b], scale=0.125)
                if diag:
                    tt(es[:, :, c0:c0 + 128], es[:, :, c0:c0 + 128], maskS32[:, None, :].to_broadcast([128, 2, 128]), ALU.mult,
                       [es_b, cb], [es_b])
                act(Ls[:, :, cols], es[:, :, cols], AF.Ln, [es_b], [Ls_b], bias=1.0)

            def pB(u, i):
                qt, jb, first, last = u
                c0, cols, qcols, diag = pgeom(u)
                es, es_b = es_p[i % PD]; Ls, Ls_b = Ls_p[i % PD]; gs, gs_b = gs_p[i % 2]; ws, ws_b = ws_p[i % PD]
                pcc = pc[0]; pcb = cbuf[0]
                for hh in range(2):
                    mm(pcc[:, hh, cols], trib, Ls[:, hh, cols], True, first, [cb, Ls_b], [pcb])
                    if not first:
                        mm(pcc[:, hh, cols], onesb, la[:, hh, cols], False, True, [cb, la_b], [pcb])
                act(gs[:, :, cols], pcc[:, :, cols], AF.Exp, [pcb], [gs_b], scale=-1.0)
                tt(ws[:, :, cols], es[:, :, cols], gs[:, :, cols], ALU.mult, [es_b, gs_b], [ws_b])
                if not last:
                    if first:
                        P.op("dve", lambda e: e.memset(la[:], 0.0), writes=[la_b])
                    tt(la[:, :, cols], la[:, :, cols], Ls[:, :, cols], ALU.add, [la_b, Ls_b], [la_b])

            def pC(u, i):
                qt, jb, first, last = u
                c0, cols, qcols, diag = pgeom(u)
                ws, ws_b = ws_p[i % PD]
                for hh in range(2):
                    hs = slice(hh * 64, (hh + 1) * 64)
                    mm(ps[6 + hh][0:64, cols], vt[0][:, jb, hs], ws[:, hh, cols], first, last, [v_b, ws_b], [pb[6 + hh]], skip=True)
                    if last:
                        cp(yT[hs, ychunk, qt * 512:(qt + 1) * 512], ps[6 + hh][0:64, :], [pb[6 + hh]], [y_b],
                           eng=("act" if hh == 0 else "dve"))

            npu = len(punits)
            for i in range(npu + 4):
                if i < npu:
                    pA(punits[i], i)
                if 2 <= i < npu + 2:
                    pB(punits[i - 2], i - 2)
                if i >= 4:
                    pC(punits[i - 4], i - 4)
            P.end_replay()
            P.sb_off = mark
            return
        SK = 2
        DEP = 6
        w_sb = [(P.sb([128, 512], BF16), Buf()) for _ in range(DEP)]
        if kind == "sb":
            e_sb = [(P.sb([128, 512], F32), Buf()) for _ in range(DEP)]
            L_sb = [(P.sb([128, 512], BF16), Buf()) for _ in range(DEP)]
            g_sb = [(P.sb([128, 512], F32), Buf()) for _ in range(2)]
            Lacc = [(P.sb([128, 512], BF16), Buf()) for _ in range(2)]
        else:
            rden = P.sb([128, 512], F32); rd_b = Buf()
        units = []
        for qt in range(NT):
            nblk = 4 * qt + 4
            order = list(range(nblk - 1, -1, -1)) if kind == "sb" else list(range(nblk))
            for idx, jb in enumerate(order):
                for hh in range(2):
                    units.append((qt, hh, jb, idx == 0, idx == nblk - 1))

        def geom(u):
            qt, hh, jb, first, last = u
            m = max(0, jb - 4 * qt)
            c0 = 128 * m
            return (slice(hh * 64, (hh + 1) * 64), c0, slice(c0, 512), slice(qt * 512 + c0, (qt + 1) * 512), jb >= 4 * qt)

        junk_b = Buf()
        nfill = nfill_sb if kind == "sb" else nfill_fox
        jbank = 5 if kind == "sb" else 3

        def stageA(u, i):
            qt, hh, jb, first, last = u
            hs, c0, cols, qcols, diag = geom(u)
            zb = i % 3
            for _ in range(nfill):
                mm(ps[jbank][:], onesb, cstb[:, 0:512], True, True, [cb], [junk_b])
            mm(ps[zb][:, cols], kTp[hs, jb * 128:(jb + 1) * 128], qTp[hs, qcols], True, True, [k_b, q_b], [pb[zb]])
            if kind == "sb":
                es, es_b = e_sb[i % DEP]; Ls, Ls_b = L_sb[i % DEP]
                act(es[:, cols], ps[zb][:, cols], AF.Exp, [pb[zb]], [es_b], scale=0.125)
                if diag:
                    tt(es[:, c0:c0 + 128], es[:, c0:c0 + 128], maskS32, ALU.mult, [es_b, cb], [es_b])
                act(Ls[:, cols], es[:, cols], AF.Ln, [es_b], [Ls_b], bias=1.0)
            else:
                ws, ws_b = w_sb[i % DEP]
                act(ws[:, cols], ps[zb][:, cols], AF.Exp, [pb[zb]], [ws_b],
                    bias=btab[:, hidx + hh, qt, jb:jb + 1], scale=0.125)
                if diag:
                    tt(ws[:, c0:c0 + 128], ws[:, c0:c0 + 128], maskIb, ALU.mult, [ws_b, cb], [ws_b])

        def stageB(u, i):
            qt, hh, jb, first, last = u
            hs, c0, cols, qcols, diag = geom(u)
            accb = 6 + hh
            ws, ws_b = w_sb[i % DEP]
            if kind == "sb":
                es, es_b = e_sb[i % DEP]; Ls, Ls_b = L_sb[i % DEP]; gs, gs_b = g_sb[i % 2]
                la, la_b = Lacc[hh]
                cb_ = 3 + (i % 2)
                mm(ps[cb_][:, cols], trib, Ls[:, cols], True, first, [cb, Ls_b], [pb[cb_]])
                if not first:
                    mm(ps[cb_][:, cols], onesb, la[:, cols], False, True, [cb, la_b], [pb[cb_]])
                act(gs[:, cols], ps[cb_][:, cols], AF.Exp, [pb[cb_]], [gs_b], scale=-1.0)
                tt(ws[:, cols], es[:, cols], gs[:, cols], ALU.mult, [es_b, gs_b], [ws_b])
                if not last:
                    if first:
                        P.op("dve", lambda e, la=la: e.memset(la[:], 0.0), writes=[la_b])
                    tt(la[:, cols], la[:, cols], Ls[:, cols], ALU.add, [la_b, Ls_b], [la_b], eng="dve")

        def stageC(u, i):
            qt, hh, jb, first, last = u
            hs, c0, cols, qcols, diag = geom(u)
            accb = 6 + hh
            ws, ws_b = w_sb[i % DEP]
            if kind == "sb":
                mm(ps[accb][0:64, cols], vt[0][:, jb, hs], ws[:, cols], first, last, [v_b, ws_b], [pb[accb]], skip=True)
            else:
                mm(ps[accb][:, cols], vt[hh][:, jb, :], ws[:, cols], first, last, [v_b, ws_b], [pb[accb]], skip=True)
            if last:
                ysl = yT[hs, ychunk, qt * 512:(qt + 1) * 512]
                if kind == "sb":
                    cp(ysl, ps[accb][0:64, :], [pb[accb]], [y_b], eng="act")
                else:
                    recip(rden[0:64, :], ps[accb][64:128, :], [pb[accb]], [rd_b])
                    tt(ysl, ps[accb][0:64, :], rden[0:64, :], ALU.mult, [pb[accb], rd_b], [y_b])

        SK2 = 2 * SK
        nu = len(units)
        for i0 in range(0, nu + SK2, 2):
            for i in (i0, i0 + 1):
                if i < nu:
                    stageA(units[i], i)
            for i in (i0, i0 + 1):
                if kind == "sb" and SK <= i < nu + SK:
                    stageB(units[i - SK], i - SK)
            for i in (i0, i0 + 1):
                if SK2 <= i < nu + SK2:
                    stageC(units[i - SK2], i - SK2)
        P.end_replay()
        P.sb_off = mark

    for j in range(4):
        cast_expert(2 * j)
        cast_expert(2 * j + 1)
        attention_pair("sb", w_in_e_v, "w_in_e", 2048 + j * 128, 2560 + j * 128, 3072 + j * 128, 4 + j)
    P.barrier()

    if stop == "sb":
        finish(xT); P.emit(); return nc
    out_proj(yT, y_b, "w_out_e", xT, h1T)
    P.barrier()
    P.sb_off = persist_mark

    def ffn_phase(src, dst, gain, moe):
        mark = P.sb_off
        GW = GT * 512
        hnG = P.sb([128, 8, GW], BF16); hnG_b = Buf()
        aT = P.sb([128, NF, GW], BF16); a_b = Buf()
        acc = P.sb([128, 8, GW], F32); acc_b = Buf()
        wd = P.sb([128, NF, D], BF16); wd_b = Buf()
        wgu = [(P.sb([128, 2, 8, 128], BF16), Buf()) for _ in range(2)]
        sq_off = (P.sb_off + 63) // 64 * 64
        sq = (P.sb([128, 8, 512], BF16), Buf())
        rsb_ = (P.sb([128, 512], F32), Buf())
        s_sb = [(P.sb([128, 512], F32), Buf()) for _ in range(2)]
        t_sb = [(P.sb([128, 512], BF16), Buf()) for _ in range(2)]
        if moe:
            comb = P.sb([128, NE, GW], BF16); comb_b = Buf()
            hn32 = P.sb([128, 8, 512], F32); hn32_b = Buf()
            wr = P.sb([128, 8, 8], F32); wr_b = Buf()
            dma("sp", wr[:], wr_o.rearrange("(c p) e -> p c e", p=128), writes=[wr_b])
            lg = P.sb([128, 8], F32); lg2 = P.sb([128, 8], F32); eq1 = P.sb([128, 8], F32); eq2 = P.sb([128, 8], F32)
            cmb = P.sb([128, 8], F32)
            sm_ = P.sb([128, 8], F32)
            De = P.sb([128, 8, 128], F32, off=sq_off)
            r_b = Buf()
        if not moe:
            dma("sp", wd[:], wb["wd_e"].rearrange("(f p) n -> p f n", p=128), reads=[wbuf["wd_e"]], writes=[wd_b])
        wcount = 0
        for g in range(NG):
            tiles = list(range(g * GT, (g + 1) * GT))
            gsl = slice(g * GW, (g + 1) * GW)
            dma("sp", acc[:], view_T(src)[:, :, gsl], reads=[hb[id(src)]], writes=[acc_b])
            for n, t in enumerate(tiles):
                xs = acc[:, :, n * 512:(n + 1) * 512]
                act(sq[0][:], xs, AF.Square, [acc_b], [sq[1]])
                for c in range(8):
                    mm(ps[0][:], onesb, sq[0][:, c, :], c == 0, c == 7, [cb, sq[1]], [pb[0]])
                r, rb_ = rsb_
                act(r[:], ps[0][:], AF.Sqrt, [pb[0]], [rb_], bias=EPS, scale=1.0 / D)
                recip(r[:], r[:], [rb_], [rb_])
                for c in range(8):
                    stt(hnG[:, c, n * 512:(n + 1) * 512], xs[:, c, :], gain[:, c:c + 1], r[:], ALU.mult, ALU.mult,
                        [acc_b, rb_, gb], [hnG_b])
                if moe:
                    for c in range(8):
                        stt(hn32[:, c, :], xs[:, c, :], gain[:, c:c + 1], r[:], ALU.mult, ALU.mult, [acc_b, rb_, gb], [hn32_b])
                    for blk in range(4):
                        bs = slice(blk * 128, (blk + 1) * 128)
                        for c in range(8):
                            mm(ps[1][:, 0:8], hn32[:, c, bs], wr[:, c, :], c == 0, c == 7, [hn32_b, wr_b], [pb[1]])
                        cp(lg[:], ps[1][:, 0:8], [pb[1]], [r_b])
                        P.op("dve", lambda e: e.reduce_max(out=sm_[:, 0:1], in_=lg[:], axis=AX.X), reads=[r_b], writes=[r_b])
                        ts(eq1[:], lg[:], sm_[:, 0:1], ALU.is_equal, [r_b], [r_b])
                        stt(lg2[:], eq1[:], -1e30, lg[:], ALU.mult, ALU.add, [r_b], [r_b])
                        P.op("dve", lambda e: e.reduce_max(out=sm_[:, 1:2], in_=lg2[:], axis=AX.X), reads=[r_b], writes=[r_b])
                        ts(eq2[:], lg2[:], sm_[:, 1:2], ALU.is_equal, [r_b], [r_b])
                        tt(sm_[:, 2:3], sm_[:, 1:2], sm_[:, 0:1], ALU.subtract, [r_b], [r_b])
                        act(sm_[:, 3:4], sm_[:, 2:3], AF.Exp, [r_b], [r_b])
                        ts(sm_[:, 4:5], sm_[:, 3:4], 1.0, ALU.add, [r_b], [r_b])
                        recip(sm_[:, 4:5], sm_[:, 4:5], [r_b], [r_b])
                        tt(sm_[:, 5:6], sm_[:, 3:4], sm_[:, 4:5], ALU.mult, [r_b], [r_b])
                        ts(cmb[:], eq1[:], sm_[:, 4:5], ALU.mult, [r_b], [r_b])
                        stt(cmb[:], eq2[:], sm_[:, 5:6], cmb[:], ALU.mult, ALU.add, [r_b], [r_b])
                        for e_ in range(NE):
                            ts(De[:, e_, :], ident, cmb[:, e_:e_ + 1], ALU.mult, [r_b, cb], [sq[1]])
                        for half in range(2):
                            mm(ps[2 + half][:], ones32, De[:, half * 4:(half + 1) * 4, :].rearrange("p e t -> p (e t)"),
                               True, True, [cb, sq[1]], [pb[2 + half]])
                            cp(comb[:, half * 4:(half + 1) * 4, n * 512 + blk * 128:n * 512 + (blk + 1) * 128],
                               ps[2 + half][:].rearrange("p (e t) -> p e t", e=4), [pb[2 + half]], [comb_b], eng="act")
            for e_ in range(NE if moe else 1):
                if moe:
                    wgv = wb["wg_m"][e_].rearrange("(c p) n -> p c n", p=128)
                    wuv = wb["wu_m"][e_].rearrange("(c p) n -> p c n", p=128)
                    wgb, wub = wbuf["wg_m"][e_], wbuf["wu_m"][e_]
                else:
                    wgv = wb["wg_e"].rearrange("(c p) n -> p c n", p=128)
                    wuv = wb["wu_e"].rearrange("(c p) n -> p c n", p=128)
                    wgb, wub = wbuf["wg_e"], wbuf["wu_e"]
                for f in range(NF):
                    wt, wt_b = wgu[wcount % 2]
                    wcount += 1
                    dma("sp", wt[:, 0, :, :], wgv[:, :, f * 128:(f + 1) * 128], reads=[wgb], writes=[wt_b])
                    dma("sp", wt[:, 1, :, :], wuv[:, :, f * 128:(f + 1) * 128], reads=[wub], writes=[wt_b])
                    if moe and f == 3:
                        dma("sp", wd[:], wb["wd_m"][e_].rearrange("(f p) n -> p f n", p=128), reads=[wbuf["wd_m"][e_]], writes=[wd_b])
                    for n in range(GT):
                        ns = slice(n * 512, (n + 1) * 512)
                        gbk, ubk = 4 + (n % 2) * 2, 5 + (n % 2) * 2
                        for c in range(8):
                            mm(ps[gbk][:], wt[:, 0, c, :], hnG[:, c, ns], c == 0, c == 7, [wt_b, hnG_b], [pb[gbk]])
                        for c in range(8):
                            mm(ps[ubk][:], wt[:, 1, c, :], hnG[:, c, ns], c == 0, c == 7, [wt_b, hnG_b], [pb[ubk]])
                        ss_, ss_b = s_sb[n % 2]
                        act(ss_[:], ps[gbk][:], AF.Silu, [pb[gbk]], [ss_b])
                        if moe:
                            tq, tq_b = t_sb[n % 2]
                            tt(tq[:], ps[ubk][:], ss_[:], ALU.mult, [pb[ubk], ss_b], [tq_b])
                            tt(aT[:, f, ns], tq[:], comb[:, e_, ns], ALU.mult, [tq_b, comb_b], [a_b], eng="pool")
                        else:
                            tt(aT[:, f, ns], ps[ubk][:], ss_[:], ALU.mult, [pb[ubk], ss_b], [a_b])
                for m in range(8):
                    for n in range(GT):
                        ns = slice(n * 512, (n + 1) * 512)
                        bk = (m * GT + n) % 4
                        for f in range(NF):
                            mm(ps[bk][:], wd[:, f, m * 128:(m + 1) * 128], aT[:, f, ns], f == 0, f == NF - 1, [wd_b, a_b], [pb[bk]])
                        tt(acc[:, m, ns], ps[bk][:], acc[:, m, ns], ALU.add, [pb[bk], acc_b], [acc_b])
            dma("sp", view_T(dst)[:, :, gsl], acc[:], reads=[acc_b], writes=[hb[id(dst)]])
        P.sb_off = mark


    def ffn_dense(src, dst, gain):
        mark = P.sb_off
        GW = GT * 512
        hnG = P.sb([128, 8, GW], BF16); hnG_b = Buf()
        aT = P.sb([128, NF, GW], BF16); a_b = Buf()
        accs = [(P.sb([128, 8, GW], F32), Buf()) for _ in range(2)]
        wd = P.sb([128, NF, D], BF16); wd_b = Buf()
        wgu = [(P.sb([128, 2, 8, 128], BF16), Buf()) for _ in range(2)]
        sq = (P.sb([128, 8, 512], BF16), Buf())
        r = P.sb([128, 512], F32); rb_ = Buf()
        s_sb = [(P.sb([128, 512], F32), Buf()) for _ in range(2)]
        dma("sp", wd[:], wb["wd_e"].rearrange("(f p) n -> p f n", p=128), reads=[wbuf["wd_e"]], writes=[wd_b])
        wgv = wb["wg_e"].rearrange("(c p) n -> p c n", p=128)
        wuv = wb["wu_e"].rearrange("(c p) n -> p c n", p=128)

        def gsl(g):
            return slice(g * GW, (g + 1) * GW)

        def load_res(g):
            acc, acc_b = accs[g % 2]
            dma("sp", acc[:], view_T(src)[:, :, gsl(g)], reads=[hb[id(src)]], writes=[acc_b])

        def norm(g):
            acc, acc_b = accs[g % 2]
            for n in range(GT):
                xs = acc[:, :, n * 512:(n + 1) * 512]
                act(sq[0][:], xs, AF.Square, [acc_b], [sq[1]])
                for c in range(8):
                    mm(ps[0][:], onesb, sq[0][:, c, :], c == 0, c == 7, [cb, sq[1]], [pb[0]])
                act(r[:], ps[0][:], AF.Sqrt, [pb[0]], [rb_], bias=EPS, scale=1.0 / D)
                recip(r[:], r[:], [rb_], [rb_])
                for c in range(8):
                    stt(hnG[:, c, n * 512:(n + 1) * 512], xs[:, c, :], gain[:, c:c + 1], r[:], ALU.mult, ALU.mult,
                        [acc_b, rb_, gb], [hnG_b])

        def load_w(f):
            wt, wt_b = wgu[f % 2]
            dma("sp", wt[:, 0, :, :], wgv[:, :, f * 128:(f + 1) * 128], reads=[wbuf["wg_e"]], writes=[wt_b])
            dma("sp", wt[:, 1, :, :], wuv[:, :, f * 128:(f + 1) * 128], reads=[wbuf["wu_e"]], writes=[wt_b])

        def gate_up(g, preloaded):
            for f in range(NF):
                wt, wt_b = wgu[f % 2]
                if f >= preloaded:
                    load_w(f)
                for n in range(GT):
                    ns = slice(n * 512, (n + 1) * 512)
                    gbk, ubk = 4 + (n % 2) * 2, 5 + (n % 2) * 2
                    for c in range(8):
                        mm(ps[gbk][:], wt[:, 0, c, :], hnG[:, c, ns], c == 0, c == 7, [wt_b, hnG_b], [pb[gbk]])
                    for c in range(8):
                        mm(ps[ubk][:], wt[:, 1, c, :], hnG[:, c, ns], c == 0, c == 7, [wt_b, hnG_b], [pb[ubk]])
                    ss_, ss_b = s_sb[n % 2]
                    act(ss_[:], ps[gbk][:], AF.Silu, [pb[gbk]], [ss_b])
                    tt(aT[:, f, ns], ps[ubk][:], ss_[:], ALU.mult, [pb[ubk], ss_b], [a_b])

        def down(g):
            acc, acc_b = accs[g % 2]
            for m in range(8):
                for n in range(GT):
                    ns = slice(n * 512, (n + 1) * 512)
                    bk = 1 + (m * GT + n) % 3
                    for f in range(NF):
                        mm(ps[bk][:], wd[:, f, m * 128:(m + 1) * 128], aT[:, f, ns], f == 0, f == NF - 1, [wd_b, a_b], [pb[bk]])
                    tt(acc[:, m, ns], ps[bk][:], acc[:, m, ns], ALU.add, [pb[bk], acc_b], [acc_b])

        def store(g):
            acc, acc_b = accs[g % 2]
            dma("sp", view_T(dst)[:, :, gsl(g)], acc[:], reads=[acc_b], writes=[hb[id(dst)]])

        load_res(0)
        norm(0)
        pre = 0
        for g in range(NG):
            if g + 1 < NG:
                load_res(g + 1)
            gate_up(g, pre)
            pre = 0
            if g + 1 < NG:
                load_w(0)
                load_w(1)
                pre = 2
                norm(g + 1)
            down(g)
            store(g)
        P.sb_off = mark

    if stop == "mix0":
        finish(h1T); P.emit(); return nc

    ffn_dense(h1T, h2T, gsb["fn_e"])
    P.barrier()
    if stop == "ffn0":
        finish(h2T); P.emit(); return nc

    hnT = P.sb([128, 8, S], BF16)
    hn_b = Buf()
    yT = P.sb([128, 8, S], BF16)
    y_b = Buf()
    btab = P.sb([128, 16, NT, NB], F32)
    bt_b = Buf()
    l1_mark = P.sb_off
    xbufs = [(P.sb([128, 8, 512], F32), Buf()) for _ in range(2)]
    sqb = (P.sb([128, 8, 512], BF16), Buf())
    rsb = (P.sb([128, 512], F32), Buf())
    norm_tiles(h2T, hb[id(h2T)], gsb["an_o"], range(NT), lambda t: (hnT[:, :, t * 512:(t + 1) * 512], hn_b), 0, xbufs, sqb, rsb)
    P.sb_off = l1_mark
    P.barrier()
    w_in_o_v = wb["w_in_o"].rearrange("(c p) n -> p c n", p=128)

    def forget_tables():
        mark = P.sb_off
        wf = P.sb([128, 8, 16], BF16); wf_b = Buf()
        dma("sp", wf[:], w_in_o_v[:, :, 3072:3088], reads=[wbuf["w_in_o"]], writes=[wf_b])
        bfs = P.sb([16, 2], F32); bfs_b = Buf()
        dma("sp", bfs[:, 0:1], bf_o, writes=[bfs_b])
        ts(bfs[:, 1:2], bfs[:, 0:1], -1.0, ALU.mult, [bfs_b], [bfs_b])
        lf = P.sb([16, S], F32); lf_b = Buf()
        cum = P.sb([16, S], F32); cum_b = Buf()
        one16 = P.sb([16, 512], F32); o16_b = Buf()
        P.op("dve", lambda e: e.memset(one16[:], 1.0), writes=[o16_b])
        for t in range(NT):
            ts_ = slice(t * 512, (t + 1) * 512)
            for c in range(8):
                mm(ps[0][0:16, :], wf[:, c, :], hnT[:, c, ts_], c == 0, c == 7, [wf_b, hn_b], [pb[0]])
            act(lf[:, ts_], ps[0][0:16, :], AF.Exp, [pb[0], bfs_b], [lf_b], bias=bfs[:, 1:2], scale=-1.0)
            act(lf[:, ts_], lf[:, ts_], AF.Ln, [lf_b], [lf_b], bias=1.0)
            init = 0.0 if t == 0 else cum[:, t * 512 - 1:t * 512]
            P.op("dve", lambda e, ts_=ts_, init=init: e.tensor_tensor_scan(out=cum[:, ts_], data0=one16[:], data1=lf[:, ts_],
                                                                           initial=init, op0=ALU.mult, op1=ALU.add),
                 reads=[lf_b, o16_b, cum_b], writes=[cum_b])
        ck = P.sb([128, NB, 16], F32); ck_b = Buf()
        for jb in range(NB):
            P.op("pe", lambda e, jb=jb: e.transpose(ps[1][:, jb * 16:(jb + 1) * 16], cum[:, jb * 128:(jb + 1) * 128], ident[0:16, 0:16]),
                 reads=[cum_b, cb], writes=[pb[1]])
        cp(ck[:].rearrange("p n h -> p (n h)"), ps[1][:, 0:NB * 16], [pb[1]], [ck_b])
        R = P.sb([16, 16, NT], F32); R_b = Buf()
        cmid = cum[:].rearrange("h (t s) -> h t s", s=512)[:, :, 255]
        for h in range(16):
            ts(R[:, h, :], cmid, ident[0:16, h:h + 1], ALU.mult, [cum_b, cb], [R_b])
        mm(ps[2][:, 0:16 * NT], ones32[0:16, :], R[:].rearrange("k h t -> k (h t)"), True, True, [cb, R_b], [pb[2]])
        cq = P.sb([128, 16, NT], F32); cq_b = Buf()
        cp(cq[:].rearrange("p h t -> p (h t)"), ps[2][:, 0:16 * NT], [pb[2]], [cq_b])
        for h in range(16):
            for t in range(NT):
                ts(btab[:, h, t, :], ck[:, :, h], cq[:, h, t:t + 1], ALU.subtract, [ck_b, cq_b], [bt_b])
        P.sb_off = mark

    forget_tables()
    P.barrier()
    for j in range(8):
        attention_pair("fox", w_in_o_v, "w_in_o", j * 128, 1024 + j * 128, 2048 + j * 128, j, btab=btab, hidx=2 * j)
    P.barrier()
    out_proj(yT, y_b, "w_out_o", h2T, h3T)
    P.barrier()
    P.sb_off = persist_mark
    if stop == "mix1":
        finish(h3T); P.emit(); return nc


    def moe_sparse():
        I32 = mybir.dt.int32
        gain = gsb["fn_o"]
        mark0 = P.sb_off
        eq1A = P.sb([128, NB, 8], F32); eq2A = P.sb([128, NB, 8], F32); rankA = P.sb([128, NB, 8], F32)
        gA = P.sb([128, NB, 2], F32)
        run = P.sb([128, 8], F32)
        rt_b = Buf()
        posF = P.sb([128, 2, NB], F32); posI = P.sb([128, 2, NB], I32); pos_b = Buf()
        wiF = P.sb([128, NS], F32); wiI = P.sb([128, NS], I32); wi_b = Buf()
        iot = P.sb([128, 22], F32); iot_b = Buf()
        dma("sp", iot[:], iot_d, writes=[iot_b])
        P.op("dve", lambda e: e.memset(run[:], 0.0), writes=[rt_b])
        hn_db = Buf(); h3_db = Buf(); xs_db = Buf(); ys_db = Buf()
        mark1 = P.sb_off
        accs1 = [(P.sb([128, 8, 512], F32), Buf()) for _ in range(2)]
        sq = (P.sb([128, 8, 512], BF16), Buf())
        r = P.sb([128, 512], F32); r_b2 = Buf()
        hnb = P.sb([128, 8, 512], BF16); hnb_b = Buf()
        hn32 = P.sb([128, 8, 512], F32); hn32_b = Buf()
        wr = P.sb([128, 8, 8], F32); wr_b = Buf()
        dma("sp", wr[:], wr_o.rearrange("(c p) e -> p c e", p=128), writes=[wr_b])
        tokb = [(P.sb([128, D], BF16), Buf()) for _ in range(2)]
        tok32 = [(P.sb([128, D], F32), Buf()) for _ in range(2)]
        smalls = [(P.sb([128, 8], F32), P.sb([128, 8], F32), P.sb([128, 8], F32), P.sb([128, 8], F32), Buf()) for _ in range(2)]
        lg, lg2, msk, sm_, r_b = smalls[0]
        def load_acc1(t):
            a_, ab_ = accs1[t % 2]
            dma("sp", a_[:], view_T(h3T)[:, :, t * 512:(t + 1) * 512], reads=[hb[id(h3T)]], writes=[ab_])

        load_acc1(0)
        for t in range(NT):
            acc, acc_b = accs1[t % 2]
            if t + 1 < NT:
                load_acc1(t + 1)
            act(sq[0][:], acc[:], AF.Square, [acc_b], [sq[1]])
            for c in range(8):
                mm(ps[0][:], onesb, sq[0][:, c, :], c == 0, c == 7, [cb, sq[1]], [pb[0]])
            act(r[:], ps[0][:], AF.Sqrt, [pb[0]], [r_b2], bias=EPS, scale=1.0 / D)
            recip(r[:], r[:], [r_b2], [r_b2])
            for c in range(8):
                stt(hn32[:, c, :], acc[:, c, :], gain[:, c:c + 1], r[:], ALU.mult, ALU.mult, [acc_b, r_b2, gb], [hn32_b])
            cp(hnb[:], hn32[:], [hn32_b], [hnb_b], eng="act")
            for blk in range(4):
                b = t * 4 + blk
                bs = slice(blk * 128, (blk + 1) * 128)
                lg, lg2, msk, sm_, r_b = smalls[b % 2]
                for c in range(8):
                    mm(ps[1][:, 0:8], hn32[:, c, bs], wr[:, c, :], c == 0, c == 7, [hn32_b, wr_b], [pb[1]])
                cp(lg[:], ps[1][:, 0:8], [pb[1]], [r_b])
                P.op("dve", lambda e, sm_=sm_, lg=lg: e.reduce_max(out=sm_[:, 0:1], in_=lg[:], axis=AX.X), reads=[r_b], writes=[r_b])
                ts(eq1A[:, b, :], lg[:], sm_[:, 0:1], ALU.is_equal, [r_b], [rt_b])
                stt(lg2[:], eq1A[:, b, :], -1e30, lg[:], ALU.mult, ALU.add, [r_b, rt_b], [r_b])
                P.op("dve", lambda e, sm_=sm_, lg2=lg2: e.reduce_max(out=sm_[:, 1:2], in_=lg2[:], axis=AX.X), reads=[r_b], writes=[r_b])
                ts(eq2A[:, b, :], lg2[:], sm_[:, 1:2], ALU.is_equal, [r_b], [rt_b])
                tt(sm_[:, 2:3], sm_[:, 1:2], sm_[:, 0:1], ALU.subtract, [r_b], [r_b])
                act(sm_[:, 3:4], sm_[:, 2:3], AF.Exp, [r_b], [r_b])
                ts(sm_[:, 4:5], sm_[:, 3:4], 1.0, ALU.add, [r_b], [r_b])
                recip(gA[:, b, 0:1], sm_[:, 4:5], [r_b], [rt_b])
                tt(gA[:, b, 1:2], sm_[:, 3:4], gA[:, b, 0:1], ALU.mult, [r_b, rt_b], [rt_b])
                tt(msk[:], eq1A[:, b, :], eq2A[:, b, :], ALU.add, [rt_b], [r_b])
                mm(ps[2][:, 0:8], maskS32, msk[:], True, True, [cb, r_b], [pb[2]])
                mm(ps[2][:, 8:16], ones32, msk[:], True, True, [cb, r_b], [pb[2]], skip=True)
                tt(rankA[:, b, :], ps[2][:, 0:8], run[:], ALU.add, [pb[2], rt_b], [rt_b])
                tt(run[:], ps[2][:, 8:16], run[:], ALU.add, [pb[2], rt_b], [rt_b])
                tb_, tb_b = tokb[b % 2]
                t32, t32_b = tok32[b % 2]
                p3 = ps[3][:].bitcast(BF16)
                for c in range(8):
                    P.op("pe", lambda e, c=c, bs=bs: e.transpose(p3[:, c * 128:(c + 1) * 128], hnb[:, c, bs], identb),
                         reads=[hnb_b, cb], writes=[pb[3]])
                cp(tb_[:], p3[:, 0:D], [pb[3]], [tb_b], eng="act")
                dma("sp", Hn_d[b * 128:(b + 1) * 128, :], tb_[:], reads=[tb_b], writes=[hn_db])
                for c in range(8):
                    bk = 4 + c // 4
                    P.op("pe", lambda e, c=c, bs=bs, bk=bk, acc=acc: e.transpose(ps[bk][:, (c % 4) * 128:(c % 4 + 1) * 128], acc[:, c, bs], ident),
                         reads=[acc_b, cb], writes=[pb[bk]])
                cp(t32[:, 0:512], ps[4][:], [pb[4]], [t32_b], eng="act")
                cp(t32[:, 512:1024], ps[5][:], [pb[5]], [t32_b], eng="dve")
                dma("sp", H3_d[b * 128:(b + 1) * 128, :], t32[:], reads=[t32_b], writes=[h3_db])
        til = P.sb([128, 8], F32); offe = P.sb([128, 8], F32); off0 = P.sb([128, 8], F32)
        tmpA = P.sb([128, NB, 8], F32)
        es = P.sb([128, NS], F32); es2 = P.sb([128, NS], F32); tmp8 = P.sb([128, 8], F32)
        P.op("dve", lambda e: e.memset(til[:], 0.0), writes=[r_b])
        for k in range(NT * 2 + 1):
            stt(til[:], run[:], 512.0 * k, til[:], ALU.is_gt, ALU.add, [rt_b, r_b], [r_b])
        cp(offe[:, 0:1], til[:, 0:1], [r_b], [r_b])
        for e_ in range(1, 8):
            tt(offe[:, e_:e_ + 1], offe[:, e_ - 1:e_], til[:, e_:e_ + 1], ALU.add, [r_b], [r_b])
        ts(offe[:], offe[:], 512.0, ALU.mult, [r_b], [r_b])
        stt(off0[:], til[:], -512.0, offe[:], ALU.mult, ALU.add, [r_b], [r_b])
        for k_, eqA in enumerate((eq1A, eq2A)):
            tt(tmpA[:], rankA[:], off0[:, None, :].to_broadcast([128, NB, 8]), ALU.add, [rt_b, r_b], [r_b])
            tt(tmpA[:], tmpA[:], eqA[:], ALU.mult, [r_b, rt_b], [r_b])
            P.op("dve", lambda e, k_=k_: e.reduce_sum(out=posF[:, k_, :], in_=tmpA[:], axis=AX.X), reads=[r_b], writes=[pos_b])
        cp(posI[:], posF[:], [pos_b], [pos_b])
        for s_ in range(NS):
            ts(tmp8[:], offe[:], 512.0 * s_, ALU.is_le, [r_b], [r_b])
            P.op("dve", lambda e, s_=s_: e.reduce_sum(out=es[:, s_:s_ + 1], in_=tmp8[:], axis=AX.X), reads=[r_b], writes=[r_b])
        usedF = P.sb([128, NS], F32); usedI = P.sb([128, NS], I32)
        for s_ in range(NS):
            ts(usedF[:, s_:s_ + 1], offe[:, 7:8], 512.0 * s_, ALU.is_gt, [r_b], [pos_b])
        if DEBUG_ZERO_FLAGS:
            P.op("dve", lambda e: e.memset(usedF[:], 0.0), writes=[pos_b])
        cp(usedI[:], usedF[:], [pos_b], [pos_b])
        ts(es[:], es[:], 7.0, ALU.min, [r_b], [r_b])
        stt(wiF[:], es[:], 128.0, iot[:, 0:1].to_broadcast([128, NS]), ALU.mult, ALU.add, [iot_b, r_b], [wi_b])
        cp(wiI[:], wiF[:], [wi_b], [wi_b])
        for b in range(NB):
            tb_, tb_b = tokb[b % 2]
            dma("sp", tb_[:], Hn_d[b * 128:(b + 1) * 128, :], reads=[hn_db], writes=[tb_b])
            for k_ in range(2):
                P.op("pool", lambda e, tb_=tb_, k_=k_, b=b: e.indirect_dma_start(
                    out=Xs_d, out_offset=bass.IndirectOffsetOnAxis(ap=posI[:, k_, b:b + 1], axis=0), in_=tb_[:], in_offset=None),
                    reads=[tb_b, pos_b], writes=[xs_db], dma=True)
        P.barrier()
        P.sb_off = mark1
        NH = NF // 2
        wgu = [(P.sb([128, 2, 8, HF], BF16), Buf()) for _ in range(2)]
        wd = P.sb([128, NF, D], BF16); wd_b = Buf()
        aT = P.sb([128, NF, 512], BF16); a_b = Buf()
        xtok = P.sb([128, 4, D], BF16); xtok_b = Buf()
        xgT = P.sb([128, 8, 512], BF16); xg_b = Buf()
        yst = [(P.sb([128, D], F32), Buf()) for _ in range(2)]
        s_sb = [(P.sb([128, 512], F32), Buf()) for _ in range(2)]
        wg2 = [wb["wg_m"][h] for h in range(2)]
        wu2 = [wb["wu_m"][h] for h in range(2)]
        wd2 = [wb["wd_m"][h] for h in range(2)]
        allw = wbuf["wg_m"] + wbuf["wu_m"] + wbuf["wd_m"]

        def gather(out, src, idx_ap, reads, writes):
            P.op("pool", lambda e: e.indirect_dma_start(out=out, out_offset=None, in_=src,
                                                        in_offset=bass.IndirectOffsetOnAxis(ap=idx_ap, axis=0)),
                 reads=reads, writes=writes, dma=True)

        def load_gu(s_, h):
            wt, wt_b = wgu[h]
            gather(wt[:, 0, :, :].rearrange("p c n -> p (c n)"), wg2[h], wiI[:, s_:s_ + 1], [wi_b] + allw, [wt_b])
            gather(wt[:, 1, :, :].rearrange("p c n -> p (c n)"), wu2[h], wiI[:, s_:s_ + 1], [wi_b] + allw, [wt_b])

        def load_d(s_):
            for h in range(2):
                gather(wd[:, h * NH:(h + 1) * NH, :].rearrange("p f n -> p (f n)"), wd2[h], wiI[:, s_:s_ + 1], [wi_b] + allw, [wd_b])

        load_gu(0, 0)
        load_gu(0, 1)
        it_y = 0
        def load_x(s_):
            dma("sp", xtok[:], Xs_d[s_ * 512:(s_ + 1) * 512, :].rearrange("(j p) d -> p j d", p=128), reads=[xs_db], writes=[xtok_b])

        load_x(0)
        for s_ in range(NS):
            if SKIP_SLOTS and s_ >= (2 * S) // 512:
                P.cur_grp = s_
                P.grp_flag[s_] = usedI[0:1, s_:s_ + 1]
            p0 = ps[0][:].bitcast(BF16)
            for c in range(8):
                for j in range(4):
                    P.op("pe", lambda e, c=c, j=j: e.transpose(p0[:, j * 128:(j + 1) * 128], xtok[:, j, c * 128:(c + 1) * 128], identb),
                         reads=[xtok_b, cb], writes=[pb[0]])
                cp(xgT[:, c, :], p0[:, 0:512], [pb[0]], [xg_b], eng=("act" if c % 2 == 0 else "dve"))
            if s_ + 1 < NS:
                load_x(s_ + 1)
            load_d(s_)
            for h in range(2):
                wt, wt_b = wgu[h]
                for fl in range(NH):
                    f = h * NH + fl
                    gbk, ubk = 4 + (f % 2) * 2, 5 + (f % 2) * 2
                    for c in range(8):
                        mm(ps[gbk][:], wt[:, 0, c, fl * 128:(fl + 1) * 128], xgT[:, c, :], c == 0, c == 7, [wt_b, xg_b], [pb[gbk]])
                    for c in range(8):
                        mm(ps[ubk][:], wt[:, 1, c, fl * 128:(fl + 1) * 128], xgT[:, c, :], c == 0, c == 7, [wt_b, xg_b], [pb[ubk]])
                    ss_, ss_b = s_sb[f % 2]
                    act(ss_[:], ps[gbk][:], AF.Silu, [pb[gbk]], [ss_b])
                    tt(aT[:, f, :], ps[ubk][:], ss_[:], ALU.mult, [pb[ubk], ss_b], [a_b])
                if s_ + 1 < NS:
                    load_gu(s_ + 1, h)
            for j in range(4):
                ys_, ys_b = yst[it_y % 2]
                it_y += 1
                for dh in range(2):
                    bk = (j * 2 + dh) % 4
                    for f in range(NF):
                        mm(ps[bk][:], aT[:, f, j * 128:(j + 1) * 128], wd[:, f, dh * 512:(dh + 1) * 512], f == 0, f == NF - 1,
                           [a_b, wd_b], [pb[bk]])
                    cp(ys_[:, dh * 512:(dh + 1) * 512], ps[bk][:], [pb[bk]], [ys_b], eng=("act" if dh == 0 else "dve"))
                dma("sp", Ys_d[s_ * 512 + j * 128:s_ * 512 + (j + 1) * 128, :], ys_[:], reads=[ys_b], writes=[ys_db])
        P.cur_grp = None
        P.barrier()
        P.sb_off = mark1
        finbc = P.sb([128, D], F32); fin_b = Buf()
        dma("sp", finbc[:], fin_row.partition_broadcast(128), writes=[fin_b])
        bufs4 = [tuple((P.sb([128, D], F32), Buf()) for _ in range(4)) for _ in range(2)]
        st4 = [(P.sb([128, 4], F32), Buf()) for _ in range(2)]
        out_b = Buf()
        def load4(b):
            (y1, y1_b), (y2, y2_b), (h3, h3_b), (o, o_b) = bufs4[b % 2]
            gather(y1[:], Ys_d, posI[:, 0, b:b + 1], [pos_b, ys_db], [y1_b])
            gather(y2[:], Ys_d, posI[:, 1, b:b + 1], [pos_b, ys_db], [y2_b])
            dma("sp", h3[:], H3_d[b * 128:(b + 1) * 128, :], reads=[h3_db], writes=[h3_b])

        load4(0)
        for b in range(NB):
            (y1, y1_b), (y2, y2_b), (h3, h3_b), (o, o_b) = bufs4[b % 2]
            st, st_b = st4[b % 2]
            if b + 1 < NB:
                load4(b + 1)
            stt(h3[:], y1[:], gA[:, b, 0:1], h3[:], ALU.mult, ALU.add, [y1_b, h3_b, rt_b], [h3_b])
            stt(h3[:], y2[:], gA[:, b, 1:2], h3[:], ALU.mult, ALU.add, [y2_b, h3_b, rt_b], [h3_b])
            P.op("act", lambda e, o=o, h3=h3, st=st: e.activation(out=o[:], in_=h3[:], func=AF.Square, accum_out=st[:, 0:1]),
                 reads=[h3_b], writes=[o_b, st_b])
            act(st[:, 1:2], st[:, 0:1], AF.Sqrt, [st_b], [st_b], bias=EPS, scale=1.0 / D)
            recip(st[:, 2:3], st[:, 1:2], [st_b], [st_b])
            stt(o[:], h3[:], st[:, 2:3], finbc[:], ALU.mult, ALU.mult, [h3_b, st_b, fin_b, o_b], [o_b])
            dma("sp", out_nat[b * 128:(b + 1) * 128, :], o[:], reads=[o_b], writes=[out_b], is_out=True)
        P.sb_off = mark0

    if sparse and stop is None:
        moe_sparse()
        P.emit()
        return nc

    ffn_phase(h3T, h4T, gsb["fn_o"], moe=True)
    P.barrier()
    if stop == "moe":
        finish(h4T); P.emit(); return nc

    mark = P.sb_off
    xbufs = [(P.sb([128, 8, 512], F32), Buf()) for _ in range(2)]
    obufs = [(P.sb([128, 8, 512], F32), Buf()) for _ in range(2)]
    sqb = (P.sb([128, 8, 512], BF16), Buf())
    rsb = (P.sb([128, 512], F32), Buf())
    norm_tiles(h4T, hb[id(h4T)], gsb["fin"], range(NT), lambda t: (obufs[t % 2][0], obufs[t % 2][1]), 0, xbufs, sqb, rsb,
               keep32=lambda t, xs, xs_b, r, r_b: dma("sp", view_T(outT)[:, :, t * 512:(t + 1) * 512], obufs[t % 2][0][:],
                                                      reads=[obufs[t % 2][1]], writes=[hb[id(outT)]], is_out=True))
    P.sb_off = mark
    P.emit()
    return nc


def make_in_maps(inputs, S, ncores):
    x = np.asarray(inputs["x"], np.float32)
    def g8(v):
        return np.ascontiguousarray(np.asarray(v, np.float32).reshape(-1, 128).T)
    common = {
        "an_e": g8(inputs["attn_norm_even"][0]), "fn_e": g8(inputs["ffn_norm_even"][0]),
        "an_o": g8(inputs["attn_norm_odd"][0]), "fn_o": g8(inputs["ffn_norm_odd"][0]),
        "fin": g8(inputs["final_norm"]), "rn_e": g8(inputs["ret_norm_even"][0]),
        "fin_row": np.ascontiguousarray(np.asarray(inputs["final_norm"], np.float32).reshape(1, -1)),
        "bf_o": np.ascontiguousarray(np.asarray(inputs["b_forget_odd"][0], np.float32).reshape(16, 1)),
        "wr_o": np.ascontiguousarray(inputs["w_router_odd"][0], dtype=np.float32),
        "w_in_e": np.ascontiguousarray(inputs["w_in_even"][0], dtype=np.float32),
        "w_out_e": np.ascontiguousarray(inputs["w_out_even"][0], dtype=np.float32),
        "wg_e": np.ascontiguousarray(inputs["w_gate_even"][0], dtype=np.float32),
        "wu_e": np.ascontiguousarray(inputs["w_up_even"][0], dtype=np.float32),
        "wd_e": np.ascontiguousarray(inputs["w_down_even"][0], dtype=np.float32),
        "w_in_o": np.ascontiguousarray(inputs["w_in_odd"][0], dtype=np.float32),
        "w_out_o": np.ascontiguousarray(inputs["w_out_odd"][0], dtype=np.float32),
        "wg_m": np.ascontiguousarray(inputs["w_gate_moe_odd"][0], dtype=np.float32),
        "wu_m": np.ascontiguousarray(inputs["w_up_moe_odd"][0], dtype=np.float32),
        "wd_m": np.ascontiguousarray(inputs["w_down_moe_odd"][0], dtype=np.float32),
    }
    common.update(host_consts(S))
    maps = []
    for b in range(ncores):
        m = dict(common)
        m["xT"] = np.ascontiguousarray(x[b, :S].T)
        maps.append(m)
    return maps


def kernel(**inputs):
    S = 4096
    nc = build(S)
    maps = make_in_maps(inputs, S, 8)
    res = run_bass_kernel_spmd(nc, maps, core_ids=list(range(8)))
    out = np.stack([np.asarray(r["out"]) for r in res.results], axis=0)
    return out.astype(np.float32)
```

```python
import numpy as np
import concourse.bass as bass
import concourse.mybir as mybir
from concourse.bass_utils import run_bass_kernel_spmd

F32 = mybir.dt.float32
BF16 = mybir.dt.bfloat16
AF = mybir.ActivationFunctionType
ALU = mybir.AluOpType
AX = mybir.AxisListType

NDSEM = {"sp": 12, "pool": 28, "act": 1, "dve": 1, "pe": 1}


FUSE_WAIT = True


class Buf:
    __slots__ = ("w", "r")

    def __init__(self):
        self.w = None
        self.r = []


class Prog:
    ENGS = ("pe", "act", "dve", "pool", "sp")

    def __init__(self, nc):
        self.nc = nc
        self.lists = {e: [] for e in self.ENGS}
        self.sb_n = 0
        arena_bytes = nc.sbuf_bytes_remaining - 6144
        arena = nc.alloc_sbuf_tensor("arena", [128, arena_bytes], mybir.dt.uint8)
        self.sb_base = nc.lookup_mloc(arena).addr
        self.sb_off = self.sb_base
        self.sb_top = self.sb_base + arena_bytes
        self.out_events = []
        self.cur_grp = None
        self.replay = None
        self.replay_store = {}
        self.grp_flag = {}

    def begin_replay(self, key):
        first = key not in self.replay_store
        if first:
            self.replay_store[key] = []
        self.replay = [self.replay_store[key], 0, first]
        return first

    def end_replay(self):
        self.replay = None

    def _replayed(self, make):
        if self.replay is None:
            return make()
        store, idx, first = self.replay
        if first:
            obj = make()
            store.append(obj)
        else:
            obj = store[idx]
        self.replay[1] = idx + 1
        return obj

    def buf(self):
        return self._replayed(Buf)

    def sb(self, shape, dtype, off=None, name=None):
        return self._replayed(lambda: self._sb(shape, dtype, off, name))

    def _sb(self, shape, dtype, off=None, name=None):
        esz = 2 if dtype == BF16 else 4
        n = 1
        for s in shape[1:]:
            n *= s
        nbytes = n * esz
        if off is None:
            off = (self.sb_off + 63) // 64 * 64
            self.sb_off = off + nbytes
        assert off >= self.sb_base and off + nbytes <= self.sb_top, (off, nbytes, self.sb_top)
        self.sb_n += 1
        t = self.nc.alloc_sbuf_tensor_at(name or f"sb{self.sb_n}", list(shape), dtype, offset=off)
        return t

    def op(self, eng, fn, reads=(), writes=(), dma=False, is_out=False):
        lst = self.lists[eng]
        idx = len(lst)
        ev = ("dma", eng, idx) if dma else ("c", eng, idx)
        deps = set()
        for b in reads:
            if b.w is not None:
                deps.add(b.w)
        for b in writes:
            if b.w is not None:
                deps.add(b.w)
            for r in b.r:
                deps.add(r)
        if not dma:
            war_only = set()
            for b in writes:
                for r in b.r:
                    if r[0] == "c" and r[1] == eng:
                        war_only.add(r)
            for b in reads:
                if b.w in war_only:
                    war_only.discard(b.w)
            for b in writes:
                if b.w in war_only:
                    war_only.discard(b.w)
            if eng == "pe":
                deps -= war_only
            if eng == "pe":
                deps = {d for d in deps if not (d[0] == "c" and d[1] == "pe")}
        deps.discard(ev)
        lst.append({"fn": fn, "deps": deps, "dma": dma, "marked": False, "grp": self.cur_grp})
        for b in reads:
            b.r.append(ev)
        for b in writes:
            b.w = ev
            b.r = []
        if is_out:
            self.out_events.append(ev)
        return ev

    def barrier(self):
        evs = set()
        for e in self.ENGS:
            lst = self.lists[e]
            last_c = None
            for i in range(len(lst) - 1, -1, -1):
                if not lst[i]["dma"] and lst[i]["fn"] is not None:
                    last_c = ("c", e, i)
                    break
            if last_c:
                evs.add(last_c)
            for i, r in enumerate(lst):
                if r["dma"] and not r.get("barriered"):
                    evs.add(("dma", e, i))
                    r["barriered"] = True
        for e in self.ENGS:
            self.lists[e].append({"fn": None, "deps": {d for d in evs if not (d[0] == "c" and d[1] == e)},
                                  "dma": False, "marked": False})

    def emit(self):
        nc = self.nc
        lists = self.lists
        for e in self.ENGS:
            seen_c = {}
            seen_d = set()
            for rec in lists[e]:
                best = {}
                dd = set()
                for d in rec["deps"]:
                    if d[0] == "c":
                        if d[2] > best.get(d[1], -1):
                            best[d[1]] = d[2]
                    else:
                        dd.add(d)
                waits = []
                for e2, i2 in best.items():
                    if seen_c.get(e2, -1) >= i2:
                        continue
                    seen_c[e2] = i2
                    waits.append(("c", e2, i2))
                    lists[e2][i2]["marked"] = True
                for d in dd:
                    if d in seen_d:
                        continue
                    seen_d.add(d)
                    waits.append(d)
                rec["waits"] = waits
        fin = []
        for d in self.out_events:
            fin.append(d)
        csem = {e: nc.alloc_semaphore(f"c_{e}") for e in self.ENGS}
        dsem = {e: [nc.alloc_semaphore(f"d_{e}{k}") for k in range(NDSEM[e])] for e in self.ENGS}
        for e in self.ENGS:
            cnt = 0
            nd = 0
            tot = [0] * NDSEM[e]
            for rec in lists[e]:
                if rec["dma"]:
                    slot = nd % NDSEM[e]
                    rec["slot"] = slot
                    rec["prev"] = tot[slot]
                    tot[slot] += 16
                    rec["val"] = tot[slot]
                    nd += 1
                elif rec["marked"]:
                    cnt += 1
                    rec["val"] = cnt
        engobj = {"pe": "tensor", "act": "scalar", "dve": "vector", "pool": "gpsimd", "sp": "sync"}

        def emit_eng(e, eng):
            lst = lists[e]
            reg = None
            n = len(lst)
            k = 0
            while k < n:
                g = lst[k].get("grp")
                k2 = k
                while k2 < n and lst[k2].get("grp") == g:
                    k2 += 1
                seg = lst[k:k2]
                if g is None:
                    for rec in seg:
                        emit_rec(e, eng, rec)
                else:
                    if reg is None:
                        reg = eng.alloc_register(f"flag_{e}")
                    eng.reg_load(reg, self.grp_flag[g])
                    gd = eng.If_ne(reg, 0)
                    gd.__enter__()
                    for rec in seg:
                        emit_rec(e, eng, rec)
                    gd.__exit__(None, None, None)
                    ncomp = sum(1 for rec in seg if (not rec["dma"]) and rec["marked"] and rec["fn"] is not None)
                    dcomp = {}
                    for rec in seg:
                        if rec["dma"] and rec["fn"] is not None:
                            dcomp[rec["slot"]] = dcomp.get(rec["slot"], 0) + 16
                    if ncomp or dcomp:
                        ge = eng.Else()
                        ge.__enter__()
                        if ncomp:
                            eng.sem_inc(csem[e], ncomp)
                        for sl, v in dcomp.items():
                            eng.sem_inc(dsem[e][sl], v)
                        ge.__exit__(None, None, None)
                k = k2
            if e == "sp":
                for w in fin:
                    r2 = lists[w[1]][w[2]]
                    eng.wait_ge(dsem[w[1]][r2["slot"]], r2["val"])

        def emit_rec(e, eng, rec):
            if True:
                wl = []
                for w in rec["waits"]:
                    r2 = lists[w[1]][w[2]]
                    if w[0] == "c":
                        wl.append((csem[w[1]], r2["val"]))
                    else:
                        wl.append((dsem[w[1]][r2["slot"]], r2["val"]))
                if rec["dma"] and rec["fn"] is not None and rec["prev"] > 0:
                    wl.append((dsem[e][rec["slot"]], rec["prev"]))
                fuse = None
                if FUSE_WAIT and rec["fn"] is not None and not rec["dma"] and wl:
                    fuse = wl.pop()
                for sm_, v_ in wl:
                    eng.wait_ge(sm_, v_)
                if rec["fn"] is None:
                    return
                ins = rec["fn"](eng)
                if fuse is not None:
                    ins._wait_ge(fuse[0], fuse[1])
                if rec["dma"]:
                    ins.then_inc(dsem[e][rec["slot"]], 16)
                elif rec["marked"]:
                    ins.then_inc(csem[e], 1)

        with nc.Block() as block:
            @block.tensor
            def _(eng):
                emit_eng("pe", eng)

            @block.scalar
            def _(eng):
                emit_eng("act", eng)

            @block.vector
            def _(eng):
                emit_eng("dve", eng)

            @block.gpsimd
            def _(eng):
                emit_eng("pool", eng)

            @block.sync
            def _(eng):
                emit_eng("sp", eng)

D = 1024
NCH = 8
DFF = 2816
NF = 22
NE = 8
EPS = 1e-6
GN_EPS = 1e-5

C_ID, C_ONE, C_TRI, C_MS, C_MI, C_BD, C_BM, C_PM = [i * 128 for i in range(8)]


def host_consts(S):
    i = np.arange(128)
    cst = np.zeros((128, 1024), np.float32)
    cst[:, C_ID:C_ID + 128] = np.eye(128)
    cst[:, C_ONE:C_ONE + 128] = 1.0
    cst[:, C_TRI:C_TRI + 128] = (i[:, None] >= i[None, :])
    cst[:, C_MS:C_MS + 128] = (i[:, None] < i[None, :])
    cst[:, C_MI:C_MI + 128] = (i[:, None] <= i[None, :])
    blk = (i[:, None] // 64 == i[None, :] // 64)
    cst[:, C_BD:C_BD + 128] = blk / 64.0
    cst[:, C_BM:C_BM + 128] = blk
    partner = (i // 64) * 64 + (i % 64 + 32) % 64
    pm = np.zeros((128, 128), np.float32)
    pm[partner, i] = 1.0
    cst[:, C_PM:C_PM + 128] = pm
    half = 32
    inv_freq = (10000.0 ** (-np.arange(half, dtype=np.float32) / half)).astype(np.float32)
    ang = (np.arange(S, dtype=np.float32)[:, None] * inv_freq[None, :]).astype(np.float32)
    cos = np.cos(ang).astype(np.float32).T
    sin = np.sin(ang).astype(np.float32).T
    d = i % 64
    cosT = cos[d % 32]
    sinS = np.where((d < 32)[:, None], -sin[d % 32], sin[d % 32])
    rot = np.stack([cosT, sinS, cosT * 0.125, sinS * 0.125]).astype(np.float32)
    lg = np.log(1.0 - 2.0 ** (-5.0 - np.arange(8, dtype=np.float32))).astype(np.float32)
    pos = np.arange(128, dtype=np.float32)
    rdt = np.zeros((4, 128, 256), np.float32)
    rkd = np.zeros((4, 128, 128), np.float32)
    rqd = np.zeros((4, 128, 512), np.float32)
    rcd = np.zeros((128, 4), np.float32)
    for j in range(4):
        for hh in range(2):
            g = lg[2 * j + hh]
            diff = pos[None, :] - pos[:, None]
            rdt[j, :, hh * 128:(hh + 1) * 128] = np.where(diff >= 0, np.exp(g * np.maximum(diff, 0)), 0.0)
            rkd[j, :, hh * 64:(hh + 1) * 64] = np.exp(g * (127.0 - pos))[:, None]
            rqd[j, hh * 64:(hh + 1) * 64, :] = np.tile(np.exp(g * (pos + 1.0)), 4)[None, :]
            rcd[hh * 64:(hh + 1) * 64, j] = np.exp(g * 128.0)
    iot = (np.arange(22, dtype=np.float32)[None, :] * 128.0 + np.arange(128, dtype=np.float32)[:, None]).astype(np.float32)
    return {"cst": cst, "rot": rot, "rdt": rdt, "rkd": rkd, "rqd": rqd, "rcd": rcd, "iot": iot}


SB_WIDE = False
GT_OVERRIDE = None
SKIP_SLOTS = False
DEBUG_ZERO_FLAGS = False


def build(S, stop=None, sparse=True, nfill_sb=0, nfill_fox=0):
    nc = bass.Bass("TRN2", target_bir_lowering=False)
    P = Prog(nc)
    NT = S // 512
    NB = S // 128
    GT = GT_OVERRIDE or (2 if NT >= 2 else 1)
    NG = NT // GT

    def din(name, shape):
        return nc.dram_tensor(name, list(shape), F32, kind="ExternalInput").ap()

    def dscr(name, shape, dt):
        return nc.dram_tensor(name, list(shape), dt).ap()

    xT = din("xT", [D, S])
    outT = None if (sparse and stop is None) else nc.dram_tensor("outT", [D, S], F32, kind="ExternalOutput").ap()
    gains = {n: din(n, [128, 8]) for n in ("an_e", "fn_e", "an_o", "fn_o", "fin")}
    rn_e = din("rn_e", [128, 4])
    bf_o = din("bf_o", [16, 1])
    wr_o = din("wr_o", [D, 8])
    wsrc = {
        "w_in_e": din("w_in_e", [D, 3584]), "w_out_e": din("w_out_e", [D, D]),
        "wg_e": din("wg_e", [D, DFF]), "wu_e": din("wu_e", [D, DFF]), "wd_e": din("wd_e", [DFF, D]),
        "w_in_o": din("w_in_o", [D, 3088]), "w_out_o": din("w_out_o", [D, D]),
        "wg_m": din("wg_m", [NE, D, DFF]), "wu_m": din("wu_m", [NE, D, DFF]), "wd_m": din("wd_m", [NE, DFF, D]),
    }
    cst_d = din("cst", [128, 1024])
    rot_d = din("rot", [4, 128, S])
    rdt_d = din("rdt", [4, 128, 256])
    rkd_d = din("rkd", [4, 128, 128])
    rqd_d = din("rqd", [4, 128, 512])
    rcd_d = din("rcd", [128, 4])
    iot_d = din("iot", [128, 22])
    fin_row = din("fin_row", [1, D])
    NS = (2 * S) // 512 + NE
    HF = DFF // 2
    out_nat = nc.dram_tensor("out", [S, D], F32, kind="ExternalOutput").ap() if (sparse and stop is None) else None
    wb = {k: dscr(k + "_b", v.shape, BF16) for k, v in wsrc.items() if not (sparse and k in ("wg_m", "wu_m", "wd_m"))}
    if sparse:
        wb["wg_m"] = [dscr(f"wgm2_{h}", [NE * 128, 8 * HF], BF16) for h in range(2)]
        wb["wu_m"] = [dscr(f"wum2_{h}", [NE * 128, 8 * HF], BF16) for h in range(2)]
        wb["wd_m"] = [dscr(f"wdm2_{h}", [NE * 128, (NF // 2) * D], BF16) for h in range(2)]
        Hn_d = dscr("Hn", [S, D], BF16)
        H3_d = dscr("H3tok", [S, D], F32)
        Xs_d = dscr("Xs", [NS * 512, D], BF16)
        Ys_d = dscr("Ys", [NS * 512, D], F32)
    wbuf = {}
    h1T = dscr("h1T", [D, S], F32)
    h2T = dscr("h2T", [D, S], F32)
    h3T = dscr("h3T", [D, S], F32)
    h4T = dscr("h4T", [D, S], F32)
    hb = {id(t): Buf() for t in (h1T, h2T, h3T, h4T, outT, xT)}
    nullb = Buf()

    def dma(q, out, in_, reads=(), writes=(), is_out=False, **kw):
        return P.op(q, lambda e: e.dma_start(out=out, in_=in_, **kw), reads=reads, writes=writes, dma=True, is_out=is_out)

    def mm(out, lhsT, rhs, start, stop, reads, writes, skip=False):
        if skip:
            return P.op("pe", lambda e: e.matmul(out, lhsT=lhsT, rhs=rhs, start=start, stop=stop, skip_group_check=True),
                        reads=reads, writes=writes)
        return P.op("pe", lambda e: e.matmul(out, lhsT=lhsT, rhs=rhs, start=start, stop=stop), reads=reads, writes=writes)

    def act(out, in_, func, reads, writes, bias=None, scale=None):
        kw = {}
        if bias is not None:
            kw["bias"] = bias
        if scale is not None:
            kw["scale"] = scale
        return P.op("act", lambda e: e.activation(out=out, in_=in_, func=func, **kw), reads=reads, writes=writes)

    def tt(out, in0, in1, op, reads, writes, eng="dve"):
        return P.op(eng, lambda e: e.tensor_tensor(out=out, in0=in0, in1=in1, op=op), reads=reads, writes=writes)

    def ts(out, in0, s1, op0, reads, writes, s2=None, op1=None, eng="dve"):
        if op1 is None:
            return P.op(eng, lambda e: e.tensor_scalar(out=out, in0=in0, scalar1=s1, scalar2=None, op0=op0), reads=reads, writes=writes)
        return P.op(eng, lambda e: e.tensor_scalar(out=out, in0=in0, scalar1=s1, scalar2=s2, op0=op0, op1=op1), reads=reads, writes=writes)

    def stt(out, in0, scalar, in1, op0, op1, reads, writes, eng="dve"):
        return P.op(eng, lambda e: e.scalar_tensor_tensor(out=out, in0=in0, scalar=scalar, in1=in1, op0=op0, op1=op1),
                    reads=reads, writes=writes)

    def cp(out, in_, reads, writes, eng="dve"):
        if eng == "act":
            return P.op("act", lambda e: e.copy(out=out, in_=in_), reads=reads, writes=writes)
        return P.op(eng, lambda e: e.tensor_copy(out=out, in_=in_), reads=reads, writes=writes)

    def recip(out, in_, reads, writes):
        return P.op("dve", lambda e: e.reciprocal(out=out, in_=in_), reads=reads, writes=writes)

    def cast_w(name):
        src, dst = wsrc[name], wb[name]
        if len(src.shape) == 3:
            bl = []
            for e_ in range(src.shape[0]):
                b = Buf()
                dma("pool", dst[e_], src[e_], writes=[b], max_dma_last_dim=4096)
                bl.append(b)
            wbuf[name] = bl
        else:
            b = Buf()
            dma("pool", dst, src, writes=[b], max_dma_last_dim=4096)
            wbuf[name] = b

    for name in ("w_in_e", "w_out_e"):
        cast_w(name)
    for name in ("wg_m", "wu_m", "wd_m"):
        wbuf[name] = []

    def cast_expert(e_):
        for name in ("wg_m", "wu_m", "wd_m"):
            if not sparse:
                b = Buf()
                dma("pool", wb[name][e_], wsrc[name][e_], writes=[b], max_dma_last_dim=4096)
                wbuf[name].append(b)
                continue
            for h in range(2):
                b = Buf()
                if name == "wd_m":
                    for fl in range(NF // 2):
                        r0 = h * HF + fl * 128
                        dma("pool", wb[name][h][e_ * 128:(e_ + 1) * 128, fl * D:(fl + 1) * D], wsrc[name][e_][r0:r0 + 128, :], writes=[b],
                            max_dma_last_dim=4096)
                else:
                    for c in range(8):
                        dma("pool", wb[name][h][e_ * 128:(e_ + 1) * 128, c * HF:(c + 1) * HF],
                            wsrc[name][e_][c * 128:(c + 1) * 128, h * HF:(h + 1) * HF], writes=[b], max_dma_last_dim=2816)
                wbuf[name].append(b)

    cst = P.sb([128, 1024], F32)
    cstb = P.sb([128, 1024], BF16)
    cb = Buf()
    gsb = {n: P.sb([128, 8], F32) for n in gains}
    rn_sb = P.sb([128, 4], F32)
    rcd_sb = P.sb([128, 4], F32)
    gb = Buf()
    dma("sp", cst[:], cst_d, writes=[cb])
    cp(cstb[:], cst[:], [cb], [cb])
    for n in gains:
        dma("sp", gsb[n][:], gains[n], writes=[gb])
    dma("sp", rn_sb[:], rn_e, writes=[gb])
    dma("sp", rcd_sb[:], rcd_d, writes=[gb])
    ident = cst[:, C_ID:C_ID + 128]
    ones32 = cst[:, C_ONE:C_ONE + 128]
    blockmask = cst[:, C_BM:C_BM + 128]
    pm32 = cst[:, C_PM:C_PM + 128]
    identb = cstb[:, C_ID:C_ID + 128]
    onesb = cstb[:, C_ONE:C_ONE + 128]
    trib = cstb[:, C_TRI:C_TRI + 128]
    maskSb = cstb[:, C_MS:C_MS + 128]
    maskIb = cstb[:, C_MI:C_MI + 128]
    maskS32 = cst[:, C_MS:C_MS + 128]
    bdb = cstb[:, C_BD:C_BD + 128]

    psall = nc.alloc_psum_tensor("psall", [128, 4096], F32)
    ps = [psall[:, i * 512:(i + 1) * 512] for i in range(8)]
    pb = [Buf() for _ in range(8)]
    persist_mark = P.sb_off
    if stop == "cast":
        hb[id(xT)] = nullb
        finish_early = True
    else:
        finish_early = False

    def view_T(dr):
        return dr.rearrange("(c p) s -> p c s", p=128)

    def finish(src):
        mark = P.sb_off
        xb_ = [(P.sb([128, 8, 512], F32), Buf()) for _ in range(2)]
        for t in range(NT):
            xs, xs_b = xb_[t % 2]
            dma("sp", xs[:], view_T(src)[:, :, t * 512:(t + 1) * 512], reads=[hb[id(src)]], writes=[xs_b])
            dma("sp", view_T(outT)[:, :, t * 512:(t + 1) * 512], xs[:], reads=[xs_b], writes=[hb[id(outT)]], is_out=True)
        P.sb_off = mark


    def norm_tiles(src, src_b, gain, tiles, dst_fn, pbank, xbufs, sqb, rsb, keep32=None):
        sqs = sqb if isinstance(sqb, list) else [sqb]
        rss = rsb if isinstance(rsb, list) else [rsb]
        pbank0 = pbank
        for n, t in enumerate(tiles):
            sq, sq_b = sqs[n % len(sqs)]
            r, r_b = rss[n % len(rss)]
            pbank = pbank0 + (n % len(sqs))
            xs, xs_b = xbufs[n % len(xbufs)]
            dma("sp", xs[:], view_T(src)[:, :, t * 512:(t + 1) * 512], reads=[src_b], writes=[xs_b])
            act(sq[:], xs[:], AF.Square, [xs_b], [sq_b])
            for c in range(8):
                mm(ps[pbank][:], onesb, sq[:, c, :], c == 0, c == 7, [cb, sq_b], [pb[pbank]])
            act(r[:], ps[pbank][:], AF.Sqrt, [pb[pbank]], [r_b], bias=EPS, scale=1.0 / D)
            recip(r[:], r[:], [r_b], [r_b])
            o, o_b = dst_fn(t)
            for c in range(8):
                stt(o[:, c, :], xs[:, c, :], gain[:, c:c + 1], r[:], ALU.mult, ALU.mult, [xs_b, r_b, gb], [o_b])
            if keep32 is not None:
                keep32(t, xs, xs_b, r, r_b)

    def out_proj(yT, y_b, wname, res, dst):
        mark = P.sb_off
        wo = P.sb([128, 8, D], BF16)
        wo_b = Buf()
        dma("sp", wo[:], wb[wname].rearrange("(c p) n -> p c n", p=128), reads=[wbuf[wname]], writes=[wo_b])
        xb2 = [(P.sb([128, 8, 512], F32), Buf()) for _ in range(2)]
        def load_res(t):
            xs, xs_b = xb2[t % 2]
            dma("sp", xs[:], view_T(res)[:, :, t * 512:(t + 1) * 512], reads=[hb.get(id(res), nullb)], writes=[xs_b])

        load_res(0)
        for t in range(NT):
            xs, xs_b = xb2[t % 2]
            if t + 1 < NT:
                load_res(t + 1)
            for m in range(8):
                bk = m % 2
                for c in range(8):
                    mm(ps[bk][:], wo[:, c, m * 128:(m + 1) * 128], yT[:, c, t * 512:(t + 1) * 512], c == 0, c == 7,
                       [wo_b, y_b], [pb[bk]])
                tt(xs[:, m, :], ps[bk][:], xs[:, m, :], ALU.add, [pb[bk], xs_b], [xs_b])
            dma("sp", view_T(dst)[:, :, t * 512:(t + 1) * 512], xs[:], reads=[xs_b], writes=[hb[id(dst)]])
        P.sb_off = mark

    if finish_early:
        xb_ = [(P.sb([128, 8, 512], F32), Buf()) for _ in range(2)]
        for t in range(NT):
            xs, xs_b = xb_[t % 2]
            dma("sp", xs[:], view_T(xT)[:, :, t * 512:(t + 1) * 512], writes=[xs_b])
            dma("sp", view_T(outT)[:, :, t * 512:(t + 1) * 512], xs[:], reads=[xs_b], writes=[hb[id(outT)]], is_out=True)
        P.barrier()
        P.emit()
        return nc
    hnT = P.sb([128, 8, S], BF16)
    hn_b = Buf()
    yT = P.sb([128, 8, S], BF16)
    y_b = Buf()
    l0_mark = P.sb_off
    xbufs = [(P.sb([128, 8, 512], F32), Buf()) for _ in range(2)]
    sqb = [(P.sb([128, 8, 512], BF16), Buf()) for _ in range(2)]
    rsb = [(P.sb([128, 512], F32), Buf()) for _ in range(2)]
    norm_tiles(xT, nullb, gsb["an_e"], range(NT), lambda t: (hnT[:, :, t * 512:(t + 1) * 512], hn_b), 0, xbufs, sqb, rsb)
    P.sb_off = l0_mark
    P.barrier()

    for name in ("wg_e", "wu_e", "wd_e", "w_in_o", "w_out_o"):
        cast_w(name)
    w_in_e_v = wb["w_in_e"].rearrange("(c p) n -> p c n", p=128)

    def retention():
        mark = P.sb_off
        wq = P.sb([128, 4, 8, 128], BF16, off=None) if False else None
        wts = [P.sb([128, 4, 8, 128], BF16) for _ in range(1)]
        wall = wts[0]
        w_b = Buf()
        dtab = P.sb([128, 256], F32)
        kdt = P.sb([128, 128], F32)
        qdt = P.sb([128, 512], F32)
        tb = Buf()
        S32 = [P.sb([128, 128], F32) for _ in range(4)]
        Sbf = [P.sb([128, 128], BF16) for _ in range(4)]
        S_b = [Buf() for _ in range(4)]
        for j in range(4):
            P.op("dve", lambda e, j=j: e.memset(S32[j][:], 0.0), writes=[S_b[j]])
            P.op("dve", lambda e, j=j: e.memset(Sbf[j][:], 0.0), writes=[S_b[j]])
        rot = [(P.sb([128, 4, 512], F32), Buf()) for _ in range(2)]
        WQ, WK, WV, WG = 0, 1, 2, 3
        q32 = P.sb([128, 512], F32); q32_b = Buf()
        ta = P.sb([128, 512], F32); ta_b = Buf()
        tb2 = P.sb([128, 512], F32); tb2_b = Buf()
        qr = P.sb([128, 512], BF16); qr_b = Buf()
        qd = P.sb([128, 512], BF16); qd_b = Buf()
        kr = P.sb([128, 512], BF16); kr_b = Buf()
        kdk = P.sb([128, 4, 128], BF16); kdk_b = Buf()
        vtk = P.sb([128, 4, 128], BF16); vtk_b = Buf()
        sg = P.sb([128, 512], F32); sg_b = Buf()
        sm = [(P.sb([128, 128], BF16), Buf()) for _ in range(2)]
        o32 = P.sb([128, 512], F32); o32_b = Buf()
        obf = P.sb([128, 512], BF16); obf_b = Buf()
        cen = P.sb([128, 512], F32); cen_b = Buf()
        c2 = P.sb([128, 512], BF16); c2_b = Buf()
        rs = P.sb([128, 512], F32); rs_b = Buf()

        def proj_fm(wt, j, t, bank):
            for c in range(8):
                mm(ps[bank][:], wall[:, wt, c, :], hnT[:, c, t * 512:(t + 1) * 512], c == 0, c == 7, [w_b, hn_b], [pb[bank]])

        def rotary(j, t, wt, ci, out_bf, out_b, rt, rt_b, dec=None):
            proj_fm(wt, j, t, 0)
            cp(q32[:], ps[0][:], [pb[0]], [q32_b], eng="act")
            mm(ps[1][:], pm32, q32[:], True, True, [cb, q32_b], [pb[1]])
            tt(ta[:], q32[:], rt[:, ci, :], ALU.mult, [q32_b, rt_b], [ta_b])
            tt(tb2[:], ps[1][:], rt[:, ci + 1, :], ALU.mult, [pb[1], rt_b], [tb2_b])
            tt(ta[:], ta[:], tb2[:], ALU.add, [ta_b, tb2_b], [ta_b])
            cp(out_bf[:], ta[:], [ta_b], [out_b], eng="act")
            if dec is not None:
                tt(dec[0][:], ta[:], qdt[:], ALU.mult, [ta_b, tb], [dec[1]])

        it_ = 0
        for j in range(4):
            for k_ in range(4):
                c0 = k_ * 512 + j * 128
                dma("sp", wall[:, k_, :, :], w_in_e_v[:, :, c0:c0 + 128], reads=[wbuf["w_in_e"]], writes=[w_b])
            dma("sp", dtab[:], rdt_d[j], writes=[tb])
            dma("sp", kdt[:], rkd_d[j], writes=[tb])
            dma("sp", qdt[:], rqd_d[j], writes=[tb])
            for t in range(NT):
                rt, rt_b = rot[it_ % 2]
                it_ += 1
                for ci in range(4):
                    dma("sp", rt[:, ci, :], rot_d[ci, :, t * 512:(t + 1) * 512], writes=[rt_b])
                rotary(j, t, WQ, 0, qr, qr_b, rt, rt_b, dec=(qd, qd_b))
                rotary(j, t, WK, 2, kr, kr_b, rt, rt_b)
                proj_fm(WG, j, t, 2)
                act(sg[:], ps[2][:], AF.Silu, [pb[2]], [sg_b])
                for n in range(4):
                    tok = slice(t * 512 + n * 128, t * 512 + (n + 1) * 128)
                    for c in range(8):
                        mm(ps[3][:, n * 128:(n + 1) * 128], hnT[:, c, tok], wall[:, WV, c, :], (n == 0 and c == 0), c == 7,
                           [hn_b, w_b], [pb[3]], skip=True)
                cp(vtk[:].rearrange("p n f -> p (n f)"), ps[3][:], [pb[3]], [vtk_b], eng="act")
                ktp = ps[4][:].bitcast(BF16)
                for n in range(4):
                    P.op("pe", lambda e, n=n: e.transpose(ktp[:, n * 128:(n + 1) * 128], kr[:, n * 128:(n + 1) * 128], identb),
                         reads=[kr_b, cb], writes=[pb[4]])
                tt(kdk[:].rearrange("p n f -> p n f"), ktp[:, 0:512].rearrange("p (n f) -> p n f", n=4),
                   kdt[:, None, :].to_broadcast([128, 4, 128]), ALU.mult, [pb[4], tb], [kdk_b])
                for n in range(4):
                    cs = slice(n * 128, (n + 1) * 128)
                    for hh in range(2):
                        hs = slice(hh * 64, (hh + 1) * 64)
                        bk = 5 + hh
                        smt, smt_b = sm[hh]
                        mm(ps[7][:, hh * 128:(hh + 1) * 128], kr[hs, cs], qr[hs, cs], True, True, [kr_b, qr_b], [pb[7]], skip=True)
                        tt(smt[:], ps[7][:, hh * 128:(hh + 1) * 128], dtab[:, hh * 128:(hh + 1) * 128], ALU.mult,
                           [pb[7], tb], [smt_b])
                        mm(ps[bk][:, cs], vtk[:, n, :], smt[:], (n == 0), False, [vtk_b, smt_b], [pb[bk]], skip=True)
                        mm(ps[bk][:, cs], Sbf[j][:], qd[:, cs], False, True, [S_b[j], qd_b], [pb[bk]], skip=True)
                    mm(ps[1][:, 0:128], kdk[:, n, :], vtk[:, n, :], True, True, [kdk_b, vtk_b], [pb[1]])
                    stt(S32[j][:], S32[j][:], rcd_sb[:, j:j + 1], ps[1][:, 0:128], ALU.mult, ALU.add, [S_b[j], pb[1], gb], [S_b[j]])
                    tt(Sbf[j][:], S32[j][:], blockmask, ALU.mult, [S_b[j], cb], [S_b[j]])
                cp(o32[0:64, :], ps[5][0:64, :], [pb[5]], [o32_b], eng="act")
                cp(o32[64:128, :], ps[6][64:128, :], [pb[6]], [o32_b], eng="act")
                cp(obf[:], o32[:], [o32_b], [obf_b], eng="act")
                mm(ps[0][:], bdb, obf[:], True, True, [cb, obf_b], [pb[0]])
                tt(cen[:], o32[:], ps[0][:], ALU.subtract, [o32_b, pb[0]], [cen_b])
                act(c2[:], cen[:], AF.Square, [cen_b], [c2_b])
                mm(ps[2][:], bdb, c2[:], True, True, [cb, c2_b], [pb[2]])
                act(rs[:], ps[2][:], AF.Sqrt, [pb[2]], [rs_b], bias=GN_EPS, scale=1.0)
                recip(rs[:], rs[:], [rs_b], [rs_b])
                tt(cen[:], cen[:], rs[:], ALU.mult, [cen_b, rs_b], [cen_b])
                stt(yT[:, j, t * 512:(t + 1) * 512], cen[:], rn_sb[:, j:j + 1], sg[:], ALU.mult, ALU.mult,
                    [cen_b, sg_b, gb], [y_b])
        P.sb_off = mark

    if stop == "norm0":
        finish(xT); P.emit(); return nc
    retention()
    P.barrier()
    if stop == "ret":
        finish(xT); P.emit(); return nc

    def attention_pair(kind, w_v, wname, qc0, kc0, vc0, ychunk, btab=None, hidx=None):
        mark = P.sb_off
        first_call = P.begin_replay(kind)
        Buf = P.buf
        wq = P.sb([128, 8, 128], BF16)
        wk = P.sb([128, 8, 128], BF16)
        wv = P.sb([128, 8, 128], BF16)
        w_b = Buf()
        for wt, c0 in ((wq, qc0), (wk, kc0), (wv, vc0)):
            dma("sp", wt[:], w_v[:, :, c0:c0 + 128], reads=[wbuf[wname]], writes=[w_b])
        qTp = P.sb([128, S], BF16); q_b = Buf()
        kTp = P.sb([128, S], BF16); k_b = Buf()
        if kind == "sb":
            vt = [P.sb([128, NB, 128], BF16)]
        else:
            vt = [P.sb([128, NB, 128], BF16), P.sb([128, NB, 128], BF16)]
        v_b = Buf()
        for t in range(NT):
            ts_ = slice(t * 512, (t + 1) * 512)
            for (wt, dst, db, bk) in ((wq, qTp, q_b, 0), (wk, kTp, k_b, 1)):
                for c in range(8):
                    mm(ps[bk][:], wt[:, c, :], hnT[:, c, ts_], c == 0, c == 7, [w_b, hn_b], [pb[bk]])
                cp(dst[:, ts_], ps[bk][:], [pb[bk]], [db], eng=("act" if bk == 0 else "dve"))
            for n in range(4):
                tok = slice(t * 512 + n * 128, t * 512 + (n + 1) * 128)
                for c in range(8):
                    mm(ps[2][:, n * 128:(n + 1) * 128], hnT[:, c, tok], wv[:, c, :], (n == 0 and c == 0), c == 7,
                       [hn_b, w_b], [pb[2]], skip=True)
            if kind == "sb":
                cp(vt[0][:, t * 4:(t + 1) * 4, :].rearrange("p n f -> p (n f)"), ps[2][:], [pb[2]], [v_b], eng="act")
            else:
                pv = ps[2][:].rearrange("p (n f) -> p n f", n=4)
                cp(vt[0][:, t * 4:(t + 1) * 4, 0:64], pv[:, :, 0:64], [pb[2]], [v_b], eng="act")
                cp(vt[1][:, t * 4:(t + 1) * 4, 0:64], pv[:, :, 64:128], [pb[2]], [v_b], eng="dve")
        if kind == "fox" and first_call:
            for hh in range(2):
                P.op("dve", lambda e, hh=hh: e.memset(vt[hh][:, :, 64:128], 1.0), writes=[v_b])
        if kind == "sb" and SB_WIDE:
            PD = 3
            es_p = [(P.sb([128, 2, 512], F32), Buf()) for _ in range(PD)]
            Ls_p = [(P.sb([128, 2, 512], BF16), Buf()) for _ in range(PD)]
            gs_p = [(P.sb([128, 2, 512], F32), Buf()) for _ in range(2)]
            ws_p = [(P.sb([128, 2, 512], BF16), Buf()) for _ in range(PD)]
            la = P.sb([128, 2, 512], BF16); la_b = Buf()
            zbufs = [Buf(), Buf()]; cbuf = [Buf()]
            pzs = [psall[:, k * 1024:(k + 1) * 1024].rearrange("p (h n) -> p h n", h=2) for k in range(2)]
            pc = [psall[:, 2048:3072].rearrange("p (h n) -> p h n", h=2)]
            punits = []
            for qt in range(NT):
                nblk = 4 * qt + 4
                for idx, jb in enumerate(range(nblk - 1, -1, -1)):
                    punits.append((qt, jb, idx == 0, idx == nblk - 1))

            def pgeom(u):
                qt, jb, first, last = u
                c0 = 128 * max(0, jb - 4 * qt)
                return c0, slice(c0, 512), slice(qt * 512 + c0, (qt + 1) * 512), jb >= 4 * qt

            def pA(u, i):
                qt, jb, first, last = u
                c0, cols, qcols, diag = pgeom(u)
                es, es_b = es_p[i % PD]; Ls, Ls_b = Ls_p[i % PD]
                pz = pzs[i % 2]; zbuf = zbufs[i % 2]
                for hh in range(2):
                    hs = slice(hh * 64, (hh + 1) * 64)
                    mm(pz[:, hh, cols], kTp[hs, jb * 128:(jb + 1) * 128], qTp[hs, qcols], True, True, [k_b, q_b], [zbuf])
                act(es[:, :, cols], pz[:, :, cols], AF.Exp, [zbuf], [es_b], scale=0.125)
                if diag:
                    tt(es[:, :, c0:c0 + 128], es[:, :, c0:c0 + 128], maskS32[:, None, :].to_broadcast([128, 2, 128]), ALU.mult,
                       [es_b, cb], [es_b])
                act(Ls[:, :, cols], es[:, :, cols], AF.Ln, [es_b], [Ls_b], bias=1.0)

            def pB(u, i):
                qt, jb, first, last = u
                c0, cols, qcols, diag = pgeom(u)
                es, es_b = es_p[i % PD]; Ls, Ls_b = Ls_p[i % PD]; gs, gs_b = gs_p[i % 2]; ws, ws_b = ws_p[i % PD]
                pcc = pc[0]; pcb = cbuf[0]
                for hh in range(2):
                    mm(pcc[:, hh, cols], trib, Ls[:, hh, cols], True, first, [cb, Ls_b], [pcb])
                    if not first:
                        mm(pcc[:, hh, cols], onesb, la[:, hh, cols], False, True, [cb, la_b], [pcb])
                act(gs[:, :, cols], pcc[:, :, cols], AF.Exp, [pcb], [gs_b], scale=-1.0)
                tt(ws[:, :, cols], es[:, :, cols], gs[:, :, cols], ALU.mult, [es_b, gs_b], [ws_b])
                if not last:
                    if first:
                        P.op("dve", lambda e: e.memset(la[:], 0.0), writes=[la_b])
                    tt(la[:, :, cols], la[:, :, cols], Ls[:, :, cols], ALU.add, [la_b, Ls_b], [la_b])

            def pC(u, i):
                qt, jb, first, last = u
                c0, cols, qcols, diag = pgeom(u)
                ws, ws_b = ws_p[i % PD]
                for hh in range(2):
                    hs = slice(hh * 64, (hh + 1) * 64)
                    mm(ps[6 + hh][0:64, cols], vt[0][:, jb, hs], ws[:, hh, cols], first, last, [v_b, ws_b], [pb[6 + hh]], skip=True)
                    if last:
                        cp(yT[hs, ychunk, qt * 512:(qt + 1) * 512], ps[6 + hh][0:64, :], [pb[6 + hh]], [y_b],
                           eng=("act" if hh == 0 else "dve"))

            npu = len(punits)
            for i in range(npu + 4):
                if i < npu:
                    pA(punits[i], i)
                if 2 <= i < npu + 2:
                    pB(punits[i - 2], i - 2)
                if i >= 4:
                    pC(punits[i - 4], i - 4)
            P.end_replay()
            P.sb_off = mark
            return
        SK = 2
        DEP = 6
        w_sb = [(P.sb([128, 512], BF16), Buf()) for _ in range(DEP)]
        if kind == "sb":
            e_sb = [(P.sb([128, 512], F32), Buf()) for _ in range(DEP)]
            L_sb = [(P.sb([128, 512], BF16), Buf()) for _ in range(DEP)]
            g_sb = [(P.sb([128, 512], F32), Buf()) for _ in range(2)]
            Lacc = [(P.sb([128, 512], BF16), Buf()) for _ in range(2)]
        else:
            rden = P.sb([128, 512], F32); rd_b = Buf()
        units = []
        for qt in range(NT):
            nblk = 4 * qt + 4
            order = list(range(nblk - 1, -1, -1)) if kind == "sb" else list(range(nblk))
            for idx, jb in enumerate(order):
                for hh in range(2):
                    units.append((qt, hh, jb, idx == 0, idx == nblk - 1))

        def geom(u):
            qt, hh, jb, first, last = u
            m = max(0, jb - 4 * qt)
            c0 = 128 * m
            return (slice(hh * 64, (hh + 1) * 64), c0, slice(c0, 512), slice(qt * 512 + c0, (qt + 1) * 512), jb >= 4 * qt)

        junk_b = Buf()
        nfill = nfill_sb if kind == "sb" else nfill_fox
        jbank = 5 if kind == "sb" else 3

        def stageA(u, i):
            qt, hh, jb, first, last = u
            hs, c0, cols, qcols, diag = geom(u)
            zb = i % 3
            for _ in range(nfill):
                mm(ps[jbank][:], onesb, cstb[:, 0:512], True, True, [cb], [junk_b])
            mm(ps[zb][:, cols], kTp[hs, jb * 128:(jb + 1) * 128], qTp[hs, qcols], True, True, [k_b, q_b], [pb[zb]])
            if kind == "sb":
                es, es_b = e_sb[i % DEP]; Ls, Ls_b = L_sb[i % DEP]
                act(es[:, cols], ps[zb][:, cols], AF.Exp, [pb[zb]], [es_b], scale=0.125)
                if diag:
                    tt(es[:, c0:c0 + 128], es[:, c0:c0 + 128], maskS32, ALU.mult, [es_b, cb], [es_b])
                act(Ls[:, cols], es[:, cols], AF.Ln, [es_b], [Ls_b], bias=1.0)
            else:
                ws, ws_b = w_sb[i % DEP]
                act(ws[:, cols], ps[zb][:, cols], AF.Exp, [pb[zb]], [ws_b],
                    bias=btab[:, hidx + hh, qt, jb:jb + 1], scale=0.125)
                if diag:
                    tt(ws[:, c0:c0 + 128], ws[:, c0:c0 + 128], maskIb, ALU.mult, [ws_b, cb], [ws_b])

        def stageB(u, i):
            qt, hh, jb, first, last = u
            hs, c0, cols, qcols, diag = geom(u)
            accb = 6 + hh
            ws, ws_b = w_sb[i % DEP]
            if kind == "sb":
                es, es_b = e_sb[i % DEP]; Ls, Ls_b = L_sb[i % DEP]; gs, gs_b = g_sb[i % 2]
                la, la_b = Lacc[hh]
                cb_ = 3 + (i % 2)
                mm(ps[cb_][:, cols], trib, Ls[:, cols], True, first, [cb, Ls_b], [pb[cb_]])
                if not first:
                    mm(ps[cb_][:, cols], onesb, la[:, cols], False, True, [cb, la_b], [pb[cb_]])
                act(gs[:, cols], ps[cb_][:, cols], AF.Exp, [pb[cb_]], [gs_b], scale=-1.0)
                tt(ws[:, cols], es[:, cols], gs[:, cols], ALU.mult, [es_b, gs_b], [ws_b])
                if not last:
                    if first:
                        P.op("dve", lambda e, la=la: e.memset(la[:], 0.0), writes=[la_b])
                    tt(la[:, cols], la[:, cols], Ls[:, cols], ALU.add, [la_b, Ls_b], [la_b], eng="dve")

        def stageC(u, i):
            qt, hh, jb, first, last = u
            hs, c0, cols, qcols, diag = geom(u)
            accb = 6 + hh
            ws, ws_b = w_sb[i % DEP]
            if kind == "sb":
                mm(ps[accb][0:64, cols], vt[0][:, jb, hs], ws[:, cols], first, last, [v_b, ws_b], [pb[accb]], skip=True)
            else:
                mm(ps[accb][:, cols], vt[hh][:, jb, :], ws[:, cols], first, last, [v_b, ws_b], [pb[accb]], skip=True)
            if last:
                ysl = yT[hs, ychunk, qt * 512:(qt + 1) * 512]
                if kind == "sb":
                    cp(ysl, ps[accb][0:64, :], [pb[accb]], [y_b], eng="act")
                else:
                    recip(rden[0:64, :], ps[accb][64:128, :], [pb[accb]], [rd_b])
                    tt(ysl, ps[accb][0:64, :], rden[0:64, :], ALU.mult, [pb[accb], rd_b], [y_b])

        SK2 = 2 * SK
        nu = len(units)
        for i0 in range(0, nu + SK2, 2):
            for i in (i0, i0 + 1):
                if i < nu:
                    stageA(units[i], i)
            for i in (i0, i0 + 1):
                if kind == "sb" and SK <= i < nu + SK:
                    stageB(units[i - SK], i - SK)
            for i in (i0, i0 + 1):
                if SK2 <= i < nu + SK2:
                    stageC(units[i - SK2], i - SK2)
        P.end_replay()
        P.sb_off = mark

    for j in range(4):
        cast_expert(2 * j)
        cast_expert(2 * j + 1)
        attention_pair("sb", w_in_e_v, "w_in_e", 2048 + j * 128, 2560 + j * 128, 3072 + j * 128, 4 + j)
    P.barrier()

    if stop == "sb":
        finish(xT); P.emit(); return nc
    out_proj(yT, y_b, "w_out_e", xT, h1T)
    P.barrier()
    P.sb_off = persist_mark

    def ffn_phase(src, dst, gain, moe):
        mark = P.sb_off
        GW = GT * 512
        hnG = P.sb([128, 8, GW], BF16); hnG_b = Buf()
        aT = P.sb([128, NF, GW], BF16); a_b = Buf()
        acc = P.sb([128, 8, GW], F32); acc_b = Buf()
        wd = P.sb([128, NF, D], BF16); wd_b = Buf()
        wgu = [(P.sb([128, 2, 8, 128], BF16), Buf()) for _ in range(2)]
        sq_off = (P.sb_off + 63) // 64 * 64
        sq = (P.sb([128, 8, 512], BF16), Buf())
        rsb_ = (P.sb([128, 512], F32), Buf())
        s_sb = [(P.sb([128, 512], F32), Buf()) for _ in range(2)]
        t_sb = [(P.sb([128, 512], BF16), Buf()) for _ in range(2)]
        if moe:
            comb = P.sb([128, NE, GW], BF16); comb_b = Buf()
            hn32 = P.sb([128, 8, 512], F32); hn32_b = Buf()
            wr = P.sb([128, 8, 8], F32); wr_b = Buf()
            dma("sp", wr[:], wr_o.rearrange("(c p) e -> p c e", p=128), writes=[wr_b])
            lg = P.sb([128, 8], F32); lg2 = P.sb([128, 8], F32); eq1 = P.sb([128, 8], F32); eq2 = P.sb([128, 8], F32)
            cmb = P.sb([128, 8], F32)
            sm_ = P.sb([128, 8], F32)
            De = P.sb([128, 8, 128], F32, off=sq_off)
            r_b = Buf()
        if not moe:
            dma("sp", wd[:], wb["wd_e"].rearrange("(f p) n -> p f n", p=128), reads=[wbuf["wd_e"]], writes=[wd_b])
        wcount = 0
        for g in range(NG):
            tiles = list(range(g * GT, (g + 1) * GT))
            gsl = slice(g * GW, (g + 1) * GW)
            dma("sp", acc[:], view_T(src)[:, :, gsl], reads=[hb[id(src)]], writes=[acc_b])
            for n, t in enumerate(tiles):
                xs = acc[:, :, n * 512:(n + 1) * 512]
                act(sq[0][:], xs, AF.Square, [acc_b], [sq[1]])
                for c in range(8):
                    mm(ps[0][:], onesb, sq[0][:, c, :], c == 0, c == 7, [cb, sq[1]], [pb[0]])
                r, rb_ = rsb_
                act(r[:], ps[0][:], AF.Sqrt, [pb[0]], [rb_], bias=EPS, scale=1.0 / D)
                recip(r[:], r[:], [rb_], [rb_])
                for c in range(8):
                    stt(hnG[:, c, n * 512:(n + 1) * 512], xs[:, c, :], gain[:, c:c + 1], r[:], ALU.mult, ALU.mult,
                        [acc_b, rb_, gb], [hnG_b])
                if moe:
                    for c in range(8):
                        stt(hn32[:, c, :], xs[:, c, :], gain[:, c:c + 1], r[:], ALU.mult, ALU.mult, [acc_b, rb_, gb], [hn32_b])
                    for blk in range(4):
                        bs = slice(blk * 128, (blk + 1) * 128)
                        for c in range(8):
                            mm(ps[1][:, 0:8], hn32[:, c, bs], wr[:, c, :], c == 0, c == 7, [hn32_b, wr_b], [pb[1]])
                        cp(lg[:], ps[1][:, 0:8], [pb[1]], [r_b])
                        P.op("dve", lambda e: e.reduce_max(out=sm_[:, 0:1], in_=lg[:], axis=AX.X), reads=[r_b], writes=[r_b])
                        ts(eq1[:], lg[:], sm_[:, 0:1], ALU.is_equal, [r_b], [r_b])
                        stt(lg2[:], eq1[:], -1e30, lg[:], ALU.mult, ALU.add, [r_b], [r_b])
                        P.op("dve", lambda e: e.reduce_max(out=sm_[:, 1:2], in_=lg2[:], axis=AX.X), reads=[r_b], writes=[r_b])
                        ts(eq2[:], lg2[:], sm_[:, 1:2], ALU.is_equal, [r_b], [r_b])
                        tt(sm_[:, 2:3], sm_[:, 1:2], sm_[:, 0:1], ALU.subtract, [r_b], [r_b])
                        act(sm_[:, 3:4], sm_[:, 2:3], AF.Exp, [r_b], [r_b])
                        ts(sm_[:, 4:5], sm_[:, 3:4], 1.0, ALU.add, [r_b], [r_b])
                        recip(sm_[:, 4:5], sm_[:, 4:5], [r_b], [r_b])
                        tt(sm_[:, 5:6], sm_[:, 3:4], sm_[:, 4:5], ALU.mult, [r_b], [r_b])
                        ts(cmb[:], eq1[:], sm_[:, 4:5], ALU.mult, [r_b], [r_b])
                        stt(cmb[:], eq2[:], sm_[:, 5:6], cmb[:], ALU.mult, ALU.add, [r_b], [r_b])
                        for e_ in range(NE):
                            ts(De[:, e_, :], ident, cmb[:, e_:e_ + 1], ALU.mult, [r_b, cb], [sq[1]])
                        for half in range(2):
                            mm(ps[2 + half][:], ones32, De[:, half * 4:(half + 1) * 4, :].rearrange("p e t -> p (e t)"),
                               True, True, [cb, sq[1]], [pb[2 + half]])
                            cp(comb[:, half * 4:(half + 1) * 4, n * 512 + blk * 128:n * 512 + (blk + 1) * 128],
                               ps[2 + half][:].rearrange("p (e t) -> p e t", e=4), [pb[2 + half]], [comb_b], eng="act")
            for e_ in range(NE if moe else 1):
                if moe:
                    wgv = wb["wg_m"][e_].rearrange("(c p) n -> p c n", p=128)
                    wuv = wb["wu_m"][e_].rearrange("(c p) n -> p c n", p=128)
                    wgb, wub = wbuf["wg_m"][e_], wbuf["wu_m"][e_]
                else:
                    wgv = wb["wg_e"].rearrange("(c p) n -> p c n", p=128)
                    wuv = wb["wu_e"].rearrange("(c p) n -> p c n", p=128)
                    wgb, wub = wbuf["wg_e"], wbuf["wu_e"]
                for f in range(NF):
                    wt, wt_b = wgu[wcount % 2]
                    wcount += 1
                    dma("sp", wt[:, 0, :, :], wgv[:, :, f * 128:(f + 1) * 128], reads=[wgb], writes=[wt_b])
                    dma("sp", wt[:, 1, :, :], wuv[:, :, f * 128:(f + 1) * 128], reads=[wub], writes=[wt_b])
                    if moe and f == 3:
                        dma("sp", wd[:], wb["wd_m"][e_].rearrange("(f p) n -> p f n", p=128), reads=[wbuf["wd_m"][e_]], writes=[wd_b])
                    for n in range(GT):
                        ns = slice(n * 512, (n + 1) * 512)
                        gbk, ubk = 4 + (n % 2) * 2, 5 + (n % 2) * 2
                        for c in range(8):
                            mm(ps[gbk][:], wt[:, 0, c, :], hnG[:, c, ns], c == 0, c == 7, [wt_b, hnG_b], [pb[gbk]])
                        for c in range(8):
                            mm(ps[ubk][:], wt[:, 1, c, :], hnG[:, c, ns], c == 0, c == 7, [wt_b, hnG_b], [pb[ubk]])
                        ss_, ss_b = s_sb[n % 2]
                        act(ss_[:], ps[gbk][:], AF.Silu, [pb[gbk]], [ss_b])
                        if moe:
                            tq, tq_b = t_sb[n % 2]
                            tt(tq[:], ps[ubk][:], ss_[:], ALU.mult, [pb[ubk], ss_b], [tq_b])
                            tt(aT[:, f, ns], tq[:], comb[:, e_, ns], ALU.mult, [tq_b, comb_b], [a_b], eng="pool")
                        else:
                            tt(aT[:, f, ns], ps[ubk][:], ss_[:], ALU.mult, [pb[ubk], ss_b], [a_b])
                for m in range(8):
                    for n in range(GT):
                        ns = slice(n * 512, (n + 1) * 512)
                        bk = (m * GT + n) % 4
                        for f in range(NF):
                            mm(ps[bk][:], wd[:, f, m * 128:(m + 1) * 128], aT[:, f, ns], f == 0, f == NF - 1, [wd_b, a_b], [pb[bk]])
                        tt(acc[:, m, ns], ps[bk][:], acc[:, m, ns], ALU.add, [pb[bk], acc_b], [acc_b])
            dma("sp", view_T(dst)[:, :, gsl], acc[:], reads=[acc_b], writes=[hb[id(dst)]])
        P.sb_off = mark


    def ffn_dense(src, dst, gain):
        mark = P.sb_off
        GW = GT * 512
        hnG = P.sb([128, 8, GW], BF16); hnG_b = Buf()
        aT = P.sb([128, NF, GW], BF16); a_b = Buf()
        accs = [(P.sb([128, 8, GW], F32), Buf()) for _ in range(2)]
        wd = P.sb([128, NF, D], BF16); wd_b = Buf()
        wgu = [(P.sb([128, 2, 8, 128], BF16), Buf()) for _ in range(2)]
        sq = (P.sb([128, 8, 512], BF16), Buf())
        r = P.sb([128, 512], F32); rb_ = Buf()
        s_sb = [(P.sb([128, 512], F32), Buf()) for _ in range(2)]
        dma("sp", wd[:], wb["wd_e"].rearrange("(f p) n -> p f n", p=128), reads=[wbuf["wd_e"]], writes=[wd_b])
        wgv = wb["wg_e"].rearrange("(c p) n -> p c n", p=128)
        wuv = wb["wu_e"].rearrange("(c p) n -> p c n", p=128)

        def gsl(g):
            return slice(g * GW, (g + 1) * GW)

        def load_res(g):
            acc, acc_b = accs[g % 2]
            dma("sp", acc[:], view_T(src)[:, :, gsl(g)], reads=[hb[id(src)]], writes=[acc_b])

        def norm(g):
            acc, acc_b = accs[g % 2]
            for n in range(GT):
                xs = acc[:, :, n * 512:(n + 1) * 512]
                act(sq[0][:], xs, AF.Square, [acc_b], [sq[1]])
                for c in range(8):
                    mm(ps[0][:], onesb, sq[0][:, c, :], c == 0, c == 7, [cb, sq[1]], [pb[0]])
                act(r[:], ps[0][:], AF.Sqrt, [pb[0]], [rb_], bias=EPS, scale=1.0 / D)
                recip(r[:], r[:], [rb_], [rb_])
                for c in range(8):
                    stt(hnG[:, c, n * 512:(n + 1) * 512], xs[:, c, :], gain[:, c:c + 1], r[:], ALU.mult, ALU.mult,
                        [acc_b, rb_, gb], [hnG_b])

        def load_w(f):
            wt, wt_b = wgu[f % 2]
            dma("sp", wt[:, 0, :, :], wgv[:, :, f * 128:(f + 1) * 128], reads=[wbuf["wg_e"]], writes=[wt_b])
            dma("sp", wt[:, 1, :, :], wuv[:, :, f * 128:(f + 1) * 128], reads=[wbuf["wu_e"]], writes=[wt_b])

        def gate_up(g, preloaded):
            for f in range(NF):
                wt, wt_b = wgu[f % 2]
                if f >= preloaded:
                    load_w(f)
                for n in range(GT):
                    ns = slice(n * 512, (n + 1) * 512)
                    gbk, ubk = 4 + (n % 2) * 2, 5 + (n % 2) * 2
                    for c in range(8):
                        mm(ps[gbk][:], wt[:, 0, c, :], hnG[:, c, ns], c == 0, c == 7, [wt_b, hnG_b], [pb[gbk]])
                    for c in range(8):
                        mm(ps[ubk][:], wt[:, 1, c, :], hnG[:, c, ns], c == 0, c == 7, [wt_b, hnG_b], [pb[ubk]])
                    ss_, ss_b = s_sb[n % 2]
                    act(ss_[:], ps[gbk][:], AF.Silu, [pb[gbk]], [ss_b])
                    tt(aT[:, f, ns], ps[ubk][:], ss_[:], ALU.mult, [pb[ubk], ss_b], [a_b])

        def down(g):
            acc, acc_b = accs[g % 2]
            for m in range(8):
                for n in range(GT):
                    ns = slice(n * 512, (n + 1) * 512)
                    bk = 1 + (m * GT + n) % 3
                    for f in range(NF):
                        mm(ps[bk][:], wd[:, f, m * 128:(m + 1) * 128], aT[:, f, ns], f == 0, f == NF - 1, [wd_b, a_b], [pb[bk]])
                    tt(acc[:, m, ns], ps[bk][:], acc[:, m, ns], ALU.add, [pb[bk], acc_b], [acc_b])

        def store(g):
            acc, acc_b = accs[g % 2]
            dma("sp", view_T(dst)[:, :, gsl(g)], acc[:], reads=[acc_b], writes=[hb[id(dst)]])

        load_res(0)
        norm(0)
        pre = 0
        for g in range(NG):
            if g + 1 < NG:
                load_res(g + 1)
            gate_up(g, pre)
            pre = 0
            if g + 1 < NG:
                load_w(0)
                load_w(1)
                pre = 2
                norm(g + 1)
            down(g)
            store(g)
        P.sb_off = mark

    if stop == "mix0":
        finish(h1T); P.emit(); return nc

    ffn_dense(h1T, h2T, gsb["fn_e"])
    P.barrier()
    if stop == "ffn0":
        finish(h2T); P.emit(); return nc

    hnT = P.sb([128, 8, S], BF16)
    hn_b = Buf()
    yT = P.sb([128, 8, S], BF16)
    y_b = Buf()
    btab = P.sb([128, 16, NT, NB], F32)
    bt_b = Buf()
    l1_mark = P.sb_off
    xbufs = [(P.sb([128, 8, 512], F32), Buf()) for _ in range(2)]
    sqb = [(P.sb([128, 8, 512], BF16), Buf()) for _ in range(2)]
    rsb = [(P.sb([128, 512], F32), Buf()) for _ in range(1)]
    norm_tiles(h2T, hb[id(h2T)], gsb["an_o"], range(NT), lambda t: (hnT[:, :, t * 512:(t + 1) * 512], hn_b), 0, xbufs, sqb, rsb)
    P.sb_off = l1_mark
    P.barrier()
    w_in_o_v = wb["w_in_o"].rearrange("(c p) n -> p c n", p=128)

    def forget_tables():
        mark = P.sb_off
        wf = P.sb([128, 8, 16], BF16); wf_b = Buf()
        dma("sp", wf[:], w_in_o_v[:, :, 3072:3088], reads=[wbuf["w_in_o"]], writes=[wf_b])
        bfs = P.sb([16, 2], F32); bfs_b = Buf()
        dma("sp", bfs[:, 0:1], bf_o, writes=[bfs_b])
        ts(bfs[:, 1:2], bfs[:, 0:1], -1.0, ALU.mult, [bfs_b], [bfs_b])
        lf = P.sb([16, S], F32); lf_b = Buf()
        cum = P.sb([16, S], F32); cum_b = Buf()
        one16 = P.sb([16, 512], F32); o16_b = Buf()
        P.op("dve", lambda e: e.memset(one16[:], 1.0), writes=[o16_b])
        for t in range(NT):
            ts_ = slice(t * 512, (t + 1) * 512)
            for c in range(8):
                mm(ps[0][0:16, :], wf[:, c, :], hnT[:, c, ts_], c == 0, c == 7, [wf_b, hn_b], [pb[0]])
            act(lf[:, ts_], ps[0][0:16, :], AF.Exp, [pb[0], bfs_b], [lf_b], bias=bfs[:, 1:2], scale=-1.0)
            act(lf[:, ts_], lf[:, ts_], AF.Ln, [lf_b], [lf_b], bias=1.0)
            init = 0.0 if t == 0 else cum[:, t * 512 - 1:t * 512]
            P.op("dve", lambda e, ts_=ts_, init=init: e.tensor_tensor_scan(out=cum[:, ts_], data0=one16[:], data1=lf[:, ts_],
                                                                           initial=init, op0=ALU.mult, op1=ALU.add),
                 reads=[lf_b, o16_b, cum_b], writes=[cum_b])
        ck = P.sb([128, NB, 16], F32); ck_b = Buf()
        for jb in range(NB):
            P.op("pe", lambda e, jb=jb: e.transpose(ps[1][:, jb * 16:(jb + 1) * 16], cum[:, jb * 128:(jb + 1) * 128], ident[0:16, 0:16]),
                 reads=[cum_b, cb], writes=[pb[1]])
        cp(ck[:].rearrange("p n h -> p (n h)"), ps[1][:, 0:NB * 16], [pb[1]], [ck_b])
        R = P.sb([16, 16, NT], F32); R_b = Buf()
        cmid = cum[:].rearrange("h (t s) -> h t s", s=512)[:, :, 255]
        for h in range(16):
            ts(R[:, h, :], cmid, ident[0:16, h:h + 1], ALU.mult, [cum_b, cb], [R_b])
        mm(ps[2][:, 0:16 * NT], ones32[0:16, :], R[:].rearrange("k h t -> k (h t)"), True, True, [cb, R_b], [pb[2]])
        cq = P.sb([128, 16, NT], F32); cq_b = Buf()
        cp(cq[:].rearrange("p h t -> p (h t)"), ps[2][:, 0:16 * NT], [pb[2]], [cq_b])
        for h in range(16):
            for t in range(NT):
                ts(btab[:, h, t, :], ck[:, :, h], cq[:, h, t:t + 1], ALU.subtract, [ck_b, cq_b], [bt_b])
        P.sb_off = mark

    forget_tables()
    P.barrier()
    for j in range(8):
        attention_pair("fox", w_in_o_v, "w_in_o", j * 128, 1024 + j * 128, 2048 + j * 128, j, btab=btab, hidx=2 * j)
    P.barrier()
    out_proj(yT, y_b, "w_out_o", h2T, h3T)
    P.barrier()
    P.sb_off = persist_mark
    if stop == "mix1":
        finish(h3T); P.emit(); return nc


    def moe_sparse():
        I32 = mybir.dt.int32
        gain = gsb["fn_o"]
        mark0 = P.sb_off
        eq1A = P.sb([128, NB, 8], F32); eq2A = P.sb([128, NB, 8], F32); rankA = P.sb([128, NB, 8], F32)
        gA = P.sb([128, NB, 2], F32)
        run = P.sb([128, 8], F32)
        rt_b = Buf()
        posF = P.sb([128, 2, NB], F32); posI = P.sb([128, 2, NB], I32); pos_b = Buf()
        wiF = P.sb([128, NS], F32); wiI = P.sb([128, NS], I32); wi_b = Buf()
        iot = P.sb([128, 22], F32); iot_b = Buf()
        dma("sp", iot[:], iot_d, writes=[iot_b])
        P.op("dve", lambda e: e.memset(run[:], 0.0), writes=[rt_b])
        hn_db = Buf(); h3_db = Buf(); xs_db = Buf(); ys_db = Buf()
        mark1 = P.sb_off
        accs1 = [(P.sb([128, 8, 512], F32), Buf()) for _ in range(2)]
        sq = (P.sb([128, 8, 512], BF16), Buf())
        r = P.sb([128, 512], F32); r_b2 = Buf()
        hnb = P.sb([128, 8, 512], BF16); hnb_b = Buf()
        hn32 = P.sb([128, 8, 512], F32); hn32_b = Buf()
        wr = P.sb([128, 8, 8], F32); wr_b = Buf()
        dma("sp", wr[:], wr_o.rearrange("(c p) e -> p c e", p=128), writes=[wr_b])
        tokb = [(P.sb([128, D], BF16), Buf()) for _ in range(2)]
        tok32 = [(P.sb([128, D], F32), Buf()) for _ in range(2)]
        smalls = [(P.sb([128, 8], F32), P.sb([128, 8], F32), P.sb([128, 8], F32), P.sb([128, 8], F32), Buf()) for _ in range(2)]
        lg, lg2, msk, sm_, r_b = smalls[0]
        def load_acc1(t):
            a_, ab_ = accs1[t % 2]
            dma("sp", a_[:], view_T(h3T)[:, :, t * 512:(t + 1) * 512], reads=[hb[id(h3T)]], writes=[ab_])

        load_acc1(0)
        for t in range(NT):
            acc, acc_b = accs1[t % 2]
            if t + 1 < NT:
                load_acc1(t + 1)
            act(sq[0][:], acc[:], AF.Square, [acc_b], [sq[1]])
            for c in range(8):
                mm(ps[0][:], onesb, sq[0][:, c, :], c == 0, c == 7, [cb, sq[1]], [pb[0]])
            act(r[:], ps[0][:], AF.Sqrt, [pb[0]], [r_b2], bias=EPS, scale=1.0 / D)
            recip(r[:], r[:], [r_b2], [r_b2])
            for c in range(8):
                stt(hn32[:, c, :], acc[:, c, :], gain[:, c:c + 1], r[:], ALU.mult, ALU.mult, [acc_b, r_b2, gb], [hn32_b])
            cp(hnb[:], hn32[:], [hn32_b], [hnb_b], eng="act")
            for blk in range(4):
                b = t * 4 + blk
                bs = slice(blk * 128, (blk + 1) * 128)
                lg, lg2, msk, sm_, r_b = smalls[b % 2]
                for c in range(8):
                    mm(ps[1][:, 0:8], hn32[:, c, bs], wr[:, c, :], c == 0, c == 7, [hn32_b, wr_b], [pb[1]])
                cp(lg[:], ps[1][:, 0:8], [pb[1]], [r_b])
                P.op("dve", lambda e, sm_=sm_, lg=lg: e.reduce_max(out=sm_[:, 0:1], in_=lg[:], axis=AX.X), reads=[r_b], writes=[r_b])
                ts(eq1A[:, b, :], lg[:], sm_[:, 0:1], ALU.is_equal, [r_b], [rt_b])
                stt(lg2[:], eq1A[:, b, :], -1e30, lg[:], ALU.mult, ALU.add, [r_b, rt_b], [r_b])
                P.op("dve", lambda e, sm_=sm_, lg2=lg2: e.reduce_max(out=sm_[:, 1:2], in_=lg2[:], axis=AX.X), reads=[r_b], writes=[r_b])
                ts(eq2A[:, b, :], lg2[:], sm_[:, 1:2], ALU.is_equal, [r_b], [rt_b])
                tt(sm_[:, 2:3], sm_[:, 1:2], sm_[:, 0:1], ALU.subtract, [r_b], [r_b])
                act(sm_[:, 3:4], sm_[:, 2:3], AF.Exp, [r_b], [r_b])
                ts(sm_[:, 4:5], sm_[:, 3:4], 1.0, ALU.add, [r_b], [r_b])
                recip(gA[:, b, 0:1], sm_[:, 4:5], [r_b], [rt_b])
                tt(gA[:, b, 1:2], sm_[:, 3:4], gA[:, b, 0:1], ALU.mult, [r_b, rt_b], [rt_b])
                tt(msk[:], eq1A[:, b, :], eq2A[:, b, :], ALU.add, [rt_b], [r_b])
                mm(ps[2][:, 0:8], maskS32, msk[:], True, True, [cb, r_b], [pb[2]])
                mm(ps[2][:, 8:16], ones32, msk[:], True, True, [cb, r_b], [pb[2]], skip=True)
                tt(rankA[:, b, :], ps[2][:, 0:8], run[:], ALU.add, [pb[2], rt_b], [rt_b])
                tt(run[:], ps[2][:, 8:16], run[:], ALU.add, [pb[2], rt_b], [rt_b])
                tb_, tb_b = tokb[b % 2]
                t32, t32_b = tok32[b % 2]
                p3 = ps[3][:].bitcast(BF16)
                for c in range(8):
                    P.op("pe", lambda e, c=c, bs=bs: e.transpose(p3[:, c * 128:(c + 1) * 128], hnb[:, c, bs], identb),
                         reads=[hnb_b, cb], writes=[pb[3]])
                cp(tb_[:], p3[:, 0:D], [pb[3]], [tb_b], eng="act")
                dma("sp", Hn_d[b * 128:(b + 1) * 128, :], tb_[:], reads=[tb_b], writes=[hn_db])
                for c in range(8):
                    bk = 4 + c // 4
                    P.op("pe", lambda e, c=c, bs=bs, bk=bk, acc=acc: e.transpose(ps[bk][:, (c % 4) * 128:(c % 4 + 1) * 128], acc[:, c, bs], ident),
                         reads=[acc_b, cb], writes=[pb[bk]])
                cp(t32[:, 0:512], ps[4][:], [pb[4]], [t32_b], eng="act")
                cp(t32[:, 512:1024], ps[5][:], [pb[5]], [t32_b], eng="dve")
                dma("sp", H3_d[b * 128:(b + 1) * 128, :], t32[:], reads=[t32_b], writes=[h3_db])
        til = P.sb([128, 8], F32); offe = P.sb([128, 8], F32); off0 = P.sb([128, 8], F32)
        tmpA = P.sb([128, NB, 8], F32)
        es = P.sb([128, NS], F32); es2 = P.sb([128, NS], F32); tmp8 = P.sb([128, 8], F32)
        P.op("dve", lambda e: e.memset(til[:], 0.0), writes=[r_b])
        for k in range(NT * 2 + 1):
            stt(til[:], run[:], 512.0 * k, til[:], ALU.is_gt, ALU.add, [rt_b, r_b], [r_b])
        cp(offe[:, 0:1], til[:, 0:1], [r_b], [r_b])
        for e_ in range(1, 8):
            tt(offe[:, e_:e_ + 1], offe[:, e_ - 1:e_], til[:, e_:e_ + 1], ALU.add, [r_b], [r_b])
        ts(offe[:], offe[:], 512.0, ALU.mult, [r_b], [r_b])
        stt(off0[:], til[:], -512.0, offe[:], ALU.mult, ALU.add, [r_b], [r_b])
        for k_, eqA in enumerate((eq1A, eq2A)):
            tt(tmpA[:], rankA[:], off0[:, None, :].to_broadcast([128, NB, 8]), ALU.add, [rt_b, r_b], [r_b])
            tt(tmpA[:], tmpA[:], eqA[:], ALU.mult, [r_b, rt_b], [r_b])
            P.op("dve", lambda e, k_=k_: e.reduce_sum(out=posF[:, k_, :], in_=tmpA[:], axis=AX.X), reads=[r_b], writes=[pos_b])
        cp(posI[:], posF[:], [pos_b], [pos_b])
        for s_ in range(NS):
            ts(tmp8[:], offe[:], 512.0 * s_, ALU.is_le, [r_b], [r_b])
            P.op("dve", lambda e, s_=s_: e.reduce_sum(out=es[:, s_:s_ + 1], in_=tmp8[:], axis=AX.X), reads=[r_b], writes=[r_b])
        usedF = P.sb([128, NS], F32); usedI = P.sb([128, NS], I32)
        for s_ in range(NS):
            ts(usedF[:, s_:s_ + 1], offe[:, 7:8], 512.0 * s_, ALU.is_gt, [r_b], [pos_b])
        if DEBUG_ZERO_FLAGS:
            P.op("dve", lambda e: e.memset(usedF[:], 0.0), writes=[pos_b])
        cp(usedI[:], usedF[:], [pos_b], [pos_b])
        ts(es[:], es[:], 7.0, ALU.min, [r_b], [r_b])
        stt(wiF[:], es[:], 128.0, iot[:, 0:1].to_broadcast([128, NS]), ALU.mult, ALU.add, [iot_b, r_b], [wi_b])
        cp(wiI[:], wiF[:], [wi_b], [wi_b])
        for b in range(NB):
            tb_, tb_b = tokb[b % 2]
            dma("sp", tb_[:], Hn_d[b * 128:(b + 1) * 128, :], reads=[hn_db], writes=[tb_b])
            for k_ in range(2):
                P.op("pool", lambda e, tb_=tb_, k_=k_, b=b: e.indirect_dma_start(
                    out=Xs_d, out_offset=bass.IndirectOffsetOnAxis(ap=posI[:, k_, b:b + 1], axis=0), in_=tb_[:], in_offset=None),
                    reads=[tb_b, pos_b], writes=[xs_db], dma=True)
        P.barrier()
        P.sb_off = mark1
        NH = NF // 2
        wgu = [(P.sb([128, 2, 8, HF], BF16), Buf()) for _ in range(2)]
        wd = P.sb([128, NF, D], BF16); wd_b = Buf()
        aT = P.sb([128, NF, 512], BF16); a_b = Buf()
        xtok = P.sb([128, 4, D], BF16); xtok_b = Buf()
        xgT = P.sb([128, 8, 512], BF16); xg_b = Buf()
        yst = [(P.sb([128, D], F32), Buf()) for _ in range(2)]
        s_sb = [(P.sb([128, 512], F32), Buf()) for _ in range(2)]
        wg2 = [wb["wg_m"][h] for h in range(2)]
        wu2 = [wb["wu_m"][h] for h in range(2)]
        wd2 = [wb["wd_m"][h] for h in range(2)]
        allw = wbuf["wg_m"] + wbuf["wu_m"] + wbuf["wd_m"]

        def gather(out, src, idx_ap, reads, writes):
            P.op("pool", lambda e: e.indirect_dma_start(out=out, out_offset=None, in_=src,
                                                        in_offset=bass.IndirectOffsetOnAxis(ap=idx_ap, axis=0)),
                 reads=reads, writes=writes, dma=True)

        def load_gu(s_, h):
            wt, wt_b = wgu[h]
            gather(wt[:, 0, :, :].rearrange("p c n -> p (c n)"), wg2[h], wiI[:, s_:s_ + 1], [wi_b] + allw, [wt_b])
            gather(wt[:, 1, :, :].rearrange("p c n -> p (c n)"), wu2[h], wiI[:, s_:s_ + 1], [wi_b] + allw, [wt_b])

        def load_d(s_):
            for h in range(2):
                gather(wd[:, h * NH:(h + 1) * NH, :].rearrange("p f n -> p (f n)"), wd2[h], wiI[:, s_:s_ + 1], [wi_b] + allw, [wd_b])

        load_gu(0, 0)
        load_gu(0, 1)
        it_y = 0
        def load_x(s_):
            dma("sp", xtok[:], Xs_d[s_ * 512:(s_ + 1) * 512, :].rearrange("(j p) d -> p j d", p=128), reads=[xs_db], writes=[xtok_b])

        load_x(0)
        for s_ in range(NS):
            if SKIP_SLOTS and s_ >= (2 * S) // 512:
                P.cur_grp = s_
                P.grp_flag[s_] = usedI[0:1, s_:s_ + 1]
            p0 = ps[0][:].bitcast(BF16)
            for c in range(8):
                for j in range(4):
                    P.op("pe", lambda e, c=c, j=j: e.transpose(p0[:, j * 128:(j + 1) * 128], xtok[:, j, c * 128:(c + 1) * 128], identb),
                         reads=[xtok_b, cb], writes=[pb[0]])
                cp(xgT[:, c, :], p0[:, 0:512], [pb[0]], [xg_b], eng=("act" if c % 2 == 0 else "dve"))
            if s_ + 1 < NS:
                load_x(s_ + 1)
            load_d(s_)
            for h in range(2):
                wt, wt_b = wgu[h]
                for fl in range(NH):
                    f = h * NH + fl
                    gbk, ubk = 4 + (f % 2) * 2, 5 + (f % 2) * 2
                    for c in range(8):
                        mm(ps[gbk][:], wt[:, 0, c, fl * 128:(fl + 1) * 128], xgT[:, c, :], c == 0, c == 7, [wt_b, xg_b], [pb[gbk]])
                    for c in range(8):
                        mm(ps[ubk][:], wt[:, 1, c, fl * 128:(fl + 1) * 128], xgT[:, c, :], c == 0, c == 7, [wt_b, xg_b], [pb[ubk]])
                    ss_, ss_b = s_sb[f % 2]
                    act(ss_[:], ps[gbk][:], AF.Silu, [pb[gbk]], [ss_b])
                    tt(aT[:, f, :], ps[ubk][:], ss_[:], ALU.mult, [pb[ubk], ss_b], [a_b])
                if s_ + 1 < NS:
                    load_gu(s_ + 1, h)
            for j in range(4):
                ys_, ys_b = yst[it_y % 2]
                it_y += 1
                for dh in range(2):
                    bk = (j * 2 + dh) % 4
                    for f in range(NF):
                        mm(ps[bk][:], aT[:, f, j * 128:(j + 1) * 128], wd[:, f, dh * 512:(dh + 1) * 512], f == 0, f == NF - 1,
                           [a_b, wd_b], [pb[bk]])
                    cp(ys_[:, dh * 512:(dh + 1) * 512], ps[bk][:], [pb[bk]], [ys_b], eng=("act" if dh == 0 else "dve"))
                dma("sp", Ys_d[s_ * 512 + j * 128:s_ * 512 + (j + 1) * 128, :], ys_[:], reads=[ys_b], writes=[ys_db])
        P.cur_grp = None
        P.barrier()
        P.sb_off = mark1
        finbc = P.sb([128, D], F32); fin_b = Buf()
        dma("sp", finbc[:], fin_row.partition_broadcast(128), writes=[fin_b])
        bufs4 = [tuple((P.sb([128, D], F32), Buf()) for _ in range(4)) for _ in range(2)]
        st4 = [(P.sb([128, 4], F32), Buf()) for _ in range(2)]
        out_b = Buf()
        def load4(b):
            (y1, y1_b), (y2, y2_b), (h3, h3_b), (o, o_b) = bufs4[b % 2]
            gather(y1[:], Ys_d, posI[:, 0, b:b + 1], [pos_b, ys_db], [y1_b])
            gather(y2[:], Ys_d, posI[:, 1, b:b + 1], [pos_b, ys_db], [y2_b])
            dma("sp", h3[:], H3_d[b * 128:(b + 1) * 128, :], reads=[h3_db], writes=[h3_b])

        load4(0)
        for b in range(NB):
            (y1, y1_b), (y2, y2_b), (h3, h3_b), (o, o_b) = bufs4[b % 2]
            st, st_b = st4[b % 2]
            if b + 1 < NB:
                load4(b + 1)
            stt(h3[:], y1[:], gA[:, b, 0:1], h3[:], ALU.mult, ALU.add, [y1_b, h3_b, rt_b], [h3_b])
            stt(h3[:], y2[:], gA[:, b, 1:2], h3[:], ALU.mult, ALU.add, [y2_b, h3_b, rt_b], [h3_b])
            P.op("act", lambda e, o=o, h3=h3, st=st: e.activation(out=o[:], in_=h3[:], func=AF.Square, accum_out=st[:, 0:1]),
                 reads=[h3_b], writes=[o_b, st_b])
            act(st[:, 1:2], st[:, 0:1], AF.Sqrt, [st_b], [st_b], bias=EPS, scale=1.0 / D)
            recip(st[:, 2:3], st[:, 1:2], [st_b], [st_b])
            stt(o[:], h3[:], st[:, 2:3], finbc[:], ALU.mult, ALU.mult, [h3_b, st_b, fin_b, o_b], [o_b])
            dma("sp", out_nat[b * 128:(b + 1) * 128, :], o[:], reads=[o_b], writes=[out_b], is_out=True)
        P.sb_off = mark0

    if sparse and stop is None:
        moe_sparse()
        P.emit()
        return nc

    ffn_phase(h3T, h4T, gsb["fn_o"], moe=True)
    P.barrier()
    if stop == "moe":
        finish(h4T); P.emit(); return nc

    mark = P.sb_off
    xbufs = [(P.sb([128, 8, 512], F32), Buf()) for _ in range(2)]
    obufs = [(P.sb([128, 8, 512], F32), Buf()) for _ in range(2)]
    sqb = (P.sb([128, 8, 512], BF16), Buf())
    rsb = (P.sb([128, 512], F32), Buf())
    norm_tiles(h4T, hb[id(h4T)], gsb["fin"], range(NT), lambda t: (obufs[t % 2][0], obufs[t % 2][1]), 0, xbufs, sqb, rsb,
               keep32=lambda t, xs, xs_b, r, r_b: dma("sp", view_T(outT)[:, :, t * 512:(t + 1) * 512], obufs[t % 2][0][:],
                                                      reads=[obufs[t % 2][1]], writes=[hb[id(outT)]], is_out=True))
    P.sb_off = mark
    P.emit()
    return nc


def make_in_maps(inputs, S, ncores):
    x = np.asarray(inputs["x"], np.float32)
    def g8(v):
        return np.ascontiguousarray(np.asarray(v, np.float32).reshape(-1, 128).T)
    common = {
        "an_e": g8(inputs["attn_norm_even"][0]), "fn_e": g8(inputs["ffn_norm_even"][0]),
        "an_o": g8(inputs["attn_norm_odd"][0]), "fn_o": g8(inputs["ffn_norm_odd"][0]),
        "fin": g8(inputs["final_norm"]), "rn_e": g8(inputs["ret_norm_even"][0]),
        "fin_row": np.ascontiguousarray(np.asarray(inputs["final_norm"], np.float32).reshape(1, -1)),
        "bf_o": np.ascontiguousarray(np.asarray(inputs["b_forget_odd"][0], np.float32).reshape(16, 1)),
        "wr_o": np.ascontiguousarray(inputs["w_router_odd"][0], dtype=np.float32),
        "w_in_e": np.ascontiguousarray(inputs["w_in_even"][0], dtype=np.float32),
        "w_out_e": np.ascontiguousarray(inputs["w_out_even"][0], dtype=np.float32),
        "wg_e": np.ascontiguousarray(inputs["w_gate_even"][0], dtype=np.float32),
        "wu_e": np.ascontiguousarray(inputs["w_up_even"][0], dtype=np.float32),
        "wd_e": np.ascontiguousarray(inputs["w_down_even"][0], dtype=np.float32),
        "w_in_o": np.ascontiguousarray(inputs["w_in_odd"][0], dtype=np.float32),
        "w_out_o": np.ascontiguousarray(inputs["w_out_odd"][0], dtype=np.float32),
        "wg_m": np.ascontiguousarray(inputs["w_gate_moe_odd"][0], dtype=np.float32),
        "wu_m": np.ascontiguousarray(inputs["w_up_moe_odd"][0], dtype=np.float32),
        "wd_m": np.ascontiguousarray(inputs["w_down_moe_odd"][0], dtype=np.float32),
    }
    common.update(host_consts(S))
    maps = []
    for b in range(ncores):
        m = dict(common)
        m["xT"] = np.ascontiguousarray(x[b, :S].T)
        maps.append(m)
    return maps


def kernel(**inputs):
    S = 4096
    nc = build(S)
    maps = make_in_maps(inputs, S, 8)
    res = run_bass_kernel_spmd(nc, maps, core_ids=list(range(8)))
    out = np.stack([np.asarray(r["out"]) for r in res.results], axis=0)
    return out.astype(np.float32)
```

```python
import numpy as np
import concourse.bass as bass
import concourse.mybir as mybir
from concourse.bass_utils import run_bass_kernel_spmd

F32 = mybir.dt.float32
BF16 = mybir.dt.bfloat16
AF = mybir.ActivationFunctionType
ALU = mybir.AluOpType
AX = mybir.AxisListType

NDSEM = {"sp": 12, "pool": 28, "act": 1, "dve": 1, "pe": 1}


FUSE_WAIT = True


class Buf:
    __slots__ = ("w", "r")

    def __init__(self):
        self.w = None
        self.r = []


class Prog:
    ENGS = ("pe", "act", "dve", "pool", "sp")

    def __init__(self, nc):
        self.nc = nc
        self.lists = {e: [] for e in self.ENGS}
        self.sb_n = 0
        arena_bytes = nc.sbuf_bytes_remaining - 6144
        arena = nc.alloc_sbuf_tensor("arena", [128, arena_bytes], mybir.dt.uint8)
        self.sb_base = nc.lookup_mloc(arena).addr
        self.sb_off = self.sb_base
        self.sb_top = self.sb_base + arena_bytes
        self.out_events = []
        self.cur_grp = None
        self.replay = None
        self.replay_store = {}
        self.grp_flag = {}

    def begin_replay(self, key):
        first = key not in self.replay_store
        if first:
            self.replay_store[key] = []
        self.replay = [self.replay_store[key], 0, first]
        return first

    def end_replay(self):
        self.replay = None

    def _replayed(self, make):
        if self.replay is None:
            return make()
        store, idx, first = self.replay
        if first:
            obj = make()
            store.append(obj)
        else:
            obj = store[idx]
        self.replay[1] = idx + 1
        return obj

    def buf(self):
        return self._replayed(Buf)

    def sb(self, shape, dtype, off=None, name=None):
        return self._replayed(lambda: self._sb(shape, dtype, off, name))

    def _sb(self, shape, dtype, off=None, name=None):
        esz = 2 if dtype == BF16 else 4
        n = 1
        for s in shape[1:]:
            n *= s
        nbytes = n * esz
        if off is None:
            off = (self.sb_off + 63) // 64 * 64
            self.sb_off = off + nbytes
        assert off >= self.sb_base and off + nbytes <= self.sb_top, (off, nbytes, self.sb_top)
        self.sb_n += 1
        t = self.nc.alloc_sbuf_tensor_at(name or f"sb{self.sb_n}", list(shape), dtype, offset=off)
        return t

    def op(self, eng, fn, reads=(), writes=(), dma=False, is_out=False):
        lst = self.lists[eng]
        idx = len(lst)
        ev = ("dma", eng, idx) if dma else ("c", eng, idx)
        deps = set()
        for b in reads:
            if b.w is not None:
                deps.add(b.w)
        for b in writes:
            if b.w is not None:
                deps.add(b.w)
            for r in b.r:
                deps.add(r)
        if not dma:
            war_only = set()
            for b in writes:
                for r in b.r:
                    if r[0] == "c" and r[1] == eng:
                        war_only.add(r)
            for b in reads:
                if b.w in war_only:
                    war_only.discard(b.w)
            for b in writes:
                if b.w in war_only:
                    war_only.discard(b.w)
            if eng == "pe":
                deps -= war_only
            if eng == "pe":
                deps = {d for d in deps if not (d[0] == "c" and d[1] == "pe")}
        deps.discard(ev)
        lst.append({"fn": fn, "deps": deps, "dma": dma, "marked": False, "grp": self.cur_grp})
        for b in reads:
            b.r.append(ev)
        for b in writes:
            b.w = ev
            b.r = []
        if is_out:
            self.out_events.append(ev)
        return ev

    def barrier(self):
        evs = set()
        for e in self.ENGS:
            lst = self.lists[e]
            last_c = None
            for i in range(len(lst) - 1, -1, -1):
                if not lst[i]["dma"] and lst[i]["fn"] is not None:
                    last_c = ("c", e, i)
                    break
            if last_c:
                evs.add(last_c)
            for i, r in enumerate(lst):
                if r["dma"] and not r.get("barriered"):
                    evs.add(("dma", e, i))
                    r["barriered"] = True
        for e in self.ENGS:
            self.lists[e].append({"fn": None, "deps": {d for d in evs if not (d[0] == "c" and d[1] == e)},
                                  "dma": False, "marked": False})

    def emit(self):
        nc = self.nc
        lists = self.lists
        for e in self.ENGS:
            seen_c = {}
            seen_d = set()
            for rec in lists[e]:
                best = {}
                dd = set()
                for d in rec["deps"]:
                    if d[0] == "c":
                        if d[2] > best.get(d[1], -1):
                            best[d[1]] = d[2]
                    else:
                        dd.add(d)
                waits = []
                for e2, i2 in best.items():
                    if seen_c.get(e2, -1) >= i2:
                        continue
                    seen_c[e2] = i2
                    waits.append(("c", e2, i2))
                    lists[e2][i2]["marked"] = True
                for d in dd:
                    if d in seen_d:
                        continue
                    seen_d.add(d)
                    waits.append(d)
                rec["waits"] = waits
        fin = []
        for d in self.out_events:
            fin.append(d)
        csem = {e: nc.alloc_semaphore(f"c_{e}") for e in self.ENGS}
        dsem = {e: [nc.alloc_semaphore(f"d_{e}{k}") for k in range(NDSEM[e])] for e in self.ENGS}
        for e in self.ENGS:
            cnt = 0
            nd = 0
            tot = [0] * NDSEM[e]
            for rec in lists[e]:
                if rec["dma"]:
                    slot = nd % NDSEM[e]
                    rec["slot"] = slot
                    rec["prev"] = tot[slot]
                    tot[slot] += 16
                    rec["val"] = tot[slot]
                    nd += 1
                elif rec["marked"]:
                    cnt += 1
                    rec["val"] = cnt
        engobj = {"pe": "tensor", "act": "scalar", "dve": "vector", "pool": "gpsimd", "sp": "sync"}

        def emit_eng(e, eng):
            lst = lists[e]
            reg = None
            n = len(lst)
            k = 0
            while k < n:
                g = lst[k].get("grp")
                k2 = k
                while k2 < n and lst[k2].get("grp") == g:
                    k2 += 1
                seg = lst[k:k2]
                if g is None:
                    for rec in seg:
                        emit_rec(e, eng, rec)
                else:
                    if reg is None:
                        reg = eng.alloc_register(f"flag_{e}")
                    eng.reg_load(reg, self.grp_flag[g])
                    gd = eng.If_ne(reg, 0)
                    gd.__enter__()
                    for rec in seg:
                        emit_rec(e, eng, rec)
                    gd.__exit__(None, None, None)
                    ncomp = sum(1 for rec in seg if (not rec["dma"]) and rec["marked"] and rec["fn"] is not None)
                    dcomp = {}
                    for rec in seg:
                        if rec["dma"] and rec["fn"] is not None:
                            dcomp[rec["slot"]] = dcomp.get(rec["slot"], 0) + 16
                    if ncomp or dcomp:
                        ge = eng.Else()
                        ge.__enter__()
                        if ncomp:
                            eng.sem_inc(csem[e], ncomp)
                        for sl, v in dcomp.items():
                            eng.sem_inc(dsem[e][sl], v)
                        ge.__exit__(None, None, None)
                k = k2
            if e == "sp":
                for w in fin:
                    r2 = lists[w[1]][w[2]]
                    eng.wait_ge(dsem[w[1]][r2["slot"]], r2["val"])

        def emit_rec(e, eng, rec):
            if True:
                wl = []
                for w in rec["waits"]:
                    r2 = lists[w[1]][w[2]]
                    if w[0] == "c":
                        wl.append((csem[w[1]], r2["val"]))
                    else:
                        wl.append((dsem[w[1]][r2["slot"]], r2["val"]))
                if rec["dma"] and rec["fn"] is not None and rec["prev"] > 0:
                    wl.append((dsem[e][rec["slot"]], rec["prev"]))
                fuse = None
                if FUSE_WAIT and rec["fn"] is not None and not rec["dma"] and wl:
                    fuse = wl.pop()
                for sm_, v_ in wl:
                    eng.wait_ge(sm_, v_)
                if rec["fn"] is None:
                    return
                ins = rec["fn"](eng)
                if fuse is not None:
                    ins._wait_ge(fuse[0], fuse[1])
                if rec["dma"]:
                    ins.then_inc(dsem[e][rec["slot"]], 16)
                elif rec["marked"]:
                    ins.then_inc(csem[e], 1)

        with nc.Block() as block:
            @block.tensor
            def _(eng):
                emit_eng("pe", eng)

            @block.scalar
            def _(eng):
                emit_eng("act", eng)

            @block.vector
            def _(eng):
                emit_eng("dve", eng)

            @block.gpsimd
            def _(eng):
                emit_eng("pool", eng)

            @block.sync
            def _(eng):
                emit_eng("sp", eng)

D = 1024
NCH = 8
DFF = 2816
NF = 22
NE = 8
EPS = 1e-6
GN_EPS = 1e-5

C_ID, C_ONE, C_TRI, C_MS, C_MI, C_BD, C_BM, C_PM = [i * 128 for i in range(8)]


def host_consts(S):
    i = np.arange(128)
    cst = np.zeros((128, 1024), np.float32)
    cst[:, C_ID:C_ID + 128] = np.eye(128)
    cst[:, C_ONE:C_ONE + 128] = 1.0
    cst[:, C_TRI:C_TRI + 128] = (i[:, None] >= i[None, :])
    cst[:, C_MS:C_MS + 128] = (i[:, None] < i[None, :])
    cst[:, C_MI:C_MI + 128] = (i[:, None] <= i[None, :])
    blk = (i[:, None] // 64 == i[None, :] // 64)
    cst[:, C_BD:C_BD + 128] = blk / 64.0
    cst[:, C_BM:C_BM + 128] = blk
    partner = (i // 64) * 64 + (i % 64 + 32) % 64
    pm = np.zeros((128, 128), np.float32)
    pm[partner, i] = 1.0
    cst[:, C_PM:C_PM + 128] = pm
    half = 32
    inv_freq = (10000.0 ** (-np.arange(half, dtype=np.float32) / half)).astype(np.float32)
    ang = (np.arange(S, dtype=np.float32)[:, None] * inv_freq[None, :]).astype(np.float32)
    cos = np.cos(ang).astype(np.float32).T
    sin = np.sin(ang).astype(np.float32).T
    d = i % 64
    cosT = cos[d % 32]
    sinS = np.where((d < 32)[:, None], -sin[d % 32], sin[d % 32])
    rot = np.stack([cosT, sinS, cosT * 0.125, sinS * 0.125]).astype(np.float32)
    lg = np.log(1.0 - 2.0 ** (-5.0 - np.arange(8, dtype=np.float32))).astype(np.float32)
    pos = np.arange(128, dtype=np.float32)
    rdt = np.zeros((4, 128, 256), np.float32)
    rkd = np.zeros((4, 128, 128), np.float32)
    rqd = np.zeros((4, 128, 512), np.float32)
    rcd = np.zeros((128, 4), np.float32)
    for j in range(4):
        for hh in range(2):
            g = lg[2 * j + hh]
            diff = pos[None, :] - pos[:, None]
            rdt[j, :, hh * 128:(hh + 1) * 128] = np.where(diff >= 0, np.exp(g * np.maximum(diff, 0)), 0.0)
            rkd[j, :, hh * 64:(hh + 1) * 64] = np.exp(g * (127.0 - pos))[:, None]
            rqd[j, hh * 64:(hh + 1) * 64, :] = np.tile(np.exp(g * (pos + 1.0)), 4)[None, :]
            rcd[hh * 64:(hh + 1) * 64, j] = np.exp(g * 128.0)
    iot = (np.arange(22, dtype=np.float32)[None, :] * 128.0 + np.arange(128, dtype=np.float32)[:, None]).astype(np.float32)
    return {"cst": cst, "rot": rot, "rdt": rdt, "rkd": rkd, "rqd": rqd, "rcd": rcd, "iot": iot}


SB_WIDE = False
GT_OVERRIDE = None
SKIP_SLOTS = False
DEBUG_ZERO_FLAGS = False


def build(S, stop=None, sparse=True, nfill_sb=0, nfill_fox=0):
    nc = bass.Bass("TRN2", target_bir_lowering=False)
    P = Prog(nc)
    NT = S // 512
    NB = S // 128
    GT = GT_OVERRIDE or (2 if NT >= 2 else 1)
    NG = NT // GT

    def din(name, shape):
        return nc.dram_tensor(name, list(shape), F32, kind="ExternalInput").ap()

    def dscr(name, shape, dt):
        return nc.dram_tensor(name, list(shape), dt).ap()

    xT = din("xT", [D, S])
    outT = None if (sparse and stop is None) else nc.dram_tensor("outT", [D, S], F32, kind="ExternalOutput").ap()
    gains = {n: din(n, [128, 8]) for n in ("an_e", "fn_e", "an_o", "fn_o", "fin")}
    rn_e = din("rn_e", [128, 4])
    bf_o = din("bf_o", [16, 1])
    wr_o = din("wr_o", [D, 8])
    wsrc = {
        "w_in_e": din("w_in_e", [D, 3584]), "w_out_e": din("w_out_e", [D, D]),
        "wg_e": din("wg_e", [D, DFF]), "wu_e": din("wu_e", [D, DFF]), "wd_e": din("wd_e", [DFF, D]),
        "w_in_o": din("w_in_o", [D, 3088]), "w_out_o": din("w_out_o", [D, D]),
        "wg_m": din("wg_m", [NE, D, DFF]), "wu_m": din("wu_m", [NE, D, DFF]), "wd_m": din("wd_m", [NE, DFF, D]),
    }
    cst_d = din("cst", [128, 1024])
    rot_d = din("rot", [4, 128, S])
    rdt_d = din("rdt", [4, 128, 256])
    rkd_d = din("rkd", [4, 128, 128])
    rqd_d = din("rqd", [4, 128, 512])
    rcd_d = din("rcd", [128, 4])
    iot_d = din("iot", [128, 22])
    fin_row = din("fin_row", [1, D])
    NS = (2 * S) // 512 + NE
    HF = DFF // 2
    out_nat = nc.dram_tensor("out", [S, D], F32, kind="ExternalOutput").ap() if (sparse and stop is None) else None
    wb = {k: dscr(k + "_b", v.shape, BF16) for k, v in wsrc.items() if not (sparse and k in ("wg_m", "wu_m", "wd_m"))}
    if sparse:
        wb["wg_m"] = [dscr(f"wgm2_{h}", [NE * 128, 8 * HF], BF16) for h in range(2)]
        wb["wu_m"] = [dscr(f"wum2_{h}", [NE * 128, 8 * HF], BF16) for h in range(2)]
        wb["wd_m"] = [dscr(f"wdm2_{h}", [NE * 128, (NF // 2) * D], BF16) for h in range(2)]
        Hn_d = dscr("Hn", [S, D], BF16)
        H3_d = dscr("H3tok", [S, D], F32)
        Xs_d = dscr("Xs", [NS * 512, D], BF16)
        Ys_d = dscr("Ys", [NS * 512, D], F32)
    wbuf = {}
    h1T = dscr("h1T", [D, S], F32)
    h2T = dscr("h2T", [D, S], F32)
    h3T = dscr("h3T", [D, S], F32)
    h4T = dscr("h4T", [D, S], F32)
    hb = {id(t): Buf() for t in (h1T, h2T, h3T, h4T, outT, xT)}
    nullb = Buf()

    def dma(q, out, in_, reads=(), writes=(), is_out=False, **kw):
        return P.op(q, lambda e: e.dma_start(out=out, in_=in_, **kw), reads=reads, writes=writes, dma=True, is_out=is_out)

    def mm(out, lhsT, rhs, start, stop, reads, writes, skip=False):
        if skip:
            return P.op("pe", lambda e: e.matmul(out, lhsT=lhsT, rhs=rhs, start=start, stop=stop, skip_group_check=True),
                        reads=reads, writes=writes)
        return P.op("pe", lambda e: e.matmul(out, lhsT=lhsT, rhs=rhs, start=start, stop=stop), reads=reads, writes=writes)

    def act(out, in_, func, reads, writes, bias=None, scale=None):
        kw = {}
        if bias is not None:
            kw["bias"] = bias
        if scale is not None:
            kw["scale"] = scale
        return P.op("act", lambda e: e.activation(out=out, in_=in_, func=func, **kw), reads=reads, writes=writes)

    def tt(out, in0, in1, op, reads, writes, eng="dve"):
        return P.op(eng, lambda e: e.tensor_tensor(out=out, in0=in0, in1=in1, op=op), reads=reads, writes=writes)

    def ts(out, in0, s1, op0, reads, writes, s2=None, op1=None, eng="dve"):
        if op1 is None:
            return P.op(eng, lambda e: e.tensor_scalar(out=out, in0=in0, scalar1=s1, scalar2=None, op0=op0), reads=reads, writes=writes)
        return P.op(eng, lambda e: e.tensor_scalar(out=out, in0=in0, scalar1=s1, scalar2=s2, op0=op0, op1=op1), reads=reads, writes=writes)

    def stt(out, in0, scalar, in1, op0, op1, reads, writes, eng="dve"):
        return P.op(eng, lambda e: e.scalar_tensor_tensor(out=out, in0=in0, scalar=scalar, in1=in1, op0=op0, op1=op1),
                    reads=reads, writes=writes)

    def cp(out, in_, reads, writes, eng="dve"):
        if eng == "act":
            return P.op("act", lambda e: e.copy(out=out, in_=in_), reads=reads, writes=writes)
        return P.op(eng, lambda e: e.tensor_copy(out=out, in_=in_), reads=reads, writes=writes)

    def recip(out, in_, reads, writes):
        return P.op("dve", lambda e: e.reciprocal(out=out, in_=in_), reads=reads, writes=writes)

    def cast_w(name):
        src, dst = wsrc[name], wb[name]
        if len(src.shape) == 3:
            bl = []
            for e_ in range(src.shape[0]):
                b = Buf()
                dma("pool", dst[e_], src[e_], writes=[b], max_dma_last_dim=4096)
                bl.append(b)
            wbuf[name] = bl
        else:
            b = Buf()
            dma("pool", dst, src, writes=[b], max_dma_last_dim=4096)
            wbuf[name] = b

    for name in ("w_in_e", "w_out_e"):
        cast_w(name)
    for name in ("wg_m", "wu_m", "wd_m"):
        wbuf[name] = []

    def cast_expert(e_):
        for name in ("wg_m", "wu_m", "wd_m"):
            if not sparse:
                b = Buf()
                dma("pool", wb[name][e_], wsrc[name][e_], writes=[b], max_dma_last_dim=4096)
                wbuf[name].append(b)
                continue
            for h in range(2):
                b = Buf()
                if name == "wd_m":
                    for fl in range(NF // 2):
                        r0 = h * HF + fl * 128
                        dma("pool", wb[name][h][e_ * 128:(e_ + 1) * 128, fl * D:(fl + 1) * D], wsrc[name][e_][r0:r0 + 128, :], writes=[b],
                            max_dma_last_dim=4096)
                else:
                    for c in range(8):
                        dma("pool", wb[name][h][e_ * 128:(e_ + 1) * 128, c * HF:(c + 1) * HF],
                            wsrc[name][e_][c * 128:(c + 1) * 128, h * HF:(h + 1) * HF], writes=[b], max_dma_last_dim=2816)
                wbuf[name].append(b)

    cst = P.sb([128, 1024], F32)
    cstb = P.sb([128, 1024], BF16)
    cb = Buf()
    gsb = {n: P.sb([128, 8], F32) for n in gains}
    rn_sb = P.sb([128, 4], F32)
    rcd_sb = P.sb([128, 4], F32)
    gb = Buf()
    dma("sp", cst[:], cst_d, writes=[cb])
    cp(cstb[:], cst[:], [cb], [cb])
    for n in gains:
        dma("sp", gsb[n][:], gains[n], writes=[gb])
    dma("sp", rn_sb[:], rn_e, writes=[gb])
    dma("sp", rcd_sb[:], rcd_d, writes=[gb])
    ident = cst[:, C_ID:C_ID + 128]
    ones32 = cst[:, C_ONE:C_ONE + 128]
    blockmask = cst[:, C_BM:C_BM + 128]
    pm32 = cst[:, C_PM:C_PM + 128]
    identb = cstb[:, C_ID:C_ID + 128]
    onesb = cstb[:, C_ONE:C_ONE + 128]
    trib = cstb[:, C_TRI:C_TRI + 128]
    maskSb = cstb[:, C_MS:C_MS + 128]
    maskIb = cstb[:, C_MI:C_MI + 128]
    maskS32 = cst[:, C_MS:C_MS + 128]
    bdb = cstb[:, C_BD:C_BD + 128]

    psall = nc.alloc_psum_tensor("psall", [128, 4096], F32)
    ps = [psall[:, i * 512:(i + 1) * 512] for i in range(8)]
    pb = [Buf() for _ in range(8)]
    persist_mark = P.sb_off
    if stop == "cast":
        hb[id(xT)] = nullb
        finish_early = True
    else:
        finish_early = False

    def view_T(dr):
        return dr.rearrange("(c p) s -> p c s", p=128)

    def finish(src):
        mark = P.sb_off
        xb_ = [(P.sb([128, 8, 512], F32), Buf()) for _ in range(2)]
        for t in range(NT):
            xs, xs_b = xb_[t % 2]
            dma("sp", xs[:], view_T(src)[:, :, t * 512:(t + 1) * 512], reads=[hb[id(src)]], writes=[xs_b])
            dma("sp", view_T(outT)[:, :, t * 512:(t + 1) * 512], xs[:], reads=[xs_b], writes=[hb[id(outT)]], is_out=True)
        P.sb_off = mark


    def norm_tiles(src, src_b, gain, tiles, dst_fn, pbank, xbufs, sqb, rsb, keep32=None):
        sqs = sqb if isinstance(sqb, list) else [sqb]
        rss = rsb if isinstance(rsb, list) else [rsb]
        pbank0 = pbank
        for n, t in enumerate(tiles):
            sq, sq_b = sqs[n % len(sqs)]
            r, r_b = rss[n % len(rss)]
            pbank = pbank0 + (n % len(sqs))
            xs, xs_b = xbufs[n % len(xbufs)]
            dma("sp", xs[:], view_T(src)[:, :, t * 512:(t + 1) * 512], reads=[src_b], writes=[xs_b])
            act(sq[:], xs[:], AF.Square, [xs_b], [sq_b])
            for c in range(8):
                mm(ps[pbank][:], onesb, sq[:, c, :], c == 0, c == 7, [cb, sq_b], [pb[pbank]])
            act(r[:], ps[pbank][:], AF.Sqrt, [pb[pbank]], [r_b], bias=EPS, scale=1.0 / D)
            recip(r[:], r[:], [r_b], [r_b])
            o, o_b = dst_fn(t)
            for c in range(8):
                stt(o[:, c, :], xs[:, c, :], gain[:, c:c + 1], r[:], ALU.mult, ALU.mult, [xs_b, r_b, gb], [o_b])
            if keep32 is not None:
                keep32(t, xs, xs_b, r, r_b)

    def out_proj(yT, y_b, wname, res, dst):
        mark = P.sb_off
        wo = P.sb([128, 8, D], BF16)
        wo_b = Buf()
        dma("sp", wo[:], wb[wname].rearrange("(c p) n -> p c n", p=128), reads=[wbuf[wname]], writes=[wo_b])
        xb2 = [(P.sb([128, 8, 512], F32), Buf()) for _ in range(2)]
        def load_res(t):
            xs, xs_b = xb2[t % 2]
            dma("sp", xs[:], view_T(res)[:, :, t * 512:(t + 1) * 512], reads=[hb.get(id(res), nullb)], writes=[xs_b])

        load_res(0)
        for t in range(NT):
            xs, xs_b = xb2[t % 2]
            if t + 1 < NT:
                load_res(t + 1)
            for m in range(8):
                bk = m % 2
                for c in range(8):
                    mm(ps[bk][:], wo[:, c, m * 128:(m + 1) * 128], yT[:, c, t * 512:(t + 1) * 512], c == 0, c == 7,
                       [wo_b, y_b], [pb[bk]])
                tt(xs[:, m, :], ps[bk][:], xs[:, m, :], ALU.add, [pb[bk], xs_b], [xs_b])
            dma("sp", view_T(dst)[:, :, t * 512:(t + 1) * 512], xs[:], reads=[xs_b], writes=[hb[id(dst)]])
        P.sb_off = mark

    if finish_early:
        xb_ = [(P.sb([128, 8, 512], F32), Buf()) for _ in range(2)]
        for t in range(NT):
            xs, xs_b = xb_[t % 2]
            dma("sp", xs[:], view_T(xT)[:, :, t * 512:(t + 1) * 512], writes=[xs_b])
            dma("sp", view_T(outT)[:, :, t * 512:(t + 1) * 512], xs[:], reads=[xs_b], writes=[hb[id(outT)]], is_out=True)
        P.barrier()
        P.emit()
        return nc
    hnT = P.sb([128, 8, S], BF16)
    hn_b = Buf()
    yT = P.sb([128, 8, S], BF16)
    y_b = Buf()
    l0_mark = P.sb_off
    xbufs = [(P.sb([128, 8, 512], F32), Buf()) for _ in range(2)]
    sqb = [(P.sb([128, 8, 512], BF16), Buf()) for _ in range(2)]
    rsb = [(P.sb([128, 512], F32), Buf()) for _ in range(2)]
    norm_tiles(xT, nullb, gsb["an_e"], range(NT), lambda t: (hnT[:, :, t * 512:(t + 1) * 512], hn_b), 0, xbufs, sqb, rsb)
    P.sb_off = l0_mark
    P.barrier()

    for name in ("wg_e", "wu_e", "wd_e", "w_in_o", "w_out_o"):
        cast_w(name)
    w_in_e_v = wb["w_in_e"].rearrange("(c p) n -> p c n", p=128)

    def retention():
        mark = P.sb_off
        wq = P.sb([128, 4, 8, 128], BF16, off=None) if False else None
        wts = [P.sb([128, 4, 8, 128], BF16) for _ in range(1)]
        wall = wts[0]
        w_b = Buf()
        dtab = P.sb([128, 256], F32)
        kdt = P.sb([128, 128], F32)
        qdt = P.sb([128, 512], F32)
        tb = Buf()
        S32 = [P.sb([128, 128], F32) for _ in range(4)]
        Sbf = [P.sb([128, 128], BF16) for _ in range(4)]
        S_b = [Buf() for _ in range(4)]
        for j in range(4):
            P.op("dve", lambda e, j=j: e.memset(S32[j][:], 0.0), writes=[S_b[j]])
            P.op("dve", lambda e, j=j: e.memset(Sbf[j][:], 0.0), writes=[S_b[j]])
        rot = [(P.sb([128, 4, 512], F32), Buf()) for _ in range(2)]
        WQ, WK, WV, WG = 0, 1, 2, 3
        q32 = P.sb([128, 512], F32); q32_b = Buf()
        ta = P.sb([128, 512], F32); ta_b = Buf()
        tb2 = P.sb([128, 512], F32); tb2_b = Buf()
        qr = P.sb([128, 512], BF16); qr_b = Buf()
        qd = P.sb([128, 512], BF16); qd_b = Buf()
        kr = P.sb([128, 512], BF16); kr_b = Buf()
        kdk = P.sb([128, 4, 128], BF16); kdk_b = Buf()
        vtk = P.sb([128, 4, 128], BF16); vtk_b = Buf()
        sg = P.sb([128, 512], F32); sg_b = Buf()
        sm = [(P.sb([128, 128], BF16), Buf()) for _ in range(2)]
        o32 = P.sb([128, 512], F32); o32_b = Buf()
        obf = P.sb([128, 512], BF16); obf_b = Buf()
        cen = P.sb([128, 512], F32); cen_b = Buf()
        c2 = P.sb([128, 512], BF16); c2_b = Buf()
        rs = P.sb([128, 512], F32); rs_b = Buf()

        def proj_fm(wt, j, t, bank):
            for c in range(8):
                mm(ps[bank][:], wall[:, wt, c, :], hnT[:, c, t * 512:(t + 1) * 512], c == 0, c == 7, [w_b, hn_b], [pb[bank]])

        def rotary(j, t, wt, ci, out_bf, out_b, rt, rt_b, dec=None):
            proj_fm(wt, j, t, 0)
            cp(q32[:], ps[0][:], [pb[0]], [q32_b], eng="act")
            mm(ps[1][:], pm32, q32[:], True, True, [cb, q32_b], [pb[1]])
            tt(ta[:], q32[:], rt[:, ci, :], ALU.mult, [q32_b, rt_b], [ta_b])
            tt(tb2[:], ps[1][:], rt[:, ci + 1, :], ALU.mult, [pb[1], rt_b], [tb2_b])
            tt(ta[:], ta[:], tb2[:], ALU.add, [ta_b, tb2_b], [ta_b])
            cp(out_bf[:], ta[:], [ta_b], [out_b], eng="act")
            if dec is not None:
                tt(dec[0][:], ta[:], qdt[:], ALU.mult, [ta_b, tb], [dec[1]])

        it_ = 0
        for j in range(4):
            for k_ in range(4):
                c0 = k_ * 512 + j * 128
                dma("sp", wall[:, k_, :, :], w_in_e_v[:, :, c0:c0 + 128], reads=[wbuf["w_in_e"]], writes=[w_b])
            dma("sp", dtab[:], rdt_d[j], writes=[tb])
            dma("sp", kdt[:], rkd_d[j], writes=[tb])
            dma("sp", qdt[:], rqd_d[j], writes=[tb])
            for t in range(NT):
                rt, rt_b = rot[it_ % 2]
                it_ += 1
                for ci in range(4):
                    dma("sp", rt[:, ci, :], rot_d[ci, :, t * 512:(t + 1) * 512], writes=[rt_b])
                rotary(j, t, WQ, 0, qr, qr_b, rt, rt_b, dec=(qd, qd_b))
                rotary(j, t, WK, 2, kr, kr_b, rt, rt_b)
                proj_fm(WG, j, t, 2)
                act(sg[:], ps[2][:], AF.Silu, [pb[2]], [sg_b])
                for n in range(4):
                    tok = slice(t * 512 + n * 128, t * 512 + (n + 1) * 128)
                    for c in range(8):
                        mm(ps[3][:, n * 128:(n + 1) * 128], hnT[:, c, tok], wall[:, WV, c, :], (n == 0 and c == 0), c == 7,
                           [hn_b, w_b], [pb[3]], skip=True)
                cp(vtk[:].rearrange("p n f -> p (n f)"), ps[3][:], [pb[3]], [vtk_b], eng="act")
                ktp = ps[4][:].bitcast(BF16)
                for n in range(4):
                    P.op("pe", lambda e, n=n: e.transpose(ktp[:, n * 128:(n + 1) * 128], kr[:, n * 128:(n + 1) * 128], identb),
                         reads=[kr_b, cb], writes=[pb[4]])
                tt(kdk[:].rearrange("p n f -> p n f"), ktp[:, 0:512].rearrange("p (n f) -> p n f", n=4),
                   kdt[:, None, :].to_broadcast([128, 4, 128]), ALU.mult, [pb[4], tb], [kdk_b])
                for n in range(4):
                    cs = slice(n * 128, (n + 1) * 128)
                    for hh in range(2):
                        hs = slice(hh * 64, (hh + 1) * 64)
                        bk = 5 + hh
                        smt, smt_b = sm[hh]
                        mm(ps[7][:, hh * 128:(hh + 1) * 128], kr[hs, cs], qr[hs, cs], True, True, [kr_b, qr_b], [pb[7]], skip=True)
                        tt(smt[:], ps[7][:, hh * 128:(hh + 1) * 128], dtab[:, hh * 128:(hh + 1) * 128], ALU.mult,
                           [pb[7], tb], [smt_b])
                        mm(ps[bk][:, cs], vtk[:, n, :], smt[:], (n == 0), False, [vtk_b, smt_b], [pb[bk]], skip=True)
                        mm(ps[bk][:, cs], Sbf[j][:], qd[:, cs], False, True, [S_b[j], qd_b], [pb[bk]], skip=True)
                    mm(ps[1][:, 0:128], kdk[:, n, :], vtk[:, n, :], True, True, [kdk_b, vtk_b], [pb[1]])
                    stt(S32[j][:], S32[j][:], rcd_sb[:, j:j + 1], ps[1][:, 0:128], ALU.mult, ALU.add, [S_b[j], pb[1], gb], [S_b[j]])
                    tt(Sbf[j][:], S32[j][:], blockmask, ALU.mult, [S_b[j], cb], [S_b[j]])
                cp(o32[0:64, :], ps[5][0:64, :], [pb[5]], [o32_b], eng="act")
                cp(o32[64:128, :], ps[6][64:128, :], [pb[6]], [o32_b], eng="act")
                cp(obf[:], o32[:], [o32_b], [obf_b], eng="act")
                mm(ps[0][:], bdb, obf[:], True, True, [cb, obf_b], [pb[0]])
                tt(cen[:], o32[:], ps[0][:], ALU.subtract, [o32_b, pb[0]], [cen_b])
                act(c2[:], cen[:], AF.Square, [cen_b], [c2_b])
                mm(ps[2][:], bdb, c2[:], True, True, [cb, c2_b], [pb[2]])
                act(rs[:], ps[2][:], AF.Sqrt, [pb[2]], [rs_b], bias=GN_EPS, scale=1.0)
                recip(rs[:], rs[:], [rs_b], [rs_b])
                tt(cen[:], cen[:], rs[:], ALU.mult, [cen_b, rs_b], [cen_b])
                stt(yT[:, j, t * 512:(t + 1) * 512], cen[:], rn_sb[:, j:j + 1], sg[:], ALU.mult, ALU.mult,
                    [cen_b, sg_b, gb], [y_b])
        P.sb_off = mark

    if stop == "norm0":
        finish(xT); P.emit(); return nc
    retention()
    P.barrier()
    if stop == "ret":
        finish(xT); P.emit(); return nc

    def attention_pair(kind, w_v, wname, qc0, kc0, vc0, ychunk, btab=None, hidx=None):
        mark = P.sb_off
        first_call = P.begin_replay(kind)
        Buf = P.buf
        wq = P.sb([128, 8, 128], BF16)
        wk = P.sb([128, 8, 128], BF16)
        wv = P.sb([128, 8, 128], BF16)
        w_b = Buf()
        for wt, c0 in ((wq, qc0), (wk, kc0), (wv, vc0)):
            dma("sp", wt[:], w_v[:, :, c0:c0 + 128], reads=[wbuf[wname]], writes=[w_b])
        qTp = P.sb([128, S], BF16); q_b = Buf()
        kTp = P.sb([128, S], BF16); k_b = Buf()
        if kind == "sb":
            vt = [P.sb([128, NB, 128], BF16)]
        else:
            vt = [P.sb([128, NB, 128], BF16), P.sb([128, NB, 128], BF16)]
        v_b = Buf()
        for t in range(NT):
            ts_ = slice(t * 512, (t + 1) * 512)
            for (wt, dst, db, bk) in ((wq, qTp, q_b, 0), (wk, kTp, k_b, 1)):
                for c in range(8):
                    mm(ps[bk][:], wt[:, c, :], hnT[:, c, ts_], c == 0, c == 7, [w_b, hn_b], [pb[bk]])
                cp(dst[:, ts_], ps[bk][:], [pb[bk]], [db], eng=("act" if bk == 0 else "dve"))
            for n in range(4):
                tok = slice(t * 512 + n * 128, t * 512 + (n + 1) * 128)
                for c in range(8):
                    mm(ps[2][:, n * 128:(n + 1) * 128], hnT[:, c, tok], wv[:, c, :], (n == 0 and c == 0), c == 7,
                       [hn_b, w_b], [pb[2]], skip=True)
            if kind == "sb":
                cp(vt[0][:, t * 4:(t + 1) * 4, :].rearrange("p n f -> p (n f)"), ps[2][:], [pb[2]], [v_b], eng="act")
            else:
                pv = ps[2][:].rearrange("p (n f) -> p n f", n=4)
                cp(vt[0][:, t * 4:(t + 1) * 4, 0:64], pv[:, :, 0:64], [pb[2]], [v_b], eng="act")
                cp(vt[1][:, t * 4:(t + 1) * 4, 0:64], pv[:, :, 64:128], [pb[2]], [v_b], eng="dve")
        if kind == "fox" and first_call:
            for hh in range(2):
                P.op("dve", lambda e, hh=hh: e.memset(vt[hh][:, :, 64:128], 1.0), writes=[v_b])
        if kind == "sb" and SB_WIDE:
            PD = 3
            es_p = [(P.sb([128, 2, 512], F32), Buf()) for _ in range(PD)]
            Ls_p = [(P.sb([128, 2, 512], BF16), Buf()) for _ in range(PD)]
            gs_p = [(P.sb([128, 2, 512], F32), Buf()) for _ in range(2)]
            ws_p = [(P.sb([128, 2, 512], BF16), Buf()) for _ in range(PD)]
            la = P.sb([128, 2, 512], BF16); la_b = Buf()
            zbufs = [Buf(), Buf()]; cbuf = [Buf()]
            pzs = [psall[:, k * 1024:(k + 1) * 1024].rearrange("p (h n) -> p h n", h=2) for k in range(2)]
            pc = [psall[:, 2048:3072].rearrange("p (h n) -> p h n", h=2)]
            punits = []
            for qt in range(NT):
                nblk = 4 * qt + 4
                for idx, jb in enumerate(range(nblk - 1, -1, -1)):
                    punits.append((qt, jb, idx == 0, idx == nblk - 1))

            def pgeom(u):
                qt, jb, first, last = u
                c0 = 128 * max(0, jb - 4 * qt)
                return c0, slice(c0, 512), slice(qt * 512 + c0, (qt + 1) * 512), jb >= 4 * qt

            def pA(u, i):
                qt, jb, first, last = u
                c0, cols, qcols, diag = pgeom(u)
                es, es_b = es_p[i % PD]; Ls, Ls_b = Ls_p[i % PD]
                pz = pzs[i % 2]; zbuf = zbufs[i % 2]
                for hh in range(2):
                    hs = slice(hh * 64, (hh + 1) * 64)
                    mm(pz[:, hh, cols], kTp[hs, jb * 128:(jb + 1) * 128], qTp[hs, qcols], True, True, [k_b, q_b], [zbuf])
                act(es[:, :, cols], pz[:, :, cols], AF.Exp, [zbuf], [es_b], scale=0.125)
                if diag:
                    tt(es[:, :, c0:c0 + 128], es[:, :, c0:c0 + 128], maskS32[:, None, :].to_broadcast([128, 2, 128]), ALU.mult,
                       [es_b, cb], [es_b])
                act(Ls[:, :, cols], es[:, :, cols], AF.Ln, [es_b], [Ls_b], bias=1.0)

            def pB(u, i):
                qt, jb, first, last = u
                c0, cols, qcols, diag = pgeom(u)
                es, es_b = es_p[i % PD]; Ls, Ls_b = Ls_p[i % PD]; gs, gs_b = gs_p[i % 2]; ws, ws_b = ws_p[i % PD]
                pcc = pc[0]; pcb = cbuf[0]
                for hh in range(2):
                    mm(pcc[:, hh, cols], trib, Ls[:, hh, cols], True, first, [cb, Ls_b], [pcb])
                    if not first:
                        mm(pcc[:, hh, cols], onesb, la[:, hh, cols], False, True, [cb, la_b], [pcb])
                act(gs[:, :, cols], pcc[:, :, cols], AF.Exp, [pcb], [gs_b], scale=-1.0)
                tt(ws[:, :, cols], es[:, :, cols], gs[:, :, cols], ALU.mult, [es_b, gs_b], [ws_b])
                if not last:
                    if first:
                        P.op("dve", lambda e: e.memset(la[:], 0.0), writes=[la_b])
                    tt(la[:, :, cols], la[:, :, cols], Ls[:, :, cols], ALU.add, [la_b, Ls_b], [la_b])

            def pC(u, i):
                qt, jb, first, last = u
                c0, cols, qcols, diag = pgeom(u)
                ws, ws_b = ws_p[i % PD]
                for hh in range(2):
                    hs = slice(hh * 64, (hh + 1) * 64)
                    mm(ps[6 + hh][0:64, cols], vt[0][:, jb, hs], ws[:, hh, cols], first, last, [v_b, ws_b], [pb[6 + hh]], skip=True)
                    if last:
                        cp(yT[hs, ychunk, qt * 512:(qt + 1) * 512], ps[6 + hh][0:64, :], [pb[6 + hh]], [y_b],
                           eng=("act" if hh == 0 else "dve"))

            npu = len(punits)
            for i in range(npu + 4):
                if i < npu:
                    pA(punits[i], i)
                if 2 <= i < npu + 2:
                    pB(punits[i - 2], i - 2)
                if i >= 4:
                    pC(punits[i - 4], i - 4)
            P.end_replay()
            P.sb_off = mark
            return
        SK = 2
        DEP = 6
        w_sb = [(P.sb([128, 512], BF16), Buf()) for _ in range(DEP)]
        if kind == "sb":
            e_sb = [(P.sb([128, 512], F32), Buf()) for _ in range(DEP)]
            L_sb = [(P.sb([128, 512], BF16), Buf()) for _ in range(DEP)]
            g_sb = [(P.sb([128, 512], F32), Buf()) for _ in range(2)]
            Lacc = [(P.sb([128, 512], BF16), Buf()) for _ in range(2)]
        else:
            rden = P.sb([128, 512], F32); rd_b = Buf()
        units = []
        for qt in range(NT):
            nblk = 4 * qt + 4
            order = list(range(nblk - 1, -1, -1)) if kind == "sb" else list(range(nblk))
            for idx, jb in enumerate(order):
                for hh in range(2):
                    units.append((qt, hh, jb, idx == 0, idx == nblk - 1))

        def geom(u):
            qt, hh, jb, first, last = u
            m = max(0, jb - 4 * qt)
            c0 = 128 * m
            return (slice(hh * 64, (hh + 1) * 64), c0, slice(c0, 512), slice(qt * 512 + c0, (qt + 1) * 512), jb >= 4 * qt)

        junk_b = Buf()
        nfill = nfill_sb if kind == "sb" else nfill_fox
        jbank = 5 if kind == "sb" else 3

        def stageA(u, i):
            qt, hh, jb, first, last = u
            hs, c0, cols, qcols, diag = geom(u)
            zb = i % 3
            for _ in range(nfill):
                mm(ps[jbank][:], onesb, cstb[:, 0:512], True, True, [cb], [junk_b])
            mm(ps[zb][:, cols], kTp[hs, jb * 128:(jb + 1) * 128], qTp[hs, qcols], True, True, [k_b, q_b], [pb[zb]])
            if kind == "sb":
                es, es_b = e_sb[i % DEP]; Ls, Ls_b = L_sb[i % DEP]
                act(es[:, cols], ps[zb][:, cols], AF.Exp, [pb[zb]], [es_b], scale=0.125)
                if diag:
                    tt(es[:, c0:c0 + 128], es[:, c0:c0 + 128], maskS32, ALU.mult, [es_b, cb], [es_b])
                act(Ls[:, cols], es[:, cols], AF.Ln, [es_b], [Ls_b], bias=1.0)
            else:
                ws, ws_b = w_sb[i % DEP]
                act(ws[:, cols], ps[zb][:, cols], AF.Exp, [pb[zb]], [ws_b],
                    bias=btab[:, hidx + hh, qt, jb:jb + 1], scale=0.125)
                if diag:
                    tt(ws[:, c0:c0 + 128], ws[:, c0:c0 + 128], maskIb, ALU.mult, [ws_b, cb], [ws_b])

        def stageB(u, i):
            qt, hh, jb, first, last = u
            hs, c0, cols, qcols, diag = geom(u)
            accb = 6 + hh
            ws, ws_b = w_sb[i % DEP]
            if kind == "sb":
                es, es_b = e_sb[i % DEP]; Ls, Ls_b = L_sb[i % DEP]; gs, gs_b = g_sb[i % 2]
                la, la_b = Lacc[hh]
                cb_ = 3 + (i % 2)
                mm(ps[cb_][:, cols], trib, Ls[:, cols], True, first, [cb, Ls_b], [pb[cb_]])
                if not first:
                    mm(ps[cb_][:, cols], onesb, la[:, cols], False, True, [cb, la_b], [pb[cb_]])
                act(gs[:, cols], ps[cb_][:, cols], AF.Exp, [pb[cb_]], [gs_b], scale=-1.0)
                tt(ws[:, cols], es[:, cols], gs[:, cols], ALU.mult, [es_b, gs_b], [ws_b])
                if not last:
                    if first:
                        P.op("dve", lambda e, la=la: e.memset(la[:], 0.0), writes=[la_b])
                    tt(la[:, cols], la[:, cols], Ls[:, cols], ALU.add, [la_b, Ls_b], [la_b], eng="dve")

        def stageC(u, i):
            qt, hh, jb, first, last = u
            hs, c0, cols, qcols, diag = geom(u)
            accb = 6 + hh
            ws, ws_b = w_sb[i % DEP]
            if kind == "sb":
                mm(ps[accb][0:64, cols], vt[0][:, jb, hs], ws[:, cols], first, last, [v_b, ws_b], [pb[accb]], skip=True)
            else:
                mm(ps[accb][:, cols], vt[hh][:, jb, :], ws[:, cols], first, last, [v_b, ws_b], [pb[accb]], skip=True)
            if last:
                ysl = yT[hs, ychunk, qt * 512:(qt + 1) * 512]
                if kind == "sb":
                    cp(ysl, ps[accb][0:64, :], [pb[accb]], [y_b], eng="act")
                else:
                    recip(rden[0:64, :], ps[accb][64:128, :], [pb[accb]], [rd_b])
                    tt(ysl, ps[accb][0:64, :], rden[0:64, :], ALU.mult, [pb[accb], rd_b], [y_b])

        SK2 = 2 * SK
        nu = len(units)
        for i0 in range(0, nu + SK2, 2):
            for i in (i0, i0 + 1):
                if i < nu:
                    stageA(units[i], i)
            for i in (i0, i0 + 1):
                if kind == "sb" and SK <= i < nu + SK:
                    stageB(units[i - SK], i - SK)
            for i in (i0, i0 + 1):
                if SK2 <= i < nu + SK2:
                    stageC(units[i - SK2], i - SK2)
        P.end_replay()
        P.sb_off = mark

    for j in range(4):
        cast_expert(2 * j)
        cast_expert(2 * j + 1)
        attention_pair("sb", w_in_e_v, "w_in_e", 2048 + j * 128, 2560 + j * 128, 3072 + j * 128, 4 + j)
    P.barrier()

    if stop == "sb":
        finish(xT); P.emit(); return nc
    out_proj(yT, y_b, "w_out_e", xT, h1T)
    P.barrier()
    P.sb_off = persist_mark

    def ffn_phase(src, dst, gain, moe):
        mark = P.sb_off
        GW = GT * 512
        hnG = P.sb([128, 8, GW], BF16); hnG_b = Buf()
        aT = P.sb([128, NF, GW], BF16); a_b = Buf()
        acc = P.sb([128, 8, GW], F32); acc_b = Buf()
        wd = P.sb([128, NF, D], BF16); wd_b = Buf()
        wgu = [(P.sb([128, 2, 8, 128], BF16), Buf()) for _ in range(2)]
        sq_off = (P.sb_off + 63) // 64 * 64
        sq = (P.sb([128, 8, 512], BF16), Buf())
        rsb_ = (P.sb([128, 512], F32), Buf())
        s_sb = [(P.sb([128, 512], F32), Buf()) for _ in range(2)]
        t_sb = [(P.sb([128, 512], BF16), Buf()) for _ in range(2)]
        if moe:
            comb = P.sb([128, NE, GW], BF16); comb_b = Buf()
            hn32 = P.sb([128, 8, 512], F32); hn32_b = Buf()
            wr = P.sb([128, 8, 8], F32); wr_b = Buf()
            dma("sp", wr[:], wr_o.rearrange("(c p) e -> p c e", p=128), writes=[wr_b])
            lg = P.sb([128, 8], F32); lg2 = P.sb([128, 8], F32); eq1 = P.sb([128, 8], F32); eq2 = P.sb([128, 8], F32)
            cmb = P.sb([128, 8], F32)
            sm_ = P.sb([128, 8], F32)
            De = P.sb([128, 8, 128], F32, off=sq_off)
            r_b = Buf()
        if not moe:
            dma("sp", wd[:], wb["wd_e"].rearrange("(f p) n -> p f n", p=128), reads=[wbuf["wd_e"]], writes=[wd_b])
        wcount = 0
        for g in range(NG):
            tiles = list(range(g * GT, (g + 1) * GT))
            gsl = slice(g * GW, (g + 1) * GW)
            dma("sp", acc[:], view_T(src)[:, :, gsl], reads=[hb[id(src)]], writes=[acc_b])
            for n, t in enumerate(tiles):
                xs = acc[:, :, n * 512:(n + 1) * 512]
                act(sq[0][:], xs, AF.Square, [acc_b], [sq[1]])
                for c in range(8):
                    mm(ps[0][:], onesb, sq[0][:, c, :], c == 0, c == 7, [cb, sq[1]], [pb[0]])
                r, rb_ = rsb_
                act(r[:], ps[0][:], AF.Sqrt, [pb[0]], [rb_], bias=EPS, scale=1.0 / D)
                recip(r[:], r[:], [rb_], [rb_])
                for c in range(8):
                    stt(hnG[:, c, n * 512:(n + 1) * 512], xs[:, c, :], gain[:, c:c + 1], r[:], ALU.mult, ALU.mult,
                        [acc_b, rb_, gb], [hnG_b])
                if moe:
                    for c in range(8):
                        stt(hn32[:, c, :], xs[:, c, :], gain[:, c:c + 1], r[:], ALU.mult, ALU.mult, [acc_b, rb_, gb], [hn32_b])
                    for blk in range(4):
                        bs = slice(blk * 128, (blk + 1) * 128)
                        for c in range(8):
                            mm(ps[1][:, 0:8], hn32[:, c, bs], wr[:, c, :], c == 0, c == 7, [hn32_b, wr_b], [pb[1]])
                        cp(lg[:], ps[1][:, 0:8], [pb[1]], [r_b])
                        P.op("dve", lambda e: e.reduce_max(out=sm_[:, 0:1], in_=lg[:], axis=AX.X), reads=[r_b], writes=[r_b])
                        ts(eq1[:], lg[:], sm_[:, 0:1], ALU.is_equal, [r_b], [r_b])
                        stt(lg2[:], eq1[:], -1e30, lg[:], ALU.mult, ALU.add, [r_b], [r_b])
                        P.op("dve", lambda e: e.reduce_max(out=sm_[:, 1:2], in_=lg2[:], axis=AX.X), reads=[r_b], writes=[r_b])
                        ts(eq2[:], lg2[:], sm_[:, 1:2], ALU.is_equal, [r_b], [r_b])
                        tt(sm_[:, 2:3], sm_[:, 1:2], sm_[:, 0:1], ALU.subtract, [r_b], [r_b])
                        act(sm_[:, 3:4], sm_[:, 2:3], AF.Exp, [r_b], [r_b])
                        ts(sm_[:, 4:5], sm_[:, 3:4], 1.0, ALU.add, [r_b], [r_b])
                        recip(sm_[:, 4:5], sm_[:, 4:5], [r_b], [r_b])
                        tt(sm_[:, 5:6], sm_[:, 3:4], sm_[:, 4:5], ALU.mult, [r_b], [r_b])
                        ts(cmb[:], eq1[:], sm_[:, 4:5], ALU.mult, [r_b], [r_b])
                        stt(cmb[:], eq2[:], sm_[:, 5:6], cmb[:], ALU.mult, ALU.add, [r_b], [r_b])
                        for e_ in range(NE):
                            ts(De[:, e_, :], ident, cmb[:, e_:e_ + 1], ALU.mult, [r_b, cb], [sq[1]])
                        for half in range(2):
                            mm(ps[2 + half][:], ones32, De[:, half * 4:(half + 1) * 4, :].rearrange("p e t -> p (e t)"),
                               True, True, [cb, sq[1]], [pb[2 + half]])
                            cp(comb[:, half * 4:(half + 1) * 4, n * 512 + blk * 128:n * 512 + (blk + 1) * 128],
                               ps[2 + half][:].rearrange("p (e t) -> p e t", e=4), [pb[2 + half]], [comb_b], eng="act")
            for e_ in range(NE if moe else 1):
                if moe:
                    wgv = wb["wg_m"][e_].rearrange("(c p) n -> p c n", p=128)
                    wuv = wb["wu_m"][e_].rearrange("(c p) n -> p c n", p=128)
                    wgb, wub = wbuf["wg_m"][e_], wbuf["wu_m"][e_]
                else:
                    wgv = wb["wg_e"].rearrange("(c p) n -> p c n", p=128)
                    wuv = wb["wu_e"].rearrange("(c p) n -> p c n", p=128)
                    wgb, wub = wbuf["wg_e"], wbuf["wu_e"]
                for f in range(NF):
                    wt, wt_b = wgu[wcount % 2]
                    wcount += 1
                    dma("sp", wt[:, 0, :, :], wgv[:, :, f * 128:(f + 1) * 128], reads=[wgb], writes=[wt_b])
                    dma("sp", wt[:, 1, :, :], wuv[:, :, f * 128:(f + 1) * 128], reads=[wub], writes=[wt_b])
                    if moe and f == 3:
                        dma("sp", wd[:], wb["wd_m"][e_].rearrange("(f p) n -> p f n", p=128), reads=[wbuf["wd_m"][e_]], writes=[wd_b])
                    for n in range(GT):
                        ns = slice(n * 512, (n + 1) * 512)
                        gbk, ubk = 4 + (n % 2) * 2, 5 + (n % 2) * 2
                        for c in range(8):
                            mm(ps[gbk][:], wt[:, 0, c, :], hnG[:, c, ns], c == 0, c == 7, [wt_b, hnG_b], [pb[gbk]])
                        for c in range(8):
                            mm(ps[ubk][:], wt[:, 1, c, :], hnG[:, c, ns], c == 0, c == 7, [wt_b, hnG_b], [pb[ubk]])
                        ss_, ss_b = s_sb[n % 2]
                        act(ss_[:], ps[gbk][:], AF.Silu, [pb[gbk]], [ss_b])
                        if moe:
                            tq, tq_b = t_sb[n % 2]
                            tt(tq[:], ps[ubk][:], ss_[:], ALU.mult, [pb[ubk], ss_b], [tq_b])
                            tt(aT[:, f, ns], tq[:], comb[:, e_, ns], ALU.mult, [tq_b, comb_b], [a_b], eng="pool")
                        else:
                            tt(aT[:, f, ns], ps[ubk][:], ss_[:], ALU.mult, [pb[ubk], ss_b], [a_b])
                for m in range(8):
                    for n in range(GT):
                        ns = slice(n * 512, (n + 1) * 512)
                        bk = (m * GT + n) % 4
                        for f in range(NF):
                            mm(ps[bk][:], wd[:, f, m * 128:(m + 1) * 128], aT[:, f, ns], f == 0, f == NF - 1, [wd_b, a_b], [pb[bk]])
                        tt(acc[:, m, ns], ps[bk][:], acc[:, m, ns], ALU.add, [pb[bk], acc_b], [acc_b])
            dma("sp", view_T(dst)[:, :, gsl], acc[:], reads=[acc_b], writes=[hb[id(dst)]])
        P.sb_off = mark


    def ffn_dense(src, dst, gain):
        mark = P.sb_off
        GW = GT * 512
        hnG = P.sb([128, 8, GW], BF16); hnG_b = Buf()
        aT = P.sb([128, NF, GW], BF16); a_b = Buf()
        accs = [(P.sb([128, 8, GW], F32), Buf()) for _ in range(2)]
        wd = P.sb([128, NF, D], BF16); wd_b = Buf()
        wgu = [(P.sb([128, 2, 8, 128], BF16), Buf()) for _ in range(3)]
        sq = (P.sb([128, 8, 512], BF16), Buf())
        r = P.sb([128, 512], F32); rb_ = Buf()
        s_sb = [(P.sb([128, 512], F32), Buf()) for _ in range(2)]
        dma("sp", wd[:], wb["wd_e"].rearrange("(f p) n -> p f n", p=128), reads=[wbuf["wd_e"]], writes=[wd_b])
        wgv = wb["wg_e"].rearrange("(c p) n -> p c n", p=128)
        wuv = wb["wu_e"].rearrange("(c p) n -> p c n", p=128)

        def gsl(g):
            return slice(g * GW, (g + 1) * GW)

        def load_res(g):
            acc, acc_b = accs[g % 2]
            dma("sp", acc[:], view_T(src)[:, :, gsl(g)], reads=[hb[id(src)]], writes=[acc_b])

        def norm(g):
            acc, acc_b = accs[g % 2]
            for n in range(GT):
                xs = acc[:, :, n * 512:(n + 1) * 512]
                act(sq[0][:], xs, AF.Square, [acc_b], [sq[1]])
                for c in range(8):
                    mm(ps[0][:], onesb, sq[0][:, c, :], c == 0, c == 7, [cb, sq[1]], [pb[0]])
                act(r[:], ps[0][:], AF.Sqrt, [pb[0]], [rb_], bias=EPS, scale=1.0 / D)
                recip(r[:], r[:], [rb_], [rb_])
                for c in range(8):
                    stt(hnG[:, c, n * 512:(n + 1) * 512], xs[:, c, :], gain[:, c:c + 1], r[:], ALU.mult, ALU.mult,
                        [acc_b, rb_, gb], [hnG_b])

        def load_w(f):
            wt, wt_b = wgu[f % 3]
            dma("sp", wt[:, 0, :, :], wgv[:, :, f * 128:(f + 1) * 128], reads=[wbuf["wg_e"]], writes=[wt_b])
            dma("sp", wt[:, 1, :, :], wuv[:, :, f * 128:(f + 1) * 128], reads=[wbuf["wu_e"]], writes=[wt_b])

        def gate_up(g, preloaded):
            for f in range(NF):
                wt, wt_b = wgu[f % 3]
                if f >= preloaded:
                    load_w(f)
                for n in range(GT):
                    ns = slice(n * 512, (n + 1) * 512)
                    gbk, ubk = 4 + (n % 2) * 2, 5 + (n % 2) * 2
                    for c in range(8):
                        mm(ps[gbk][:], wt[:, 0, c, :], hnG[:, c, ns], c == 0, c == 7, [wt_b, hnG_b], [pb[gbk]])
                    for c in range(8):
                        mm(ps[ubk][:], wt[:, 1, c, :], hnG[:, c, ns], c == 0, c == 7, [wt_b, hnG_b], [pb[ubk]])
                    ss_, ss_b = s_sb[n % 2]
                    act(ss_[:], ps[gbk][:], AF.Silu, [pb[gbk]], [ss_b])
                    tt(aT[:, f, ns], ps[ubk][:], ss_[:], ALU.mult, [pb[ubk], ss_b], [a_b])

        def down(g):
            acc, acc_b = accs[g % 2]
            for m in range(8):
                for n in range(GT):
                    ns = slice(n * 512, (n + 1) * 512)
                    bk = 1 + (m * GT + n) % 3
                    for f in range(NF):
                        mm(ps[bk][:], wd[:, f, m * 128:(m + 1) * 128], aT[:, f, ns], f == 0, f == NF - 1, [wd_b, a_b], [pb[bk]])
                    tt(acc[:, m, ns], ps[bk][:], acc[:, m, ns], ALU.add, [pb[bk], acc_b], [acc_b])

        def store(g):
            acc, acc_b = accs[g % 2]
            dma("sp", view_T(dst)[:, :, gsl(g)], acc[:], reads=[acc_b], writes=[hb[id(dst)]])

        load_res(0)
        norm(0)
        pre = 0
        for g in range(NG):
            if g + 1 < NG:
                load_res(g + 1)
            gate_up(g, pre)
            pre = 0
            if g + 1 < NG:
                load_w(0)
                load_w(1)
                load_w(2)
                pre = 3
                norm(g + 1)
            down(g)
            store(g)
        P.sb_off = mark

    if stop == "mix0":
        finish(h1T); P.emit(); return nc

    ffn_dense(h1T, h2T, gsb["fn_e"])
    P.barrier()
    if stop == "ffn0":
        finish(h2T); P.emit(); return nc

    hnT = P.sb([128, 8, S], BF16)
    hn_b = Buf()
    yT = P.sb([128, 8, S], BF16)
    y_b = Buf()
    btab = P.sb([128, 16, NT, NB], F32)
    bt_b = Buf()
    l1_mark = P.sb_off
    xbufs = [(P.sb([128, 8, 512], F32), Buf()) for _ in range(2)]
    sqb = [(P.sb([128, 8, 512], BF16), Buf()) for _ in range(2)]
    rsb = [(P.sb([128, 512], F32), Buf()) for _ in range(1)]
    norm_tiles(h2T, hb[id(h2T)], gsb["an_o"], range(NT), lambda t: (hnT[:, :, t * 512:(t + 1) * 512], hn_b), 0, xbufs, sqb, rsb)
    P.sb_off = l1_mark
    P.barrier()
    w_in_o_v = wb["w_in_o"].rearrange("(c p) n -> p c n", p=128)

    def forget_tables():
        mark = P.sb_off
        wf = P.sb([128, 8, 16], BF16); wf_b = Buf()
        dma("sp", wf[:], w_in_o_v[:, :, 3072:3088], reads=[wbuf["w_in_o"]], writes=[wf_b])
        bfs = P.sb([16, 2], F32); bfs_b = Buf()
        dma("sp", bfs[:, 0:1], bf_o, writes=[bfs_b])
        ts(bfs[:, 1:2], bfs[:, 0:1], -1.0, ALU.mult, [bfs_b], [bfs_b])
        lf = P.sb([16, S], F32); lf_b = Buf()
        cum = P.sb([16, S], F32); cum_b = Buf()
        one16 = P.sb([16, 512], F32); o16_b = Buf()
        P.op("dve", lambda e: e.memset(one16[:], 1.0), writes=[o16_b])
        for t in range(NT):
            ts_ = slice(t * 512, (t + 1) * 512)
            for c in range(8):
                mm(ps[0][0:16, :], wf[:, c, :], hnT[:, c, ts_], c == 0, c == 7, [wf_b, hn_b], [pb[0]])
            act(lf[:, ts_], ps[0][0:16, :], AF.Exp, [pb[0], bfs_b], [lf_b], bias=bfs[:, 1:2], scale=-1.0)
            act(lf[:, ts_], lf[:, ts_], AF.Ln, [lf_b], [lf_b], bias=1.0)
            init = 0.0 if t == 0 else cum[:, t * 512 - 1:t * 512]
            P.op("dve", lambda e, ts_=ts_, init=init: e.tensor_tensor_scan(out=cum[:, ts_], data0=one16[:], data1=lf[:, ts_],
                                                                           initial=init, op0=ALU.mult, op1=ALU.add),
                 reads=[lf_b, o16_b, cum_b], writes=[cum_b])
        ck = P.sb([128, NB, 16], F32); ck_b = Buf()
        for jb in range(NB):
            P.op("pe", lambda e, jb=jb: e.transpose(ps[1][:, jb * 16:(jb + 1) * 16], cum[:, jb * 128:(jb + 1) * 128], ident[0:16, 0:16]),
                 reads=[cum_b, cb], writes=[pb[1]])
        cp(ck[:].rearrange("p n h -> p (n h)"), ps[1][:, 0:NB * 16], [pb[1]], [ck_b])
        R = P.sb([16, 16, NT], F32); R_b = Buf()
        cmid = cum[:].rearrange("h (t s) -> h t s", s=512)[:, :, 255]
        for h in range(16):
            ts(R[:, h, :], cmid, ident[0:16, h:h + 1], ALU.mult, [cum_b, cb], [R_b])
        mm(ps[2][:, 0:16 * NT], ones32[0:16, :], R[:].rearrange("k h t -> k (h t)"), True, True, [cb, R_b], [pb[2]])
        cq = P.sb([128, 16, NT], F32); cq_b = Buf()
        cp(cq[:].rearrange("p h t -> p (h t)"), ps[2][:, 0:16 * NT], [pb[2]], [cq_b])
        for h in range(16):
            for t in range(NT):
                ts(btab[:, h, t, :], ck[:, :, h], cq[:, h, t:t + 1], ALU.subtract, [ck_b, cq_b], [bt_b])
        P.sb_off = mark

    forget_tables()
    P.barrier()
    for j in range(8):
        attention_pair("fox", w_in_o_v, "w_in_o", j * 128, 1024 + j * 128, 2048 + j * 128, j, btab=btab, hidx=2 * j)
    P.barrier()
    out_proj(yT, y_b, "w_out_o", h2T, h3T)
    P.barrier()
    P.sb_off = persist_mark
    if stop == "mix1":
        finish(h3T); P.emit(); return nc


    def moe_sparse():
        I32 = mybir.dt.int32
        gain = gsb["fn_o"]
        mark0 = P.sb_off
        eq1A = P.sb([128, NB, 8], F32); eq2A = P.sb([128, NB, 8], F32); rankA = P.sb([128, NB, 8], F32)
        gA = P.sb([128, NB, 2], F32)
        run = P.sb([128, 8], F32)
        rt_b = Buf()
        posF = P.sb([128, 2, NB], F32); posI = P.sb([128, 2, NB], I32); pos_b = Buf()
        wiF = P.sb([128, NS], F32); wiI = P.sb([128, NS], I32); wi_b = Buf()
        iot = P.sb([128, 22], F32); iot_b = Buf()
        dma("sp", iot[:], iot_d, writes=[iot_b])
        P.op("dve", lambda e: e.memset(run[:], 0.0), writes=[rt_b])
        hn_db = Buf(); h3_db = Buf(); xs_db = Buf(); ys_db = Buf()
        mark1 = P.sb_off
        accs1 = [(P.sb([128, 8, 512], F32), Buf()) for _ in range(2)]
        sq = (P.sb([128, 8, 512], BF16), Buf())
        r = P.sb([128, 512], F32); r_b2 = Buf()
        hnb = P.sb([128, 8, 512], BF16); hnb_b = Buf()
        hn32 = P.sb([128, 8, 512], F32); hn32_b = Buf()
        wr = P.sb([128, 8, 8], F32); wr_b = Buf()
        dma("sp", wr[:], wr_o.rearrange("(c p) e -> p c e", p=128), writes=[wr_b])
        tokb = [(P.sb([128, D], BF16), Buf()) for _ in range(2)]
        tok32 = [(P.sb([128, D], F32), Buf()) for _ in range(2)]
        smalls = [(P.sb([128, 8], F32), P.sb([128, 8], F32), P.sb([128, 8], F32), P.sb([128, 8], F32), Buf()) for _ in range(2)]
        lg, lg2, msk, sm_, r_b = smalls[0]
        def load_acc1(t):
            a_, ab_ = accs1[t % 2]
            dma("sp", a_[:], view_T(h3T)[:, :, t * 512:(t + 1) * 512], reads=[hb[id(h3T)]], writes=[ab_])

        load_acc1(0)
        for t in range(NT):
            acc, acc_b = accs1[t % 2]
            if t + 1 < NT:
                load_acc1(t + 1)
            act(sq[0][:], acc[:], AF.Square, [acc_b], [sq[1]])
            for c in range(8):
                mm(ps[0][:], onesb, sq[0][:, c, :], c == 0, c == 7, [cb, sq[1]], [pb[0]])
            act(r[:], ps[0][:], AF.Sqrt, [pb[0]], [r_b2], bias=EPS, scale=1.0 / D)
            recip(r[:], r[:], [r_b2], [r_b2])
            for c in range(8):
                stt(hn32[:, c, :], acc[:, c, :], gain[:, c:c + 1], r[:], ALU.mult, ALU.mult, [acc_b, r_b2, gb], [hn32_b])
            cp(hnb[:], hn32[:], [hn32_b], [hnb_b], eng="act")
            for blk in range(4):
                b = t * 4 + blk
                bs = slice(blk * 128, (blk + 1) * 128)
                lg, lg2, msk, sm_, r_b = smalls[b % 2]
                for c in range(8):
                    mm(ps[1][:, 0:8], hn32[:, c, bs], wr[:, c, :], c == 0, c == 7, [hn32_b, wr_b], [pb[1]])
                cp(lg[:], ps[1][:, 0:8], [pb[1]], [r_b])
                P.op("dve", lambda e, sm_=sm_, lg=lg: e.reduce_max(out=sm_[:, 0:1], in_=lg[:], axis=AX.X), reads=[r_b], writes=[r_b])
                ts(eq1A[:, b, :], lg[:], sm_[:, 0:1], ALU.is_equal, [r_b], [rt_b])
                stt(lg2[:], eq1A[:, b, :], -1e30, lg[:], ALU.mult, ALU.add, [r_b, rt_b], [r_b])
                P.op("dve", lambda e, sm_=sm_, lg2=lg2: e.reduce_max(out=sm_[:, 1:2], in_=lg2[:], axis=AX.X), reads=[r_b], writes=[r_b])
                ts(eq2A[:, b, :], lg2[:], sm_[:, 1:2], ALU.is_equal, [r_b], [rt_b])
                tt(sm_[:, 2:3], sm_[:, 1:2], sm_[:, 0:1], ALU.subtract, [r_b], [r_b])
                act(sm_[:, 3:4], sm_[:, 2:3], AF.Exp, [r_b], [r_b])
                ts(sm_[:, 4:5], sm_[:, 3:4], 1.0, ALU.add, [r_b], [r_b])
                recip(gA[:, b, 0:1], sm_[:, 4:5], [r_b], [rt_b])
                tt(gA[:, b, 1:2], sm_[:, 3:4], gA[:, b, 0:1], ALU.mult, [r_b, rt_b], [rt_b])
                tt(msk[:], eq1A[:, b, :], eq2A[:, b, :], ALU.add, [rt_b], [r_b])
                mm(ps[2][:, 0:8], maskS32, msk[:], True, True, [cb, r_b], [pb[2]])
                mm(ps[2][:, 8:16], ones32, msk[:], True, True, [cb, r_b], [pb[2]], skip=True)
                tt(rankA[:, b, :], ps[2][:, 0:8], run[:], ALU.add, [pb[2], rt_b], [rt_b])
                tt(run[:], ps[2][:, 8:16], run[:], ALU.add, [pb[2], rt_b], [rt_b])
                tb_, tb_b = tokb[b % 2]
                t32, t32_b = tok32[b % 2]
                p3 = ps[3][:].bitcast(BF16)
                for c in range(8):
                    P.op("pe", lambda e, c=c, bs=bs: e.transpose(p3[:, c * 128:(c + 1) * 128], hnb[:, c, bs], identb),
                         reads=[hnb_b, cb], writes=[pb[3]])
                cp(tb_[:], p3[:, 0:D], [pb[3]], [tb_b], eng="act")
                dma("sp", Hn_d[b * 128:(b + 1) * 128, :], tb_[:], reads=[tb_b], writes=[hn_db])
                for c in range(8):
                    bk = 4 + c // 4
                    P.op("pe", lambda e, c=c, bs=bs, bk=bk, acc=acc: e.transpose(ps[bk][:, (c % 4) * 128:(c % 4 + 1) * 128], acc[:, c, bs], ident),
                         reads=[acc_b, cb], writes=[pb[bk]])
                cp(t32[:, 0:512], ps[4][:], [pb[4]], [t32_b], eng="act")
                cp(t32[:, 512:1024], ps[5][:], [pb[5]], [t32_b], eng="dve")
                dma("sp", H3_d[b * 128:(b + 1) * 128, :], t32[:], reads=[t32_b], writes=[h3_db])
        til = P.sb([128, 8], F32); offe = P.sb([128, 8], F32); off0 = P.sb([128, 8], F32)
        tmpA = P.sb([128, NB, 8], F32)
        es = P.sb([128, NS], F32); es2 = P.sb([128, NS], F32); tmp8 = P.sb([128, 8], F32)
        P.op("dve", lambda e: e.memset(til[:], 0.0), writes=[r_b])
        for k in range(NT * 2 + 1):
            stt(til[:], run[:], 512.0 * k, til[:], ALU.is_gt, ALU.add, [rt_b, r_b], [r_b])
        cp(offe[:, 0:1], til[:, 0:1], [r_b], [r_b])
        for e_ in range(1, 8):
            tt(offe[:, e_:e_ + 1], offe[:, e_ - 1:e_], til[:, e_:e_ + 1], ALU.add, [r_b], [r_b])
        ts(offe[:], offe[:], 512.0, ALU.mult, [r_b], [r_b])
        stt(off0[:], til[:], -512.0, offe[:], ALU.mult, ALU.add, [r_b], [r_b])
        for k_, eqA in enumerate((eq1A, eq2A)):
            tt(tmpA[:], rankA[:], off0[:, None, :].to_broadcast([128, NB, 8]), ALU.add, [rt_b, r_b], [r_b])
            tt(tmpA[:], tmpA[:], eqA[:], ALU.mult, [r_b, rt_b], [r_b])
            P.op("dve", lambda e, k_=k_: e.reduce_sum(out=posF[:, k_, :], in_=tmpA[:], axis=AX.X), reads=[r_b], writes=[pos_b])
        cp(posI[:], posF[:], [pos_b], [pos_b])
        for s_ in range(NS):
            ts(tmp8[:], offe[:], 512.0 * s_, ALU.is_le, [r_b], [r_b])
            P.op("dve", lambda e, s_=s_: e.reduce_sum(out=es[:, s_:s_ + 1], in_=tmp8[:], axis=AX.X), reads=[r_b], writes=[r_b])
        usedF = P.sb([128, NS], F32); usedI = P.sb([128, NS], I32)
        for s_ in range(NS):
            ts(usedF[:, s_:s_ + 1], offe[:, 7:8], 512.0 * s_, ALU.is_gt, [r_b], [pos_b])
        if DEBUG_ZERO_FLAGS:
            P.op("dve", lambda e: e.memset(usedF[:], 0.0), writes=[pos_b])
        cp(usedI[:], usedF[:], [pos_b], [pos_b])
        ts(es[:], es[:], 7.0, ALU.min, [r_b], [r_b])
        stt(wiF[:], es[:], 128.0, iot[:, 0:1].to_broadcast([128, NS]), ALU.mult, ALU.add, [iot_b, r_b], [wi_b])
        cp(wiI[:], wiF[:], [wi_b], [wi_b])
        for b in range(NB):
            tb_, tb_b = tokb[b % 2]
            dma("sp", tb_[:], Hn_d[b * 128:(b + 1) * 128, :], reads=[hn_db], writes=[tb_b])
            for k_ in range(2):
                P.op("pool", lambda e, tb_=tb_, k_=k_, b=b: e.indirect_dma_start(
                    out=Xs_d, out_offset=bass.IndirectOffsetOnAxis(ap=posI[:, k_, b:b + 1], axis=0), in_=tb_[:], in_offset=None),
                    reads=[tb_b, pos_b], writes=[xs_db], dma=True)
        P.barrier()
        P.sb_off = mark1
        NH = NF // 2
        wgu = [(P.sb([128, 2, 8, HF], BF16), Buf()) for _ in range(2)]
        wd = P.sb([128, NF, D], BF16); wd_b = Buf()
        aT = P.sb([128, NF, 512], BF16); a_b = Buf()
        xtok = P.sb([128, 4, D], BF16); xtok_b = Buf()
        xgT = P.sb([128, 8, 512], BF16); xg_b = Buf()
        yst = [(P.sb([128, D], F32), Buf()) for _ in range(2)]
        s_sb = [(P.sb([128, 512], F32), Buf()) for _ in range(2)]
        wg2 = [wb["wg_m"][h] for h in range(2)]
        wu2 = [wb["wu_m"][h] for h in range(2)]
        wd2 = [wb["wd_m"][h] for h in range(2)]
        allw = wbuf["wg_m"] + wbuf["wu_m"] + wbuf["wd_m"]

        def gather(out, src, idx_ap, reads, writes):
            P.op("pool", lambda e: e.indirect_dma_start(out=out, out_offset=None, in_=src,
                                                        in_offset=bass.IndirectOffsetOnAxis(ap=idx_ap, axis=0)),
                 reads=reads, writes=writes, dma=True)

        def load_gu(s_, h):
            wt, wt_b = wgu[h]
            gather(wt[:, 0, :, :].rearrange("p c n -> p (c n)"), wg2[h], wiI[:, s_:s_ + 1], [wi_b] + allw, [wt_b])
            gather(wt[:, 1, :, :].rearrange("p c n -> p (c n)"), wu2[h], wiI[:, s_:s_ + 1], [wi_b] + allw, [wt_b])

        def load_d(s_):
            for h in range(2):
                gather(wd[:, h * NH:(h + 1) * NH, :].rearrange("p f n -> p (f n)"), wd2[h], wiI[:, s_:s_ + 1], [wi_b] + allw, [wd_b])

        load_gu(0, 0)
        load_gu(0, 1)
        it_y = 0
        def load_x(s_):
            dma("sp", xtok[:], Xs_d[s_ * 512:(s_ + 1) * 512, :].rearrange("(j p) d -> p j d", p=128), reads=[xs_db], writes=[xtok_b])

        load_x(0)
        for s_ in range(NS):
            if SKIP_SLOTS and s_ >= (2 * S) // 512:
                P.cur_grp = s_
                P.grp_flag[s_] = usedI[0:1, s_:s_ + 1]
            p0 = ps[0][:].bitcast(BF16)
            for c in range(8):
                for j in range(4):
                    P.op("pe", lambda e, c=c, j=j: e.transpose(p0[:, j * 128:(j + 1) * 128], xtok[:, j, c * 128:(c + 1) * 128], identb),
                         reads=[xtok_b, cb], writes=[pb[0]])
                cp(xgT[:, c, :], p0[:, 0:512], [pb[0]], [xg_b], eng=("act" if c % 2 == 0 else "dve"))
            if s_ + 1 < NS:
                load_x(s_ + 1)
            load_d(s_)
            for h in range(2):
                wt, wt_b = wgu[h]
                for fl in range(NH):
                    f = h * NH + fl
                    gbk, ubk = 4 + (f % 2) * 2, 5 + (f % 2) * 2
                    for c in range(8):
                        mm(ps[gbk][:], wt[:, 0, c, fl * 128:(fl + 1) * 128], xgT[:, c, :], c == 0, c == 7, [wt_b, xg_b], [pb[gbk]])
                    for c in range(8):
                        mm(ps[ubk][:], wt[:, 1, c, fl * 128:(fl + 1) * 128], xgT[:, c, :], c == 0, c == 7, [wt_b, xg_b], [pb[ubk]])
                    ss_, ss_b = s_sb[f % 2]
                    act(ss_[:], ps[gbk][:], AF.Silu, [pb[gbk]], [ss_b])
                    tt(aT[:, f, :], ps[ubk][:], ss_[:], ALU.mult, [pb[ubk], ss_b], [a_b])
                if s_ + 1 < NS:
                    load_gu(s_ + 1, h)
            for j in range(4):
                ys_, ys_b = yst[it_y % 2]
                it_y += 1
                for dh in range(2):
                    bk = (j * 2 + dh) % 4
                    for f in range(NF):
                        mm(ps[bk][:], aT[:, f, j * 128:(j + 1) * 128], wd[:, f, dh * 512:(dh + 1) * 512], f == 0, f == NF - 1,
                           [a_b, wd_b], [pb[bk]])
                    cp(ys_[:, dh * 512:(dh + 1) * 512], ps[bk][:], [pb[bk]], [ys_b], eng=("act" if dh == 0 else "dve"))
                dma("sp", Ys_d[s_ * 512 + j * 128:s_ * 512 + (j + 1) * 128, :], ys_[:], reads=[ys_b], writes=[ys_db])
        P.cur_grp = None
        P.barrier()
        P.sb_off = mark1
        finbc = P.sb([128, D], F32); fin_b = Buf()
        dma("sp", finbc[:], fin_row.partition_broadcast(128), writes=[fin_b])
        bufs4 = [tuple((P.sb([128, D], F32), Buf()) for _ in range(4)) for _ in range(2)]
        st4 = [(P.sb([128, 4], F32), Buf()) for _ in range(2)]
        out_b = Buf()
        def load4(b):
            (y1, y1_b), (y2, y2_b), (h3, h3_b), (o, o_b) = bufs4[b % 2]
            gather(y1[:], Ys_d, posI[:, 0, b:b + 1], [pos_b, ys_db], [y1_b])
            gather(y2[:], Ys_d, posI[:, 1, b:b + 1], [pos_b, ys_db], [y2_b])
            dma("sp", h3[:], H3_d[b * 128:(b + 1) * 128, :], reads=[h3_db], writes=[h3_b])

        load4(0)
        for b in range(NB):
            (y1, y1_b), (y2, y2_b), (h3, h3_b), (o, o_b) = bufs4[b % 2]
            st, st_b = st4[b % 2]
            if b + 1 < NB:
                load4(b + 1)
            stt(h3[:], y1[:], gA[:, b, 0:1], h3[:], ALU.mult, ALU.add, [y1_b, h3_b, rt_b], [h3_b])
            stt(h3[:], y2[:], gA[:, b, 1:2], h3[:], ALU.mult, ALU.add, [y2_b, h3_b, rt_b], [h3_b])
            P.op("act", lambda e, o=o, h3=h3, st=st: e.activation(out=o[:], in_=h3[:], func=AF.Square, accum_out=st[:, 0:1]),
                 reads=[h3_b], writes=[o_b, st_b])
            act(st[:, 1:2], st[:, 0:1], AF.Sqrt, [st_b], [st_b], bias=EPS, scale=1.0 / D)
            recip(st[:, 2:3], st[:, 1:2], [st_b], [st_b])
            stt(o[:], h3[:], st[:, 2:3], finbc[:], ALU.mult, ALU.mult, [h3_b, st_b, fin_b, o_b], [o_b])
            dma("sp", out_nat[b * 128:(b + 1) * 128, :], o[:], reads=[o_b], writes=[out_b], is_out=True)
        P.sb_off = mark0

    if sparse and stop is None:
        moe_sparse()
        P.emit()
        return nc

    ffn_phase(h3T, h4T, gsb["fn_o"], moe=True)
    P.barrier()
    if stop == "moe":
        finish(h4T); P.emit(); return nc

    mark = P.sb_off
    xbufs = [(P.sb([128, 8, 512], F32), Buf()) for _ in range(2)]
    obufs = [(P.sb([128, 8, 512], F32), Buf()) for _ in range(2)]
    sqb = (P.sb([128, 8, 512], BF16), Buf())
    rsb = (P.sb([128, 512], F32), Buf())
    norm_tiles(h4T, hb[id(h4T)], gsb["fin"], range(NT), lambda t: (obufs[t % 2][0], obufs[t % 2][1]), 0, xbufs, sqb, rsb,
               keep32=lambda t, xs, xs_b, r, r_b: dma("sp", view_T(outT)[:, :, t * 512:(t + 1) * 512], obufs[t % 2][0][:],
                                                      reads=[obufs[t % 2][1]], writes=[hb[id(outT)]], is_out=True))
    P.sb_off = mark
    P.emit()
    return nc


def make_in_maps(inputs, S, ncores):
    x = np.asarray(inputs["x"], np.float32)
    def g8(v):
        return np.ascontiguousarray(np.asarray(v, np.float32).reshape(-1, 128).T)
    common = {
        "an_e": g8(inputs["attn_norm_even"][0]), "fn_e": g8(inputs["ffn_norm_even"][0]),
        "an_o": g8(inputs["attn_norm_odd"][0]), "fn_o": g8(inputs["ffn_norm_odd"][0]),
        "fin": g8(inputs["final_norm"]), "rn_e": g8(inputs["ret_norm_even"][0]),
        "fin_row": np.ascontiguousarray(np.asarray(inputs["final_norm"], np.float32).reshape(1, -1)),
        "bf_o": np.ascontiguousarray(np.asarray(inputs["b_forget_odd"][0], np.float32).reshape(16, 1)),
        "wr_o": np.ascontiguousarray(inputs["w_router_odd"][0], dtype=np.float32),
        "w_in_e": np.ascontiguousarray(inputs["w_in_even"][0], dtype=np.float32),
        "w_out_e": np.ascontiguousarray(inputs["w_out_even"][0], dtype=np.float32),
        "wg_e": np.ascontiguousarray(inputs["w_gate_even"][0], dtype=np.float32),
        "wu_e": np.ascontiguousarray(inputs["w_up_even"][0], dtype=np.float32),
        "wd_e": np.ascontiguousarray(inputs["w_down_even"][0], dtype=np.float32),
        "w_in_o": np.ascontiguousarray(inputs["w_in_odd"][0], dtype=np.float32),
        "w_out_o": np.ascontiguousarray(inputs["w_out_odd"][0], dtype=np.float32),
        "wg_m": np.ascontiguousarray(inputs["w_gate_moe_odd"][0], dtype=np.float32),
        "wu_m": np.ascontiguousarray(inputs["w_up_moe_odd"][0], dtype=np.float32),
        "wd_m": np.ascontiguousarray(inputs["w_down_moe_odd"][0], dtype=np.float32),
    }
    common.update(host_consts(S))
    maps = []
    for b in range(ncores):
        m = dict(common)
        m["xT"] = np.ascontiguousarray(x[b, :S].T)
        maps.append(m)
    return maps


def kernel(**inputs):
    S = 4096
    nc = build(S)
    maps = make_in_maps(inputs, S, 8)
    res = run_bass_kernel_spmd(nc, maps, core_ids=list(range(8)))
    out = np.stack([np.asarray(r["out"]) for r in res.results], axis=0)
    return out.astype(np.float32)
```
